# Optimizing a Trainium2 kernel written in Bass

```python
import math
import jax, jax.numpy as jnp
from jax import lax
import numpy as np

D_MODEL = 1024
BATCH = 8
SEQ = 8192
DEPTH = 1

MIX_WIDTH = D_MODEL
GM_WIDTH = MIX_WIDTH // 2
GM_HEADS = 4
GM_HEAD_DIM = GM_WIDTH // GM_HEADS
CHUNK = 128
SSM_WIDTH = MIX_WIDTH - GM_WIDTH
SSM_GROUP = 16
SSM_GROUPS = SSM_WIDTH // SSM_GROUP
SSM_STATE = 64
IN_WIDTH = 2 * GM_WIDTH + SSM_WIDTH
PEER_HEADS = 8
PEER_NKEYS = 128
PEER_EXPERTS = PEER_NKEYS * PEER_NKEYS
PEER_TOPK = 16
PEER_DKEY = 128
PEER_BLOCK = 128
N_MOD = 6
EPS = 1e-6

kernel_name = "hybrid_gmlp_s5_peer_adaln_block"


def _rmsnorm(x, g):
    xf = x.astype(jnp.float32)
    y = xf * lax.rsqrt(jnp.mean(xf * xf, axis=-1, keepdims=True) + EPS)
    return (y * g.astype(jnp.float32)).astype(x.dtype)


def _layernorm(x, g, b):
    xf = x.astype(jnp.float32)
    mu = jnp.mean(xf, axis=-1, keepdims=True)
    var = jnp.mean(jnp.square(xf - mu), axis=-1, keepdims=True)
    y = (xf - mu) * lax.rsqrt(var + EPS)
    return (y * g.astype(jnp.float32) + b.astype(jnp.float32)).astype(x.dtype)


def _sgu(zu, zv, ln_g, ln_b, w_s, b_s):
    u = jax.nn.gelu(zu)
    v = _layernorm(jax.nn.gelu(zv), ln_g, ln_b)
    bsz, seq, _ = v.shape
    vc = v.reshape(bsz, seq // CHUNK, CHUNK, GM_HEADS, GM_HEAD_DIM)
    mask = jnp.tril(jnp.ones((CHUNK, CHUNK), dtype=w_s.dtype))
    mixed = jnp.einsum('hij,bnjhd->bnihd', w_s * mask, vc)
    mixed = mixed + jnp.transpose(b_s)[:, :, None]
    return u * mixed.reshape(bsz, seq, GM_WIDTH)


def _ssm_combine(e1, e2):
    a1, b1 = e1
    a2, b2 = e2
    return a1 * a2, a2 * b1 + b2


def _s5(u, a_re, a_im, log_dt, b_re, b_im, c_re, c_im, d_skip, w_glu, b_glu):
    f32 = jnp.float32
    bsz, seq, _ = u.shape
    lam = lax.complex(jnp.minimum(a_re.astype(f32), -1e-4), a_im.astype(f32))
    delta = jnp.exp(log_dt.astype(f32))[:, None]
    a_bar = jnp.exp(lam * delta)
    b_mat = lax.complex(b_re.astype(f32), b_im.astype(f32))
    b_bar = ((a_bar - 1.0) / lam)[:, :, None] * b_mat
    c_mat = lax.complex(c_re.astype(f32), c_im.astype(f32))
    ug = u.astype(f32).reshape(bsz, seq, SSM_GROUPS, SSM_GROUP)

    def one_sequence(us):
        bu = jnp.einsum('gph,sgh->sgp', b_bar, us.astype(jnp.complex64))
        a = jnp.broadcast_to(a_bar, bu.shape)
        _, states = lax.associative_scan(_ssm_combine, (a, bu), axis=0)
        return jnp.einsum('ghp,sgp->sgh', c_mat, states).real

    y = lax.map(one_sequence, ug) + d_skip.astype(f32) * ug
    y = jax.nn.gelu(y.reshape(bsz, seq, SSM_WIDTH))
    y = y * jax.nn.sigmoid(y @ w_glu.astype(f32) + b_glu.astype(f32))
    return y.astype(u.dtype)


def _peer(h, w_q, keys, u_tab, v_tab):
    f32 = jnp.float32
    bsz, seq, dm = h.shape
    hb = h.reshape(-1, PEER_BLOCK, dm)

    def block(t):
        tb = t.shape[0]
        q = (t @ w_q).reshape(tb, PEER_HEADS, 2, PEER_DKEY // 2)
        s = jnp.einsum('thcd,hckd->thck', q.astype(f32), keys.astype(f32))
        top_s, top_i = lax.top_k(s, PEER_TOPK)
        cand_s = (top_s[:, :, 0, :, None] + top_s[:, :, 1, None, :]).reshape(tb, PEER_HEADS, -1)
        cand_i = (top_i[:, :, 0, :, None] * PEER_NKEYS + top_i[:, :, 1, None, :]).reshape(tb, PEER_HEADS, -1)
        best_s, pos = lax.top_k(cand_s, PEER_TOPK)
        idx = jnp.take_along_axis(cand_i, pos, axis=-1)
        g = jax.nn.softmax(best_s, axis=-1)
        act = jax.nn.gelu(jnp.einsum('thkd,td->thk', u_tab[idx], t).astype(f32))
        w = (g * act).astype(t.dtype)
        return jnp.einsum('thk,thkd->td', w, v_tab[idx])

    return lax.map(block, hb).reshape(bsz, seq, dm)


def setup_inputs(seed: int = 0) -> dict:
    key = jax.random.key(seed)
    ks = jax.random.split(key, 32)
    f32 = jnp.float32

    def nrm(k, shape, scale):
        return jax.random.normal(k, shape, f32) * scale

    L = DEPTH
    n_idx = jnp.arange(SSM_STATE, dtype=f32)
    a_im = jnp.broadcast_to(math.pi * n_idx, (L, SSM_GROUPS, SSM_STATE)) + nrm(ks[11], (L, SSM_GROUPS, SSM_STATE), 0.01)
    a_re = -0.5 + nrm(ks[10], (L, SSM_GROUPS, SSM_STATE), 0.01)
    log_dt = jax.random.uniform(ks[12], (L, SSM_GROUPS), f32, math.log(1e-3), math.log(1e-1))
    return {
        "x": nrm(ks[0], (BATCH, SEQ, D_MODEL), 1.0),
        "c": nrm(ks[1], (BATCH, D_MODEL), 1.0),
        "w_ada": nrm(ks[2], (L, D_MODEL, N_MOD * D_MODEL), 0.5 * D_MODEL ** -0.5),
        "b_ada": nrm(ks[3], (L, N_MOD * D_MODEL), 0.02),
        "g_mix": 1.0 + nrm(ks[4], (L, D_MODEL), 0.02),
        "w_in": nrm(ks[5], (L, D_MODEL, IN_WIDTH), D_MODEL ** -0.5),
        "sgu_ln_g": 1.0 + nrm(ks[6], (L, GM_WIDTH), 0.02),
        "sgu_ln_b": nrm(ks[7], (L, GM_WIDTH), 0.02),
        "w_s": nrm(ks[8], (L, GM_HEADS, CHUNK, CHUNK), CHUNK ** -0.5),
        "b_s": 1.0 + nrm(ks[9], (L, GM_HEADS, CHUNK), 0.02),
        "ssm_a_re": a_re,
        "ssm_a_im": a_im,
        "ssm_log_dt": log_dt,
        "ssm_b_re": nrm(ks[13], (L, SSM_GROUPS, SSM_STATE, SSM_GROUP), (2 * SSM_GROUP) ** -0.5),
        "ssm_b_im": nrm(ks[14], (L, SSM_GROUPS, SSM_STATE, SSM_GROUP), (2 * SSM_GROUP) ** -0.5),
        "ssm_c_re": nrm(ks[15], (L, SSM_GROUPS, SSM_GROUP, SSM_STATE), 2.0 * SSM_STATE ** -0.5),
        "ssm_c_im": nrm(ks[16], (L, SSM_GROUPS, SSM_GROUP, SSM_STATE), 2.0 * SSM_STATE ** -0.5),
        "ssm_d": nrm(ks[17], (L, SSM_GROUPS, SSM_GROUP), 0.5),
        "w_glu": nrm(ks[18], (L, SSM_WIDTH, SSM_WIDTH), SSM_WIDTH ** -0.5),
        "b_glu": nrm(ks[19], (L, SSM_WIDTH), 0.02),
        "w_out": nrm(ks[20], (L, MIX_WIDTH, D_MODEL), MIX_WIDTH ** -0.5),
        "g_ffn": 1.0 + nrm(ks[21], (L, D_MODEL), 0.02),
        "w_q": nrm(ks[22], (L, D_MODEL, PEER_HEADS * PEER_DKEY), D_MODEL ** -0.5),
        "peer_keys": nrm(ks[23], (L, PEER_HEADS, 2, PEER_NKEYS, PEER_DKEY // 2), (PEER_DKEY // 2) ** -0.5),
        "peer_u": nrm(ks[24], (L, PEER_EXPERTS, D_MODEL), D_MODEL ** -0.5),
        "peer_v": nrm(ks[25], (L, PEER_EXPERTS, D_MODEL), PEER_HEADS ** -0.5),
        "g_final": 1.0 + nrm(ks[26], (D_MODEL,), 0.02),
    }


def reference(x, c, w_ada, b_ada, g_mix, w_in, sgu_ln_g, sgu_ln_b, w_s, b_s,
              ssm_a_re, ssm_a_im, ssm_log_dt, ssm_b_re, ssm_b_im, ssm_c_re, ssm_c_im,
              ssm_d, w_glu, b_glu, w_out, g_ffn, w_q, peer_keys, peer_u, peer_v, g_final):
    cond = jax.nn.silu(c)
    for l in range(DEPTH):
        mod = cond @ w_ada[l] + b_ada[l]
        sh1, sc1, gt1, sh2, sc2, gt2 = jnp.split(mod[:, None, :], N_MOD, axis=-1)

        h = _rmsnorm(x, g_mix[l]) * (1.0 + sc1) + sh1
        z = h @ w_in[l]
        zu = z[..., :GM_WIDTH]
        zv = z[..., GM_WIDTH:2 * GM_WIDTH]
        zs = z[..., 2 * GM_WIDTH:]
        y_gm = _sgu(zu, zv, sgu_ln_g[l], sgu_ln_b[l], w_s[l], b_s[l])
        y_ssm = _s5(zs, ssm_a_re[l], ssm_a_im[l], ssm_log_dt[l], ssm_b_re[l], ssm_b_im[l],
                    ssm_c_re[l], ssm_c_im[l], ssm_d[l], w_glu[l], b_glu[l])
        y = jnp.concatenate([y_gm, y_ssm], axis=-1) @ w_out[l]
        x = x + gt1 * y

        h2 = _rmsnorm(x, g_ffn[l]) * (1.0 + sc2) + sh2
        x = x + gt2 * _peer(h2, w_q[l], peer_keys[l], peer_u[l], peer_v[l])
    return _rmsnorm(x, g_final)
```

```python
import math
from contextlib import ExitStack

import numpy as np
import concourse.bass as bass
import concourse.mybir as mybir
from concourse.bass_utils import run_bass_kernel_spmd

F32 = mybir.dt.float32
BF16 = mybir.dt.bfloat16
U32 = mybir.dt.uint32
I32 = mybir.dt.int32
AF = mybir.ActivationFunctionType
ALU = mybir.AluOpType
AX = mybir.AxisListType

D = 1024
NCORES = 8
SEQ = 8192
EPS = 1e-6
PI = math.pi
TWO_PI = 2.0 * math.pi
ENGS = ("sync", "scalar", "vector", "gpsimd", "tensor")


class _DSem:
    def __init__(self, sem, name):
        self.sem = sem
        self.name = name
        self.issued = 0


class Sched:
    def __init__(self, nc, es, n_dsem=40):
        self.nc = nc
        self.sem = {e: es.enter_context(nc.semaphore("s_" + e)) for e in ENGS}
        self.cnt = {e: 0 for e in ENGS}
        self.ops = {e: [] for e in ENGS}
        self.waited = {e: {} for e in ENGS}
        self.dsems = [_DSem(es.enter_context(nc.semaphore("d%d" % i)), "d%d" % i) for i in range(n_dsem)]
        self.last_w = {}
        self.readers = {}
        self.pending = {e: ([], []) for e in ENGS}
        self.semobj = {e: self.sem[e] for e in ENGS}
        for d in self.dsems:
            self.semobj[d.name] = d.sem
        self.n_ins = 0

    def _need(self, eng, reads, writes, skip_self=False):
        need = {}

        def add(src, val):
            if skip_self and src == eng:
                return
            if need.get(src, 0) < val:
                need[src] = val

        for k in reads:
            w = self.last_w.get(k)
            if w:
                add(*w)
        for k in writes:
            w = self.last_w.get(k)
            if w:
                add(*w)
            for r in self.readers.get(k, {}).items():
                add(*r)
        out = []
        for src, val in need.items():
            if self.waited[eng].get(src, 0) >= val:
                continue
            self.waited[eng][src] = val
            out.append((src, val))
        return out

    def _emit_waits(self, eng, waits):
        for src, val in waits:
            so = self.semobj[src]
            self.ops[eng].append(lambda e, so=so, val=val: e.wait_ge(so, val))

    def _commit(self, tag, reads, writes):
        for k in reads:
            rd = self.readers.setdefault(k, {})
            if rd.get(tag[0], 0) < tag[1]:
                rd[tag[0]] = tag[1]
        for k in writes:
            self.last_w[k] = tag
            self.readers[k] = {}

    def op(self, eng, fn, reads=(), writes=(), inc=True):
        self.n_ins += 1
        waits = self._need(eng, reads, writes, skip_self=(eng == "tensor"))
        self._emit_waits(eng, waits)
        pr, pw = self.pending[eng]
        pr.extend(reads)
        pw.extend(writes)
        if inc:
            self.cnt[eng] += 1
            so = self.sem[eng]
            self.ops[eng].append(lambda e, fn=fn, so=so: fn(e).then_inc(so, 1))
            self._commit((eng, self.cnt[eng]), pr, pw)
            self.pending[eng] = ([], [])
        else:
            self.ops[eng].append(lambda e, fn=fn: fn(e))

    def dma(self, eng, out, in_, reads=(), writes=(), dsem=0, **kw):
        self.n_ins += 1
        d = self.dsems[dsem]
        waits = self._need(eng, reads, writes)
        if d.issued and self.waited[eng].get(d.name, 0) < 16 * d.issued:
            self.waited[eng][d.name] = 16 * d.issued
            waits.append((d.name, 16 * d.issued))
        self._emit_waits(eng, waits)
        d.issued += 1
        so = d.sem
        self.ops[eng].append(
            lambda e, so=so, out=out, in_=in_, kw=kw: e.dma_start(out=out, in_=in_, **kw).then_inc(so, 16))
        self._commit((d.name, 16 * d.issued), reads, writes)

    def wait_all(self, eng, keys):
        self._emit_waits(eng, self._need(eng, keys, ()))

    def barrier(self):
        for e in ENGS:
            assert not self.pending[e][0] and not self.pending[e][1], e
        for e in ENGS:
            waits = []
            for src in ENGS:
                if src == e and e == "tensor":
                    continue
                val = self.cnt[src]
                if val and self.waited[e].get(src, 0) < val:
                    self.waited[e][src] = val
                    waits.append((src, val))
            for d in self.dsems:
                val = 16 * d.issued
                if val and self.waited[e].get(d.name, 0) < val:
                    self.waited[e][d.name] = val
                    waits.append((d.name, val))
            self._emit_waits(e, waits)

    def replay(self):
        with self.nc.Block() as block:
            for name in ENGS:
                ops = self.ops[name]

                def body(e, ops=ops):
                    for f in ops:
                        f(e)

                getattr(block, name)(body)
        self.ops = {e: [] for e in ENGS}

    def flush(self):
        self.barrier()
        self.replay()


INTERLEAVE = True


def build_program(T=SEQ, debug=False, TB=256, GI=4):
    assert T % TB == 0 and TB % 128 == 0
    NCH = T // 128
    NBLK = T // TB
    NSUB = TB // 128
    nc = bass.Bass("TRN2", target_bir_lowering=False)

    def din(name, shape, dt=F32):
        return nc.dram_tensor(name, list(shape), dt, kind="ExternalInput").ap()

    x_d = din("x", [T, D])
    cvec_d = din("cvec", [128, 8])
    wada_d = din("w_ada", [D, 6 * D])
    badac_d = din("b_ada_c", [128, 48])
    badar_d = din("b_ada_r", [1, 6 * D])
    gmix_d = din("g_mix_c", [128, 8])
    gffn_d = din("g_ffn_c", [128, 8])
    win_d = din("w_in", [D, 1536])
    wout_d = din("w_out", [D, D])
    wq_d = din("w_q", [D, D])
    wglu_d = din("w_glu", [512, 512])
    lng_d = din("ln_g_r", [1, 512])
    lnb_d = din("ln_b_r", [1, 512])
    bgluc_d = din("b_glu_c", [128, 4])
    wsT_d = din("w_sT", [128, 4, 128])
    bsc_d = din("b_s_c", [128, 4])
    are_r_d = din("a_re_r", [1, 2048])
    aim_r_d = din("a_im_r", [1, 2048])
    ldt_r_d = din("ldt_r", [1, 2048])
    are_c_d = din("a_re_c", [128, 16])
    aim_c_d = din("a_im_c", [128, 16])
    ldt_c_d = din("ldt_c", [128, 16])
    BR_d = din("BR", [128, 4, 512])
    BI_d = din("BI", [128, 4, 512])
    CR_d = din("CR", [128, 16, 32])
    CI_d = din("CI", [128, 16, 32])
    Dd_d = din("Dd", [128, 4, 128])
    keys_d = din("keysblk", [128, 8, 256])
    uT_d = din("uT", [D, 16384])
    v_d = din("v", [16384, D])
    gfin_d = din("g_final_r", [1, D])
    out_d = nc.dram_tensor("out", [T, D], F32, kind="ExternalOutput").ap()

    NG = 128 // GI
    uTg_d = nc.dram_tensor("uTgs", [NG, 128, 8, GI * 128], BF16, kind="Internal").ap()
    vg_d = nc.dram_tensor("vgs", [NG, 128, GI, D], BF16, kind="Internal").ap()
    x1s_d = nc.dram_tensor("x1s", [T, D], F32, kind="Internal").ap()

    dbg_outs = {}

    with ExitStack() as es:
        S = Sched(nc, es)

        def V(fn, r=(), w=(), inc=True):
            S.op("vector", fn, r, w, inc)

        def G(fn, r=(), w=(), inc=True):
            S.op("gpsimd", fn, r, w, inc)

        def A(fn, r=(), w=(), inc=True):
            S.op("scalar", fn, r, w, inc)

        def PE(fn, r=(), w=(), inc=True):
            S.op("tensor", fn, r, w, inc)

        dsn = [8]

        def next_ds():
            dsn[0] += 1
            if dsn[0] >= 40:
                dsn[0] = 8
            return dsn[0]

        def dump(name, ap, key, shape, dt=F32):
            if not debug:
                return
            o = nc.dram_tensor("dbg_" + name, list(shape), dt, kind="ExternalOutput").ap()
            dbg_outs["dbg_" + name] = o
            S.dma("sync", o, ap, reads=[key], writes=["dbg_" + name], dsem=next_ds())

        def sb(stack, name, shape, dt):
            return stack.enter_context(nc.sbuf_tensor(name, list(shape), dt))

        def ps(stack, name, shape, dt):
            return stack.enter_context(nc.psum_tensor(name, list(shape), dt))

        P = [ps(es, "P%d" % i, [128, 512], F32) for i in range(7)]
        PT = ps(es, "PT", [128, 1024], BF16)
        PK = ["P%d" % i for i in range(7)]

        identb = sb(es, "identb", [128, 128], BF16)
        identf = sb(es, "identf", [128, 128], F32)
        trib = sb(es, "trib", [128, 128], BF16)
        iota_f = sb(es, "iota_f", [128, 128], F32)
        iota_b = sb(es, "iota_b", [128, 128], BF16)
        jcol = sb(es, "jcol", [128, 1], F32)
        njcol = sb(es, "njcol", [128, 1], F32)
        mhalf = sb(es, "mhalf", [128, 1], F32)
        a1c = sb(es, "a1c", [128, 8], F32)
        sh1c = sb(es, "sh1c", [128, 8], F32)
        a2c = sb(es, "a2c", [128, 8], F32)
        sh2c = sb(es, "sh2c", [128, 8], F32)
        gt1r = sb(es, "gt1r", [128, D], F32)
        gt2r = sb(es, "gt2r", [128, D], F32)
        gfr = sb(es, "gfr", [128, D], F32)

        G(lambda e: e.iota(iota_f[:], [[1, 128]], base=0, channel_multiplier=0, allow_small_or_imprecise_dtypes=True), w=["iota_f"])
        G(lambda e: e.iota(jcol[:], [[0, 1]], base=0, channel_multiplier=1, allow_small_or_imprecise_dtypes=True), w=["jcol"])
        V(lambda e: e.tensor_copy(iota_b[:], iota_f[:]), r=["iota_f"], w=["iota_b"])
        V(lambda e: e.tensor_scalar(njcol[:], jcol[:], -1.0, None, ALU.mult), r=["jcol"], w=["njcol"])
        V(lambda e: e.memset(mhalf[:], -0.5), w=["mhalf"])
        V(lambda e: e.tensor_scalar(identf[:], iota_f[:], jcol[:, 0:1], None, ALU.is_equal), r=["iota_f", "jcol"], w=["identf"])
        V(lambda e: e.tensor_copy(identb[:], identf[:]), r=["identf"], w=["identb"])
        V(lambda e: e.tensor_scalar(trib[:], iota_f[:], jcol[:, 0:1], None, ALU.is_ge), r=["iota_f", "jcol"], w=["trib"])
        S.dma("sync", gfr[:], gfin_d.partition_broadcast(128), writes=["gfr"], dsem=0)

        uT_v = uT_d.rearrange("(k p) e -> p k e", p=128)
        for g in range(NG):
            S.dma("gpsimd", uTg_d[g], uT_v[:, :, g * GI * 128:(g + 1) * GI * 128], writes=["uTg"], dsem=1)
        for g in range(NG):
            S.dma("gpsimd", vg_d[g], v_d[g * GI * 128:(g + 1) * GI * 128, :].rearrange("(il j) d -> j il d", j=128), writes=["vg"], dsem=2)

        with ExitStack() as sa:
            cv = sb(sa, "cv", [128, 8], F32)
            cond = sb(sa, "cond", [128, 8], F32)
            condrep = sb(sa, "condrep", [128, 8, 128], F32)
            wp = [sb(sa, "wadap%d" % i, [128, 8, D], F32) for i in range(2)]
            badac = sb(sa, "badac", [128, 48], F32)
            badar = sb(sa, "badar", [128, 2, D], F32)
            modc = sb(sa, "modc", [128, 48], F32)
            gmixc = sb(sa, "gmixc", [128, 8], F32)
            gffnc = sb(sa, "gffnc", [128, 8], F32)
            S.dma("sync", cv[:], cvec_d, writes=["cv"], dsem=3)
            S.dma("sync", badac[:], badac_d, writes=["badac"], dsem=4)
            S.dma("sync", badar[:, 0, :], badar_d[:, 2 * D:3 * D].partition_broadcast(128), writes=["badar0"], dsem=5)
            S.dma("sync", badar[:, 1, :], badar_d[:, 5 * D:6 * D].partition_broadcast(128), writes=["badar1"], dsem=6)
            S.dma("sync", gmixc[:], gmix_d, writes=["gmixc"], dsem=7)
            S.dma("sync", gffnc[:], gffn_d, writes=["gffnc"], dsem=3)
            A(lambda e: e.activation(cond[:], cv[:], AF.Silu), r=["cv"], w=["cond"])
            V(lambda e: e.tensor_copy(condrep[:], cond[:].unsqueeze(2).to_broadcast([128, 8, 128])), r=["cond"], w=["condrep"])
            wada_v = wada_d.rearrange("(k p) n -> p k n", p=128)
            for piece in range(6):
                sl = piece % 2
                S.dma("sync", wp[sl][:], wada_v[:, :, piece * D:(piece + 1) * D], writes=["wp%d" % sl], dsem=4 + sl)
                if piece in (2, 5):
                    ri = 0 if piece == 2 else 1
                    dst = gt1r if piece == 2 else gt2r
                    for half in range(2):
                        for k in range(8):
                            PE(lambda e, k=k, half=half, sl=sl: e.matmul(
                                P[half][:], condrep[:, k, :], wp[sl][:, k, half * 512:(half + 1) * 512],
                                start=(k == 0), stop=(k == 7)),
                               r=["condrep", "wp%d" % sl], w=[PK[half]], inc=(k == 7))
                        V(lambda e, half=half, ri=ri, dst=dst: e.tensor_tensor(
                            dst[:, half * 512:(half + 1) * 512], P[half][:], badar[:, ri, half * 512:(half + 1) * 512], ALU.add),
                          r=[PK[half], "badar%d" % ri], w=["gtr%d" % ri])
                else:
                    for nl in range(8):
                        col = piece * 8 + nl
                        for k in range(8):
                            PE(lambda e, k=k, nl=nl, sl=sl, col=col: e.matmul(
                                P[2][:, col:col + 1], wp[sl][:, k, nl * 128:(nl + 1) * 128], cond[:, k:k + 1],
                                start=(k == 0), stop=(k == 7)),
                               r=["cond", "wp%d" % sl], w=["P2"], inc=(k == 7 and nl == 7))
            V(lambda e: e.memset(modc[:], 0.0), w=["modc"])
            V(lambda e: e.tensor_tensor(modc[:, 0:16], P[2][:, 0:16], badac[:, 0:16], ALU.add), r=["P2", "badac"], w=["modc"])
            V(lambda e: e.tensor_tensor(modc[:, 24:40], P[2][:, 24:40], badac[:, 24:40], ALU.add), r=["P2", "badac"], w=["modc"])
            V(lambda e: e.tensor_copy(sh1c[:], modc[:, 0:8]), r=["modc"], w=["sh1c"])
            V(lambda e: e.tensor_copy(sh2c[:], modc[:, 24:32]), r=["modc"], w=["sh2c"])
            V(lambda e: e.scalar_tensor_tensor(a1c[:], modc[:, 8:16], 1.0, gmixc[:], ALU.add, ALU.mult), r=["modc", "gmixc"], w=["a1c"])
            V(lambda e: e.scalar_tensor_tensor(a2c[:], modc[:, 32:40], 1.0, gffnc[:], ALU.add, ALU.mult), r=["modc", "gffnc"], w=["a2c"])
            dump("a1c", a1c[:], "a1c", [128, 8])
            dump("sh1c", sh1c[:], "sh1c", [128, 8])
            dump("a2c", a2c[:], "a2c", [128, 8])
            dump("gt1r", gt1r[:], "gtr0", [128, D])
            dump("gt2r", gt2r[:], "gtr1", [128, D])
            S.flush()

        with ExitStack() as pa:
            win = sb(pa, "win", [128, 8, 1536], BF16)
            wout = sb(pa, "wout", [128, 8, D], BF16)
            wglu = sb(pa, "wglu", [128, 4, 512], BF16)
            bblk = sb(pa, "bblk", [128, 4, 1024], BF16)
            crb = sb(pa, "crb", [128, 16, 32], BF16)
            cinb = sb(pa, "cinb", [128, 16, 32], BF16)
            ncrb = sb(pa, "ncrb", [128, 16, 32], BF16)
            ntrib = sb(pa, "ntrib", [128, 128], BF16)
            ddb = sb(pa, "ddb", [128, 4, 128], BF16)
            wmT = sb(pa, "wmT", [128, 4, 128], BF16)
            bsc = sb(pa, "bsc", [128, 4], F32)
            bgluc = sb(pa, "bgluc", [128, 4], F32)
            lngr = sb(pa, "lngr", [128, 512], F32)
            lnbr = sb(pa, "lnbr", [128, 512], F32)
            Mre = sb(pa, "Mre", [128, 2048], F32)
            Mim = sb(pa, "Mim", [128, 2048], F32)
            Pre = sb(pa, "Pre", [128, 16, 128], F32)
            Pim = sb(pa, "Pim", [128, 16, 128], F32)
            A128re = sb(pa, "A128re", [128, 16], F32)
            A128im = sb(pa, "A128im", [128, 16], F32)
            Kre = sb(pa, "Kre", [128, 16], F32)
            Kim = sb(pa, "Kim", [128, 16], F32)

            S.dma("gpsimd", win[:], win_d.rearrange("(k p) n -> p k n", p=128), writes=["win"], dsem=20)
            S.dma("gpsimd", wglu[:], wglu_d.rearrange("(k p) n -> p k n", p=128), writes=["wglu"], dsem=20)
            S.dma("gpsimd", crb[:], CR_d, writes=["crb"], dsem=20)
            S.dma("gpsimd", ddb[:], Dd_d, writes=["ddb"], dsem=20)
            S.dma("sync", bsc[:], bsc_d, writes=["bsc"], dsem=4)
            S.dma("sync", bgluc[:], bgluc_d, writes=["bgluc"], dsem=4)
            S.dma("sync", lngr[:], lng_d.partition_broadcast(128), writes=["lngr"], dsem=5)
            S.dma("sync", lnbr[:], lnb_d.partition_broadcast(128), writes=["lnbr"], dsem=5)
            V(lambda e: e.memset(Kre[:], 0.0), w=["Kre"])
            V(lambda e: e.memset(Kim[:], 0.0), w=["Kim"])

            with ExitStack() as s1_:
                woutf = sb(s1_, "woutf", [128, 8, D], F32)
                S.dma("sync", woutf[:], wout_d.rearrange("(k p) n -> p k n", p=128), writes=["woutf"], dsem=6)
                for k in range(8):
                    V(lambda e, k=k: e.tensor_tensor(wout[:, k, :], woutf[:, k, :], gt1r[:], ALU.mult),
                      r=["woutf", "gtr0"], w=["wout"])
                wsTf = sb(s1_, "wsTf", [128, 4, 128], F32)
                S.dma("sync", wsTf[:], wsT_d, writes=["wsTf"], dsem=7)
                V(lambda e: e.tensor_tensor(wmT[:], wsTf[:], trib[:].unsqueeze(1).to_broadcast([128, 4, 128]), ALU.mult),
                  r=["wsTf", "trib"], w=["wmT"])
                cif = sb(s1_, "cif", [128, 16, 32], F32)
                S.dma("sync", cif[:], CI_d, writes=["cif"], dsem=7)
                V(lambda e: e.tensor_scalar(cinb[:], cif[:], -1.0, None, ALU.mult), r=["cif"], w=["cinb"])
                V(lambda e: e.tensor_scalar(ncrb[:], crb[:], -1.0, None, ALU.mult), r=["crb"], w=["ncrb"])
                V(lambda e: e.tensor_scalar(ntrib[:], trib[:], -1.0, None, ALU.mult), r=["trib"], w=["ntrib"])
                S.flush()

            with ExitStack() as ss_:
                W_ = 512

                def tmp(name, dt=F32):
                    return sb(ss_, name, [128, W_], dt)

                def sincos(ang, ak, osin, ks, ocos, kc, ti, kti, ap=lambda t: t[:]):
                    V(lambda e: e.tensor_scalar(ap(ocos), ap(ang), 1.0 / TWO_PI, None, ALU.mult), r=[ak], w=[kc])
                    V(lambda e: e.tensor_copy(ap(ti), ap(ocos)), r=[kc], w=[kti])
                    V(lambda e: e.tensor_copy(ap(ocos), ap(ti)), r=[kti], w=[kc])
                    V(lambda e: e.scalar_tensor_tensor(ap(ang), ap(ocos), -TWO_PI, ap(ang), ALU.mult, ALU.add), r=[kc, ak], w=[ak])
                    V(lambda e: e.tensor_scalar(ap(ang), ap(ang), -PI, PI, ALU.max, ALU.min), r=[ak], w=[ak])
                    A(lambda e: e.activation(ap(osin), ap(ang), AF.Sin), r=[ak], w=[ks])
                    V(lambda e: e.tensor_scalar(ap(ocos), ap(ang), PI / 2, -TWO_PI, ALU.is_gt, ALU.mult), r=[ak], w=[kc])
                    V(lambda e: e.scalar_tensor_tensor(ap(ocos), ap(ang), PI / 2, ap(ocos), ALU.add, ALU.add), r=[ak, kc], w=[kc])
                    V(lambda e: e.tensor_scalar(ap(ocos), ap(ocos), -PI, PI, ALU.max, ALU.min), r=[kc], w=[kc])
                    A(lambda e: e.activation(ap(ocos), ap(ocos), AF.Sin), r=[kc], w=[kc])

                ebase = tmp("ebase")
                V(lambda e: e.memset(ebase[:], math.e), w=["ebase"])
                are = tmp("are"); aim = tmp("aim"); dlt = tmp("dlt"); lr = tmp("lr"); th = tmp("th")
                ang = tmp("ang"); sn = tmp("sn"); cs_ = tmp("cs_"); ti = tmp("ti", I32); mg = tmp("mg")
                den = tmp("den"); t2 = tmp("t2"); cre = tmp("cre"); cim = tmp("cim")
                brf = tmp("brf"); bif = tmp("bif"); tb1 = tmp("tb1"); tb2 = tmp("tb2")
                for c in range(4):
                    csl = slice(c * W_, (c + 1) * W_)
                    S.dma("sync", are[:], are_r_d[:, csl].partition_broadcast(128), writes=["are"], dsem=3)
                    S.dma("sync", aim[:], aim_r_d[:, csl].partition_broadcast(128), writes=["aim"], dsem=4)
                    S.dma("sync", dlt[:], ldt_r_d[:, csl].partition_broadcast(128), writes=["dlt"], dsem=5)
                    S.dma("sync", brf[:], BR_d[:, c, :], writes=["brf"], dsem=6)
                    S.dma("sync", bif[:], BI_d[:, c, :], writes=["bif"], dsem=7)
                    G(lambda e: e.tensor_tensor(dlt[:], ebase[:], dlt[:], ALU.pow), r=["dlt", "ebase"], w=["dlt"])
                    V(lambda e: e.tensor_scalar(are[:], are[:], -1e-4, None, ALU.min), r=["are"], w=["are"])
                    V(lambda e: e.tensor_tensor(lr[:], are[:], dlt[:], ALU.mult), r=["are", "dlt"], w=["lr"])
                    V(lambda e: e.tensor_tensor(th[:], aim[:], dlt[:], ALU.mult), r=["aim", "dlt"], w=["th"])
                    V(lambda e: e.tensor_copy(ang[:], th[:]), r=["th"], w=["ang"])
                    sincos(ang, "ang", sn, "sn", cs_, "cs_", ti, "ti")
                    A(lambda e: e.activation(mg[:], lr[:], AF.Exp), r=["lr"], w=["mg"])
                    V(lambda e: e.tensor_tensor(cs_[:], mg[:], cs_[:], ALU.mult), r=["mg", "cs_"], w=["cs_"])
                    V(lambda e: e.tensor_scalar(cs_[:], cs_[:], -1.0, None, ALU.add), r=["cs_"], w=["cs_"])
                    V(lambda e: e.tensor_tensor(sn[:], mg[:], sn[:], ALU.mult), r=["mg", "sn"], w=["sn"])
                    V(lambda e: e.tensor_tensor(den[:], are[:], are[:], ALU.mult), r=["are"], w=["den"])
                    V(lambda e: e.tensor_tensor(t2[:], aim[:], aim[:], ALU.mult), r=["aim"], w=["t2"])
                    V(lambda e: e.tensor_tensor(den[:], den[:], t2[:], ALU.add), r=["den", "t2"], w=["den"])
                    V(lambda e: e.reciprocal(den[:], den[:]), r=["den"], w=["den"])
                    V(lambda e: e.tensor_tensor(cre[:], cs_[:], are[:], ALU.mult), r=["cs_", "are"], w=["cre"])
                    V(lambda e: e.tensor_tensor(t2[:], sn[:], aim[:], ALU.mult), r=["sn", "aim"], w=["t2"])
                    V(lambda e: e.tensor_tensor(cre[:], cre[:], t2[:], ALU.add), r=["cre", "t2"], w=["cre"])
                    V(lambda e: e.tensor_tensor(cre[:], cre[:], den[:], ALU.mult), r=["cre", "den"], w=["cre"])
                    V(lambda e: e.tensor_tensor(cim[:], sn[:], are[:], ALU.mult), r=["sn", "are"], w=["cim"])
                    V(lambda e: e.tensor_tensor(t2[:], cs_[:], aim[:], ALU.mult), r=["cs_", "aim"], w=["t2"])
                    V(lambda e: e.tensor_tensor(cim[:], cim[:], t2[:], ALU.subtract), r=["cim", "t2"], w=["cim"])
                    V(lambda e: e.tensor_tensor(cim[:], cim[:], den[:], ALU.mult), r=["cim", "den"], w=["cim"])
                    if c == 0:
                        dump("cre", cre[:], "cre", [128, W_])
                        dump("cim", cim[:], "cim", [128, W_])
                    V(lambda e: e.tensor_tensor(tb1[:], cre[:], brf[:], ALU.mult), r=["cre", "brf"], w=["tb1"])
                    V(lambda e: e.tensor_tensor(tb2[:], cim[:], bif[:], ALU.mult), r=["cim", "bif"], w=["tb2"])
                    V(lambda e, c=c: e.tensor_tensor(bblk[:, c, 0:512], tb1[:], tb2[:], ALU.subtract), r=["tb1", "tb2"], w=["bblk"])
                    V(lambda e: e.tensor_tensor(tb1[:], cre[:], bif[:], ALU.mult), r=["cre", "bif"], w=["tb1"])
                    V(lambda e: e.tensor_tensor(tb2[:], cim[:], brf[:], ALU.mult), r=["cim", "brf"], w=["tb2"])
                    V(lambda e, c=c: e.tensor_tensor(bblk[:, c, 512:1024], tb1[:], tb2[:], ALU.add), r=["tb1", "tb2"], w=["bblk"])
                    V(lambda e: e.tensor_scalar(ang[:], th[:], jcol[:, 0:1], None, ALU.mult), r=["th", "jcol"], w=["ang"])
                    sincos(ang, "ang", sn, "sn", cs_, "cs_", ti, "ti")
                    A(lambda e: e.activation(mg[:], lr[:], AF.Exp, scale=njcol[:, 0:1]), r=["lr", "njcol"], w=["mg"])
                    V(lambda e, csl=csl: e.tensor_tensor(Mre[:, csl], mg[:], cs_[:], ALU.mult), r=["mg", "cs_"], w=["Mre"])
                    V(lambda e, csl=csl: e.scalar_tensor_tensor(Mim[:, csl], mg[:], -1.0, sn[:], ALU.mult, ALU.mult), r=["mg", "sn"], w=["Mim"])
                arc = sb(ss_, "arc", [128, 16], F32)
                aic = sb(ss_, "aic", [128, 16], F32)
                dtc = sb(ss_, "dtc", [128, 16], F32)
                lrc = sb(ss_, "lrc", [128, 16], F32)
                thc = sb(ss_, "thc", [128, 16], F32)
                S.dma("sync", arc[:], are_c_d, writes=["arc"], dsem=3)
                S.dma("sync", aic[:], aim_c_d, writes=["aic"], dsem=4)
                S.dma("sync", dtc[:], ldt_c_d, writes=["dtc"], dsem=5)
                G(lambda e: e.tensor_tensor(dtc[:], ebase[:, 0:16], dtc[:], ALU.pow), r=["dtc", "ebase"], w=["dtc"])
                V(lambda e: e.tensor_scalar(arc[:], arc[:], -1e-4, None, ALU.min), r=["arc"], w=["arc"])
                V(lambda e: e.tensor_tensor(lrc[:], arc[:], dtc[:], ALU.mult), r=["arc", "dtc"], w=["lrc"])
                V(lambda e: e.tensor_tensor(thc[:], aic[:], dtc[:], ALU.mult), r=["aic", "dtc"], w=["thc"])
                v3 = lambda t: t[:].rearrange("p (q i) -> p q i", q=4)
                iot3 = iota_f[:].unsqueeze(1).to_broadcast([128, 4, 128])
                for c in range(4):
                    qs = slice(4 * c, 4 * c + 4)
                    V(lambda e, qs=qs: e.tensor_tensor(v3(ang), thc[:, qs].unsqueeze(2).to_broadcast([128, 4, 128]), iot3, ALU.mult),
                      r=["thc", "iota_f"], w=["ang"])
                    V(lambda e, qs=qs: e.tensor_tensor(v3(t2), lrc[:, qs].unsqueeze(2).to_broadcast([128, 4, 128]), iot3, ALU.mult),
                      r=["lrc", "iota_f"], w=["t2"])
                    sincos(ang, "ang", sn, "sn", cs_, "cs_", ti, "ti")
                    A(lambda e: e.activation(mg[:], t2[:], AF.Exp), r=["t2"], w=["mg"])
                    V(lambda e, qs=qs: e.tensor_tensor(Pre[:, qs, :], v3(mg), v3(cs_), ALU.mult), r=["mg", "cs_"], w=["Pre"])
                    V(lambda e, qs=qs: e.tensor_tensor(Pim[:, qs, :], v3(mg), v3(sn), ALU.mult), r=["mg", "sn"], w=["Pim"])
                a16 = lambda t: t[:, 0:16]
                V(lambda e: e.tensor_scalar(a16(ang), thc[:], 128.0, None, ALU.mult), r=["thc"], w=["ang"])
                sincos(ang, "ang", sn, "sn", cs_, "cs_", ti, "ti", ap=a16)
                A(lambda e: e.activation(a16(mg), lrc[:], AF.Exp, scale=128.0), r=["lrc"], w=["mg"])
                V(lambda e: e.tensor_tensor(A128re[:], a16(mg), a16(cs_), ALU.mult), r=["mg", "cs_"], w=["A128re"])
                V(lambda e: e.tensor_tensor(A128im[:], a16(mg), a16(sn), ALU.mult), r=["mg", "sn"], w=["A128im"])
                dump("Mre", Mre[:], "Mre", [128, 2048])
                dump("Mim", Mim[:], "Mim", [128, 2048])
                dump("Pre", Pre[:], "Pre", [128, 16, 128])
                dump("Pim", Pim[:], "Pim", [128, 16, 128])
                dump("A128re", A128re[:], "A128re", [128, 16])
                S.flush()

            XT = [sb(pa, "xt%d" % i, [128, D], F32) for i in range(2)]
            junk = sb(pa, "junk", [128, D], BF16)
            ssq = sb(pa, "ssq", [128, 1], F32)
            ssq2 = sb(pa, "ssq2", [128, 1], F32)
            rstd = sb(pa, "rstd", [128, 1], F32)
            xn = sb(pa, "xn", [128, D], BF16)
            hT = sb(pa, "hT", [128, 8, 128], BF16)
            ug = sb(pa, "ug", [128, 512], F32)
            vg = sb(pa, "vg", [128, 512], F32)
            ZST = [sb(pa, "zsT%d" % i, [128, 4, 128], BF16) for i in range(2)]
            FMAX = int(nc.vector.BN_STATS_FMAX)
            nst = max(1, 512 // FMAX)
            bst = sb(pa, "bst", [128, nst, int(nc.vector.BN_STATS_DIM)], F32)
            bmv = sb(pa, "bmv", [128, int(nc.vector.BN_AGGR_DIM)], F32)
            var2 = sb(pa, "var2", [128, 1], F32)
            rstdv = sb(pa, "rstdv", [128, 1], F32)
            vn = sb(pa, "vn", [128, 512], F32)
            vn2 = sb(pa, "vn2", [128, 512], F32)
            vb16 = sb(pa, "vb16", [128, 512], BF16)
            ygm = sb(pa, "ygm", [128, 512], BF16)
            YCT = [sb(pa, "ycatT%d" % i, [128, 8, 128], BF16) for i in range(2)]
            bq = sb(pa, "bq", [128, 4, 4, 512], BF16)
            ure = sb(pa, "ure", [128, 4, 128], F32)
            uim = sb(pa, "uim", [128, 4, 128], F32)
            ctmp = [sb(pa, "ctmp%d" % i, [128, 4], F32) for i in range(4)]
            xq = sb(pa, "xq", [128, 16, 4, 128], BF16)
            yg = sb(pa, "yg", [128, 512], BF16)
            ygT = sb(pa, "ygT", [128, 4, 128], BF16)
            sig = sb(pa, "sig", [128, 4, 128], BF16)
            x1t = sb(pa, "x1t", [128, D], F32)

            def front(n):
                sl = n % 2
                yield
                xt = XT[sl]
                yield
                xk = "xt%d" % sl
                zsT = ZST[sl]
                zk = "zsT%d" % sl
                ycatT = YCT[sl]
                yk = "ycatT%d" % sl
                yield
                S.dma("sync", xt[:], x_d[n * 128:(n + 1) * 128, :], writes=[xk], dsem=8 + sl)
                yield
                yield
                V(lambda e, xt=xt: e.scalar_tensor_tensor(junk[:], xt[:], 1.0, xt[:], ALU.mult, ALU.mult, accum_out=ssq[:]),
                  r=[xk], w=["junk", "ssq"])
                yield
                V(lambda e: e.tensor_scalar(ssq2[:], ssq[:], 1.0 / D, EPS, ALU.mult, ALU.add), r=["ssq"], w=["ssq2"])
                yield
                A(lambda e: e.activation(rstd[:], ssq2[:], AF.Sqrt), r=["ssq2"], w=["rstd"])
                V(lambda e: e.reciprocal(rstd[:], rstd[:]), r=["rstd"], w=["rstd"])
                yield
                V(lambda e, xt=xt: e.tensor_scalar(xn[:], xt[:], rstd[:, 0:1], None, ALU.mult), r=[xk, "rstd"], w=["xn"])
                yield
                for k in range(8):
                    PE(lambda e, k=k: e.transpose(PT[:, k * 128:(k + 1) * 128], xn[:, k * 128:(k + 1) * 128], identb[:]),
                       r=["xn", "identb"], w=["PT"], inc=(k == 7))
                yield
                for k in range(8):
                    A(lambda e, k=k: e.activation(hT[:, k, :], PT[:, k * 128:(k + 1) * 128], AF.Identity,
                                                  bias=sh1c[:, k:k + 1], scale=a1c[:, k:k + 1]),
                      r=["PT", "sh1c", "a1c"], w=["hT"])
                yield
                yield
                for half in range(2):
                    for k in range(8):
                        PE(lambda e, k=k, half=half: e.matmul(P[half][:], hT[:, k, :], win[:, k, half * 512:(half + 1) * 512],
                                                              start=(k == 0), stop=(k == 7)),
                           r=["hT", "win"], w=[PK[half]], inc=(k == 7))
                yield
                for f in range(4):
                    for k in range(8):
                        PE(lambda e, k=k, f=f: e.matmul(P[2][:, f * 128:(f + 1) * 128], win[:, k, 1024 + f * 128:1024 + (f + 1) * 128],
                                                        hT[:, k, :], start=(k == 0), stop=(k == 7)),
                           r=["hT", "win"], w=["P2"], inc=(k == 7 and f == 3))
                yield
                A(lambda e: e.activation(ug[:], P[0][:], AF.Gelu), r=["P0"], w=["ug"])
                yield
                A(lambda e: e.activation(vg[:], P[1][:], AF.Gelu), r=["P1"], w=["vg"])
                yield
                A(lambda e: e.copy(zsT[:].rearrange("p c t -> p (c t)"), P[2][:]), r=["P2"], w=[zk])
                yield
                yield
                for i in range(nst):
                    w_ = 512 // nst
                    V(lambda e, i=i, w_=w_: e.bn_stats(bst[:, i, :], vg[:, i * w_:(i + 1) * w_]), r=["vg"], w=["bst"])
                yield
                V(lambda e: e.bn_aggr(bmv[:], bst[:]), r=["bst"], w=["bmv"])
                yield
                V(lambda e: e.tensor_scalar(var2[:], bmv[:, 1:2], EPS, None, ALU.add), r=["bmv"], w=["var2"])
                yield
                A(lambda e: e.activation(rstdv[:], var2[:], AF.Sqrt), r=["var2"], w=["rstdv"])
                V(lambda e: e.reciprocal(rstdv[:], rstdv[:]), r=["rstdv"], w=["rstdv"])
                yield
                V(lambda e: e.tensor_scalar(vn[:], vg[:], bmv[:, 0:1], rstdv[:, 0:1], ALU.subtract, ALU.mult),
                  r=["vg", "bmv", "rstdv"], w=["vn"])
                yield
                V(lambda e: e.tensor_tensor(vn2[:], vn[:], lngr[:], ALU.mult), r=["vn", "lngr"], w=["vn2"])
                yield
                V(lambda e: e.tensor_tensor(vb16[:], vn2[:], lnbr[:], ALU.add), r=["vn2", "lnbr"], w=["vb16"])
                yield
                yield
                for h in range(4):
                    PE(lambda e, h=h: e.matmul(P[2][:, h * 128:(h + 1) * 128], wmT[:, h, :], vb16[:, h * 128:(h + 1) * 128],
                                               start=True, stop=True),
                       r=["wmT", "vb16"], w=["P2"], inc=(h == 3))
                yield
                for h in range(4):
                    V(lambda e, h=h: e.scalar_tensor_tensor(ygm[:, h * 128:(h + 1) * 128], P[2][:, h * 128:(h + 1) * 128],
                                                            bsc[:, h:h + 1], ug[:, h * 128:(h + 1) * 128], ALU.add, ALU.mult),
                      r=["P2", "bsc", "ug"], w=["ygm"])
                yield
                for h in range(4):
                    PE(lambda e, h=h: e.transpose(PT[:, h * 128:(h + 1) * 128], ygm[:, h * 128:(h + 1) * 128], identb[:]),
                       r=["ygm", "identb"], w=["PT"], inc=(h == 3))
                yield
                A(lambda e: e.copy(ycatT[:, 0:4, :].rearrange("p c t -> p (c t)"), PT[:, 0:512]), r=["PT"], w=[yk])
                yield

            def back(n):
                sl = n % 2
                xt = XT[sl]
                xk = "xt%d" % sl
                zsT = ZST[sl]
                zk = "zsT%d" % sl
                ycatT = YCT[sl]
                yk = "ycatT%d" % sl
                yield
                for c in range(4):
                    pr_, pi_ = (4, 5)
                    PE(lambda e, c=c, pr_=pr_: e.matmul(P[pr_][:], zsT[:, c, :], bblk[:, c, 0:512], start=True, stop=True),
                       r=[zk, "bblk"], w=[PK[pr_]], inc=False)
                    PE(lambda e, c=c, pi_=pi_: e.matmul(P[pi_][:], zsT[:, c, :], bblk[:, c, 512:1024], start=True, stop=True),
                       r=[zk, "bblk"], w=[PK[pi_]], inc=True)
                    csl = slice(c * 512, (c + 1) * 512)
                    bk_ = "bum%d" % c
                    V(lambda e, c=c, pr_=pr_, csl=csl: e.tensor_tensor(bq[:, c, 0, :], P[pr_][:], Mre[:, csl], ALU.mult),
                      r=[PK[pr_], "Mre"], w=[bk_])
                    V(lambda e, c=c, pi_=pi_, csl=csl: e.tensor_tensor(bq[:, c, 1, :], P[pi_][:], Mim[:, csl], ALU.mult),
                      r=[PK[pi_], "Mim"], w=[bk_])
                    V(lambda e, c=c, pi_=pi_, csl=csl: e.tensor_tensor(bq[:, c, 2, :], P[pi_][:], Mre[:, csl], ALU.mult),
                      r=[PK[pi_], "Mre"], w=[bk_])
                    V(lambda e, c=c, pr_=pr_, csl=csl: e.tensor_tensor(bq[:, c, 3, :], P[pr_][:], Mim[:, csl], ALU.mult),
                      r=[PK[pr_], "Mim"], w=[bk_])
                for c in range(4):
                    for (ja, jb, tb_, pb) in ((0, 1, ntrib, 6), (2, 3, trib, 3)):
                        for ql in range(4):
                            PE(lambda e, c=c, ja=ja, pb=pb, ql=ql: e.matmul(
                                P[pb][:, ql * 128:(ql + 1) * 128], bq[:, c, ja, ql * 128:(ql + 1) * 128], trib[:],
                                start=True, stop=False),
                               r=["bum%d" % c, "trib"], w=[PK[pb]], inc=False)
                            PE(lambda e, c=c, jb=jb, tb_=tb_, pb=pb, ql=ql: e.matmul(
                                P[pb][:, ql * 128:(ql + 1) * 128], bq[:, c, jb, ql * 128:(ql + 1) * 128], tb_[:],
                                start=False, stop=True),
                               r=["bum%d" % c, "trib", "ntrib"], w=[PK[pb]], inc=(ql == 3))
                    qs = slice(4 * c, 4 * c + 4)
                    V(lambda e, qs=qs: e.tensor_tensor(ure[:], P[6][:].rearrange("p (q i) -> p q i", q=4),
                                                       Kre[:, qs].unsqueeze(2).to_broadcast([128, 4, 128]), ALU.add),
                      r=["P6", "Kre"], w=["ure"])
                    V(lambda e, qs=qs: e.tensor_tensor(uim[:], P[3][:].rearrange("p (q i) -> p q i", q=4),
                                                       Kim[:, qs].unsqueeze(2).to_broadcast([128, 4, 128]), ALU.add),
                      r=["P3", "Kim"], w=["uim"])
                    ur7 = ure[:, :, 127:128].rearrange("p q o -> p (q o)")
                    ui7 = uim[:, :, 127:128].rearrange("p q o -> p (q o)")
                    G(lambda e, qs=qs, ur7=ur7: e.tensor_tensor(ctmp[0][:], A128re[:, qs], ur7, ALU.mult), r=["A128re", "ure"], w=["ct0"])
                    G(lambda e, qs=qs, ui7=ui7: e.tensor_tensor(ctmp[1][:], A128im[:, qs], ui7, ALU.mult), r=["A128im", "uim"], w=["ct1"])
                    G(lambda e, qs=qs, ui7=ui7: e.tensor_tensor(ctmp[2][:], A128re[:, qs], ui7, ALU.mult), r=["A128re", "uim"], w=["ct2"])
                    G(lambda e, qs=qs, ur7=ur7: e.tensor_tensor(ctmp[3][:], A128im[:, qs], ur7, ALU.mult), r=["A128im", "ure"], w=["ct3"])
                    G(lambda e, qs=qs: e.tensor_tensor(Kre[:, qs], ctmp[0][:], ctmp[1][:], ALU.subtract), r=["ct0", "ct1"], w=["Kre"])
                    G(lambda e, qs=qs: e.tensor_tensor(Kim[:, qs], ctmp[2][:], ctmp[3][:], ALU.add), r=["ct2", "ct3"], w=["Kim"])
                    xk_ = "xs%d" % c
                    u3r = ure[:]
                    u3i = uim[:]
                    V(lambda e, qs=qs, u3r=u3r: e.tensor_tensor(xq[:, qs, 0, :], u3r, Pre[:, qs, :], ALU.mult), r=["ure", "Pre"], w=[xk_])
                    V(lambda e, qs=qs, u3i=u3i: e.tensor_tensor(xq[:, qs, 1, :], u3i, Pim[:, qs, :], ALU.mult), r=["uim", "Pim"], w=[xk_])
                    G(lambda e, qs=qs, u3i=u3i: e.tensor_tensor(xq[:, qs, 2, :], u3i, Pre[:, qs, :], ALU.mult), r=["uim", "Pre"], w=[xk_])
                    G(lambda e, qs=qs, u3r=u3r: e.tensor_tensor(xq[:, qs, 3, :], u3r, Pim[:, qs, :], ALU.mult), r=["ure", "Pim"], w=[xk_])
                    yield
                yield
                for c in range(4):
                    PE(lambda e, c=c: e.matmul(P[4][:, c * 128:(c + 1) * 128], zsT[:, c, :], ddb[:, c, :], start=True, stop=False,
                                               skip_group_check=True),
                       r=[zk, "ddb"], w=["P4"], inc=False)
                    for ql in range(4):
                        q = 4 * c + ql
                        for (j_, ct_, ck_) in ((0, crb, "crb"), (1, ncrb, "ncrb"), (2, cinb, "cinb"), (3, cinb, "cinb")):
                            PE(lambda e, q=q, j_=j_, ct_=ct_: e.matmul(P[4][:, q * 32:(q + 1) * 32], xq[:, q, j_, :], ct_[:, q, :],
                                                                       start=False, stop=(j_ == 3), skip_group_check=True),
                               r=["xs%d" % c, ck_], w=["P4"], inc=(ql == 3 and j_ == 3))
                yield
                A(lambda e: e.activation(yg[:], P[4][:], AF.Gelu), r=["P4"], w=["yg"])
                yield
                for c in range(4):
                    PE(lambda e, c=c: e.transpose(PT[:, 512 + c * 128:512 + (c + 1) * 128], yg[:, c * 128:(c + 1) * 128], identb[:]),
                       r=["yg", "identb"], w=["PT"], inc=(c == 3))
                yield
                V(lambda e: e.tensor_copy(ygT[:].rearrange("p c t -> p (c t)"), PT[:, 512:1024]), r=["PT"], w=["ygT"])
                yield
                for fo in range(4):
                    for c in range(4):
                        PE(lambda e, fo=fo, c=c: e.matmul(P[6][:, fo * 128:(fo + 1) * 128], wglu[:, c, fo * 128:(fo + 1) * 128], ygT[:, c, :],
                                                          start=(c == 0), stop=(c == 3)),
                           r=["wglu", "ygT"], w=["P6"], inc=(c == 3 and fo == 3))
                yield
                for fo in range(4):
                    A(lambda e, fo=fo: e.activation(sig[:, fo, :], P[6][:, fo * 128:(fo + 1) * 128], AF.Sigmoid, bias=bgluc[:, fo:fo + 1]),
                      r=["P6", "bgluc"], w=["sig"])
                yield
                V(lambda e: e.tensor_tensor(ycatT[:, 4:8, :], ygT[:], sig[:], ALU.mult), r=["ygT", "sig"], w=[yk])
                yield
                yield
                for half in range(2):
                    pb = 4 + half
                    for k in range(8):
                        PE(lambda e, k=k, half=half, pb=pb: e.matmul(P[pb][:], ycatT[:, k, :], wout[:, k, half * 512:(half + 1) * 512],
                                                                     start=(k == 0), stop=(k == 7)),
                           r=[yk, "wout"], w=[PK[pb]], inc=(k == 7))
                    V(lambda e, half=half, pb=pb, xt=xt: e.tensor_tensor(x1t[:, half * 512:(half + 1) * 512], P[pb][:],
                                                                         xt[:, half * 512:(half + 1) * 512], ALU.add),
                      r=[PK[pb], xk], w=["x1t"])
                yield
                S.dma("sync", x1s_d[n * 128:(n + 1) * 128, :], x1t[:], reads=["x1t"], writes=["x1s"], dsem=10)
                yield
                if debug and n == 0:
                    dump("hT", hT[:], "hT", [128, 8, 128], BF16)
                    dump("ug", ug[:], "ug", [128, 512])
                    dump("vb16", vb16[:], "vb16", [128, 512], BF16)
                    dump("ygm", ygm[:], "ygm", [128, 512], BF16)
                    dump("zsT", zsT[:], zk, [128, 4, 128], BF16)
                    dump("yg", yg[:], "yg", [128, 512], BF16)
                    dump("ycatT", ycatT[:], yk, [128, 8, 128], BF16)
                    dump("x1t", x1t[:], "x1t", [128, D])
                yield

            def drain(g):
                for _ in g:
                    pass

            drain(front(0))
            for n in range(NCH):
                fg = front(n + 1) if n + 1 < NCH else iter(())
                for _ in back(n):
                    for _r in range(4):
                        next(fg, None)
                drain(fg)
            S.wait_all("sync", ["x1s"])
            S.flush()

        with ExitStack() as pb_:
            wq = sb(pb_, "wq", [128, 8, D], BF16)
            keysb = sb(pb_, "keysb", [128, 8, 256], BF16)
            S.dma("gpsimd", wq[:], wq_d.rearrange("(k p) n -> p k n", p=128), writes=["wq"], dsem=21)
            S.dma("gpsimd", keysb[:], keys_d, writes=["keysb"], dsem=21)
            x1b = [sb(pb_, "x1b%d" % i, [128, NSUB, D], F32) for i in range(2)]
            h2T = [sb(pb_, "h2T%d" % i, [128, 8, TB], BF16) for i in range(2)]
            qT = sb(pb_, "qT", [128, 8, TB], BF16)
            gT = sb(pb_, "gT", [128, TB], BF16)
            ihT = sb(pb_, "ihT", [128, TB], BF16)
            jlT = sb(pb_, "jlT", [128, TB], BF16)
            Wsel = sb(pb_, "Wsel", [128, TB, 128], BF16)
            ssb = sb(pb_, "ssb", [128, 1], F32)
            ssb2 = sb(pb_, "ssb2", [128, 1], F32)
            rstb = sb(pb_, "rstb", [128, 1], F32)
            ssr = sb(pb_, "ssr", [128, 1], F32)
            ssr2 = sb(pb_, "ssr2", [128, 1], F32)
            rstr = sb(pb_, "rstr", [128, 1], F32)
            pt1 = sb(pb_, "pt1", [128, D], F32)
            junk2 = sb(pb_, "junk2", [128, D], BF16)
            TS = 8
            xn2 = sb(pb_, "xn2", [128, D], BF16)
            scs = sb(pb_, "scs", [128, 16, 128], F32)
            work = [sb(pb_, "work%d" % i, [128, 256], F32) for i in range(2)]
            v1 = sb(pb_, "v1", [128, 16, 16], F32)
            i1 = sb(pb_, "i1", [128, 16, 16], U32)
            i1f = sb(pb_, "i1f", [128, 16, 16], F32)
            cand = sb(pb_, "cand", [128, 8, 256], F32)
            b1 = sb(pb_, "b1", [128, 8, 16], F32)
            pos = sb(pb_, "pos", [128, 8, 16], U32)
            posa = sb(pb_, "posa", [128, 8, 16], U32)
            posb = sb(pb_, "posb", [128, 8, 16], U32)
            posaf = sb(pb_, "posaf", [128, 8, 16], F32)
            posbf = sb(pb_, "posbf", [128, 8, 16], F32)
            oh = sb(pb_, "oh", [128, 8, 16, 16], BF16)
            sm = sb(pb_, "sm", [128, 8, 16], F32)
            smz = sb(pb_, "smz", [128, 8], F32)
            gw = sb(pb_, "gw", [128, 128], F32)
            ihi = sb(pb_, "ihi", [128, 128], F32)
            jlo = sb(pb_, "jlo", [128, 128], F32)
            OA = [sb(pb_, "OA%d" % i, [128, TS, 128], BF16) for i in range(2)]
            OBw = [sb(pb_, "OBw%d" % i, [128, TS, 128], BF16) for i in range(2)]
            ub = [sb(pb_, "ub%d" % i, [128, 8, GI * 128], BF16) for i in range(2)]
            vbuf = [sb(pb_, "vbuf%d" % i, [128, GI, D], BF16) for i in range(2)]
            actg = [sb(pb_, "actg%d" % i, [128, TB], BF16) for i in range(2)]
            wf = [sb(pb_, "wf%d" % i, [128, TB], BF16) for i in range(2)]
            nact = [0]

            def routing_stages(blk):
                hb = blk % 2
                t0 = blk * TB
                xb = x1b[hb]
                xbk = "x1b%d" % hb
                hT_ = h2T[hb]
                hk = "h2T%d" % hb
                st = []

                def add(f):
                    st.append(f)

                add(lambda: S.dma("sync", xb[:], x1s_d[t0:t0 + TB, :].rearrange("(s p) d -> p s d", p=128),
                                  reads=["x1s"], writes=[xbk], dsem=18 + hb))
                for s in range(NSUB):
                    tsl = slice(s * 128, (s + 1) * 128)

                    def f_rms(s=s):
                        V(lambda e: e.scalar_tensor_tensor(junk2[:], xb[:, s, :], 1.0, xb[:, s, :], ALU.mult, ALU.mult, accum_out=ssr[:]),
                          r=[xbk], w=["junk2", "ssr"])
                        V(lambda e: e.tensor_scalar(ssr2[:], ssr[:], 1.0 / D, EPS, ALU.mult, ALU.add), r=["ssr"], w=["ssr2"])
                        G(lambda e: e.tensor_tensor(rstr[:], ssr2[:], mhalf[:], ALU.pow), r=["ssr2", "mhalf"], w=["rstr"])
                    add(f_rms)
                    add(lambda s=s: V(lambda e: e.tensor_scalar(xn2[:], xb[:, s, :], rstr[:, 0:1], None, ALU.mult), r=[xbk, "rstr"], w=["xn2"]))

                    def f_tr():
                        for k in range(8):
                            PE(lambda e, k=k: e.transpose(PT[:, k * 128:(k + 1) * 128], xn2[:, k * 128:(k + 1) * 128], identb[:]),
                               r=["xn2", "identb"], w=["PT"], inc=(k == 7))
                    add(f_tr)
                    for k0 in (0, 4):
                        def f_ev(k0=k0, tsl=tsl):
                            for k in range(k0, k0 + 4):
                                A(lambda e, k=k: e.activation(hT_[:, k, tsl], PT[:, k * 128:(k + 1) * 128], AF.Identity,
                                                              bias=sh2c[:, k:k + 1], scale=a2c[:, k:k + 1]),
                                  r=["PT", "sh2c", "a2c"], w=[hk])
                        add(f_ev)
                for m in range(8):
                    hsl = slice((m % 2) * 256, (m % 2) * 256 + TB)
                    pk6 = "P6"

                    def f_q(m=m, hsl=hsl, pk6=pk6):
                        for k in range(8):
                            PE(lambda e, k=k: e.matmul(P[6][:, hsl], wq[:, k, m * 128:(m + 1) * 128], hT_[:, k, :],
                                                       start=(k == 0), stop=(k == 7)),
                               r=["wq", hk], w=[pk6], inc=(k == 7))
                    add(f_q)
                    add(lambda m=m, hsl=hsl, pk6=pk6: A(lambda e: e.copy(qT[:, m, :], P[6][:, hsl]), r=[pk6], w=["qT"]))
                for s in range(NSUB):
                    tsl = slice(s * 128, (s + 1) * 128)
                    for h in range(8):
                        hsl = slice((h % 2) * 256, (h % 2 + 1) * 256)
                        pk6 = "P6"

                        def f_sc(h=h, hsl=hsl, pk6=pk6, tsl=tsl):
                            PE(lambda e: e.matmul(P[6][:, hsl], qT[:, h, tsl], keysb[:, h, :], start=True, stop=True),
                               r=["qT", "keysb"], w=[pk6])
                            A(lambda e: e.copy(scs[:, 2 * h:2 * h + 2, :].rearrange("p a k -> p (a k)"), P[6][:, hsl]),
                              r=[pk6], w=["scs%d" % (2 * h), "scs%d" % (2 * h + 1)])
                        add(f_sc)
                    for hc in range(16):
                        wk = work[hc % 2]
                        wkk = "work%d" % (hc % 2)
                        sk = "scs%d" % hc
                        vk = "v1_%d" % hc
                        ik = "i1_%d" % hc

                        def f_a(hc=hc, wk=wk, wkk=wkk, sk=sk, vk=vk):
                            V(lambda e: e.max(v1[:, hc, 0:8], scs[:, hc, :]), r=[sk], w=[vk])
                            V(lambda e: e.match_replace(wk[:, 0:128], v1[:, hc, 0:8], scs[:, hc, :], -1e30), r=[sk, vk], w=[wkk])
                            V(lambda e: e.max(v1[:, hc, 8:16], wk[:, 0:128]), r=[wkk], w=[vk])
                        add(f_a)

                        def f_b(hc=hc, sk=sk, vk=vk, ik=ik):
                            V(lambda e: e.max_index(i1[:, hc, 0:8], v1[:, hc, 0:8], scs[:, hc, :]), r=[sk, vk], w=[ik])
                            V(lambda e: e.max_index(i1[:, hc, 8:16], v1[:, hc, 8:16], scs[:, hc, :]), r=[sk, vk], w=[ik])
                        add(f_b)
                    i1keys = ["i1_%d" % hc for hc in range(16)]
                    add(lambda: V(lambda e: e.tensor_copy(i1f[:], i1[:]), r=i1keys, w=["i1f"]))
                    v1v = v1[:].rearrange("p (h c) k -> p h c k", c=2)
                    i1v = i1f[:].rearrange("p (h c) k -> p h c k", c=2)
                    for h in range(8):
                        wk = work[h % 2]
                        wkk = "work%d" % (h % 2)
                        ck = "cand%d" % h
                        bk = "b1_%d" % h
                        pk_ = "pos%d" % h

                        def f_c(h=h, wk=wk, wkk=wkk, ck=ck, bk=bk):
                            V(lambda e: e.tensor_tensor(cand[:, h, :].rearrange("p (a b) -> p a b", a=16),
                                                        v1v[:, h, 0, :].unsqueeze(2).to_broadcast([128, 16, 16]),
                                                        v1v[:, h, 1, :].unsqueeze(1).to_broadcast([128, 16, 16]), ALU.add),
                              r=["v1_%d" % (2 * h), "v1_%d" % (2 * h + 1)], w=[ck])
                            V(lambda e: e.max(b1[:, h, 0:8], cand[:, h, :]), r=[ck], w=[bk])
                            V(lambda e: e.match_replace(wk[:], b1[:, h, 0:8], cand[:, h, :], -1e30), r=[ck, bk], w=[wkk])
                        add(f_c)

                        def f_d(h=h, wk=wk, wkk=wkk, ck=ck, bk=bk, pk_=pk_):
                            V(lambda e: e.max(b1[:, h, 8:16], wk[:]), r=[wkk], w=[bk])
                            V(lambda e: e.max_index(pos[:, h, 0:8], b1[:, h, 0:8], cand[:, h, :]), r=[ck, bk], w=[pk_])
                            V(lambda e: e.max_index(pos[:, h, 8:16], b1[:, h, 8:16], cand[:, h, :]), r=[ck, bk], w=[pk_])
                        add(f_d)
                    b1keys = ["b1_%d" % h for h in range(8)]
                    poskeys = ["pos%d" % h for h in range(8)]

                    def f_sm():
                        V(lambda e: e.tensor_tensor(sm[:], b1[:], b1[:, :, 0:1].to_broadcast([128, 8, 16]), ALU.subtract), r=b1keys, w=["sm"])
                        A(lambda e: e.activation(sm[:], sm[:], AF.Exp), r=["sm"], w=["sm"])
                    add(f_sm)

                    def f_sm2():
                        V(lambda e: e.tensor_reduce(smz[:], sm[:], AX.X, ALU.add), r=["sm"], w=["smz"])
                        V(lambda e: e.reciprocal(smz[:], smz[:]), r=["smz"], w=["smz"])
                        V(lambda e: e.tensor_tensor(gw[:].rearrange("p (h k) -> p h k", h=8), sm[:],
                                                    smz[:].unsqueeze(2).to_broadcast([128, 8, 16]), ALU.mult),
                          r=["sm", "smz"], w=["gw"])
                    add(f_sm2)

                    def f_pos():
                        V(lambda e: e.tensor_single_scalar(posa[:], pos[:], 4, ALU.logical_shift_right), r=poskeys, w=["posa"])
                        V(lambda e: e.tensor_single_scalar(posb[:], pos[:], 15, ALU.bitwise_and), r=poskeys, w=["posb"])
                        V(lambda e: e.tensor_copy(posaf[:], posa[:]), r=["posa"], w=["posaf"])
                        V(lambda e: e.tensor_copy(posbf[:], posb[:]), r=["posb"], w=["posbf"])
                    add(f_pos)
                    io16 = iota_f[:, 0:16]
                    for (pf, pk_, cc, dst, dk) in ((posaf, "posaf", 0, ihi, "ihi"), (posbf, "posbf", 1, jlo, "jlo")):
                        for h in range(8):
                            def f_oh(h=h, pf=pf, pk_=pk_, cc=cc):
                                V(lambda e: e.tensor_tensor(oh[:, h, :, :], pf[:, h, :].unsqueeze(2).to_broadcast([128, 16, 16]),
                                                            io16.unsqueeze(1).to_broadcast([128, 16, 16]), ALU.is_equal),
                                  r=[pk_, "iota_f"], w=["oh%d" % h])
                                G(lambda e: e.tensor_tensor(oh[:, h, :, :], oh[:, h, :, :],
                                                            i1v[:, h, cc, :].unsqueeze(1).to_broadcast([128, 16, 16]), ALU.mult),
                                  r=["oh%d" % h, "i1f"], w=["oh%d" % h])
                            add(f_oh)
                        ohkeys = ["oh%d" % h for h in range(8)]
                        add(lambda dst=dst, dk=dk, ohkeys=ohkeys: V(
                            lambda e: e.tensor_reduce(dst[:], oh[:].rearrange("p h k a -> p (h k) a"), AX.X, ALU.add), r=ohkeys, w=[dk]))
                    if debug and blk == 0 and s == 0:
                        def f_dbg():
                            dump("scs", scs[:], "scs0", [128, 16, 128])
                            dump("gw", gw[:], "gw", [128, 128])
                            dump("ihi", ihi[:], "ihi", [128, 128])
                            dump("jlo", jlo[:], "jlo", [128, 128])
                        add(f_dbg)
                    for (src, sk_, dstT, dk) in ((gw, "gw", gT, "gT"), (ihi, "ihi", ihT, "ihT"), (jlo, "jlo", jlT, "jlT")):
                        def f_t(src=src, sk_=sk_, dstT=dstT, dk=dk, tsl=tsl):
                            PE(lambda e: e.transpose(P[6][:, 0:128], src[:], identf[:]), r=[sk_, "identf"], w=["P6"])
                            A(lambda e: e.copy(dstT[:, tsl], P[6][:, 0:128]), r=["P6"], w=[dk])
                        add(f_t)
                return st

            def wsel_build(blk):
                for sbk in range(TB // TS):
                    o = sbk % 2
                    ts0 = sbk * TS
                    io3 = iota_b[:].unsqueeze(1).to_broadcast([128, TS, 128])
                    V(lambda e, o=o, ts0=ts0, io3=io3: e.tensor_tensor(OA[o][:], io3, jlT[:, ts0:ts0 + TS].unsqueeze(2).to_broadcast([128, TS, 128]),
                                                                       ALU.is_equal),
                      r=["iota_b", "jlT"], w=["OA%d" % o])
                    V(lambda e, o=o, ts0=ts0, io3=io3: e.tensor_tensor(OBw[o][:], io3, ihT[:, ts0:ts0 + TS].unsqueeze(2).to_broadcast([128, TS, 128]),
                                                                       ALU.is_equal),
                      r=["iota_b", "ihT"], w=["OBw%d" % o])
                    V(lambda e, o=o, ts0=ts0: e.tensor_tensor(OBw[o][:], OBw[o][:], gT[:, ts0:ts0 + TS].unsqueeze(2).to_broadcast([128, TS, 128]),
                                                              ALU.mult),
                      r=["OBw%d" % o, "gT"], w=["OBw%d" % o])
                    for tl in range(TS):
                        pbk = 4 + (tl // 4) % 2
                        PE(lambda e, o=o, tl=tl, pbk=pbk: e.matmul(P[pbk][:, (tl % 4) * 128:(tl % 4 + 1) * 128], OA[o][:, tl, :], OBw[o][:, tl, :],
                                                                   start=True, stop=True),
                           r=["OA%d" % o, "OBw%d" % o], w=[PK[pbk]], inc=(tl % 4 == 3))
                        if tl % 4 == 3:
                            tb0 = ts0 + tl - 3
                            A(lambda e, pbk=pbk, tb0=tb0: e.copy(Wsel[:, tb0:tb0 + 4, :].rearrange("p t i -> p (t i)"), P[pbk][:]),
                              r=[PK[pbk]], w=["Wsel"])
                if debug and blk == 0:
                    dump("Wsel", Wsel[:, 0:8, :], "Wsel", [128, 8, 128], BF16)
                    dump("h2T", h2T[0][:], "h2T0", [128, 8, TB], BF16)

            def expert_loop(blk, stages):
                hb = blk % 2
                hT_ = h2T[hb]
                hk = "h2T%d" % hb
                nst = len(stages)
                per = (nst + 111) // 112 if nst else 0
                sp = [0]

                def run_stages(n):
                    while n > 0 and sp[0] < nst:
                        stages[sp[0]]()
                        sp[0] += 1
                        n -= 1

                def Vm(i):
                    o = (i // GI) % 2
                    il = i % GI
                    a_ = i % 2
                    for s in range(NSUB):
                        for half in range(2):
                            pbk = s * 2 + half
                            PE(lambda e, a_=a_, s=s, half=half, pbk=pbk, o=o, il=il, i=i: e.matmul(
                                P[pbk][:], wf[a_][:, s * 128:(s + 1) * 128], vbuf[o][:, il, half * 512:(half + 1) * 512],
                                start=(i == 0), stop=(i == 127)),
                               r=["wf%d" % a_, "vbuf%d" % o], w=[PK[pbk]], inc=(s == NSUB - 1 and half == 1))

                for i in range(128):
                    ig = i // GI
                    il = i % GI
                    o = ig % 2
                    a_ = i % 2
                    pa_ = 4 + a_
                    if il == 0:
                        S.dma("sync", ub[o][:], uTg_d[ig], reads=["uTg"], writes=["ub%d" % o], dsem=12 + o)
                        S.dma("sync", vbuf[o][:], vg_d[ig], reads=["vg"], writes=["vbuf%d" % o], dsem=14 + o)
                    for k in range(8):
                        PE(lambda e, o=o, il=il, k=k, pa_=pa_: e.matmul(P[pa_][:, 0:TB], ub[o][:, k, il * 128:(il + 1) * 128], hT_[:, k, :],
                                                                        start=(k == 0), stop=(k == 7)),
                           r=["ub%d" % o, hk], w=[PK[pa_]], inc=(k == 7))
                    A(lambda e, a_=a_, pa_=pa_: e.activation(actg[a_][:], P[pa_][:, 0:TB], AF.Gelu), r=[PK[pa_]], w=["actg%d" % a_])
                    G(lambda e, a_=a_, i=i: e.tensor_tensor(wf[a_][:], actg[a_][:], Wsel[:, :, i], ALU.mult),
                      r=["actg%d" % a_, "Wsel"], w=["wf%d" % a_])
                    if i >= 1:
                        Vm(i - 1)
                    if i >= 4:
                        run_stages(per)
                Vm(127)
                run_stages(nst)

            def final_evac(blk):
                hb = blk % 2
                t0 = blk * TB
                xb = x1b[hb]
                xbk = "x1b%d" % hb
                for s in range(NSUB):
                    for half in range(2):
                        pbk = s * 2 + half
                        hs = slice(half * 512, (half + 1) * 512)
                        V(lambda e, pbk=pbk, hs=hs: e.tensor_tensor(pt1[:, hs], P[pbk][:], gt2r[:, hs], ALU.mult), r=[PK[pbk], "gtr1"], w=["pt1"])
                    if debug and blk == 0 and s == 0:
                        dump("peer", pt1[:], "pt1", [128, D])
                    G(lambda e, s=s: e.tensor_tensor(pt1[:], pt1[:], xb[:, s, :], ALU.add), r=["pt1", xbk], w=["pt1"])
                    V(lambda e: e.scalar_tensor_tensor(junk2[:], pt1[:], 1.0, pt1[:], ALU.mult, ALU.mult, accum_out=ssb[:]),
                      r=["pt1"], w=["junk2", "ssb"])
                    V(lambda e: e.tensor_scalar(ssb2[:], ssb[:], 1.0 / D, EPS, ALU.mult, ALU.add), r=["ssb"], w=["ssb2"])
                    G(lambda e: e.tensor_tensor(rstb[:], ssb2[:], mhalf[:], ALU.pow), r=["ssb2", "mhalf"], w=["rstb"])
                    V(lambda e: e.scalar_tensor_tensor(pt1[:], pt1[:], rstb[:, 0:1], gfr[:], ALU.mult, ALU.mult),
                      r=["pt1", "rstb", "gfr"], w=["pt1"])
                    S.dma("sync", out_d[t0 + s * 128:t0 + (s + 1) * 128, :], pt1[:], reads=["pt1"], writes=["out"], dsem=16)

            if INTERLEAVE:
                for f in routing_stages(0):
                    f()
                for blk in range(NBLK):
                    wsel_build(blk)
                    nxt = routing_stages(blk + 1) if blk + 1 < NBLK else []
                    expert_loop(blk, nxt)
                    final_evac(blk)
            else:
                for blk in range(NBLK):
                    for f in routing_stages(blk):
                        f()
                    wsel_build(blk)
                    expert_loop(blk, [])
                    final_evac(blk)
            S.wait_all("sync", ["out"] + list(dbg_outs.keys()))
            S.flush()
    return nc, S.n_ins


def prep_shared(inp):
    f = np.float32
    sh = {}
    sh["w_ada"] = np.ascontiguousarray(inp["w_ada"][0], f)
    sh["b_ada_c"] = np.ascontiguousarray(inp["b_ada"][0].reshape(48, 128).T, f)
    sh["b_ada_r"] = np.ascontiguousarray(inp["b_ada"][0].reshape(1, 6 * D), f)
    sh["g_mix_c"] = np.ascontiguousarray(inp["g_mix"][0].reshape(8, 128).T, f)
    sh["g_ffn_c"] = np.ascontiguousarray(inp["g_ffn"][0].reshape(8, 128).T, f)
    sh["w_in"] = np.ascontiguousarray(inp["w_in"][0], f)
    sh["w_out"] = np.ascontiguousarray(inp["w_out"][0], f)
    sh["w_q"] = np.ascontiguousarray(inp["w_q"][0], f)
    sh["w_glu"] = np.ascontiguousarray(inp["w_glu"][0], f)
    sh["ln_g_r"] = np.ascontiguousarray(inp["sgu_ln_g"][0].reshape(1, 512), f)
    sh["ln_b_r"] = np.ascontiguousarray(inp["sgu_ln_b"][0].reshape(1, 512), f)
    sh["b_glu_c"] = np.ascontiguousarray(inp["b_glu"][0].reshape(4, 128).T, f)
    sh["w_sT"] = np.ascontiguousarray(np.transpose(inp["w_s"][0], (2, 0, 1)), f)
    sh["b_s_c"] = np.ascontiguousarray(inp["b_s"][0].T, f)
    a_re = np.asarray(inp["ssm_a_re"][0], f)
    a_im = np.asarray(inp["ssm_a_im"][0], f)
    ldt = np.repeat(np.asarray(inp["ssm_log_dt"][0], f)[:, None], 64, axis=1)
    sh["a_re_r"] = np.ascontiguousarray(a_re.reshape(1, 2048))
    sh["a_im_r"] = np.ascontiguousarray(a_im.reshape(1, 2048))
    sh["ldt_r"] = np.ascontiguousarray(ldt.reshape(1, 2048))

    def cols(a):
        return np.ascontiguousarray(a.reshape(16, 2, 64).transpose(1, 2, 0).reshape(128, 16))

    sh["a_re_c"] = cols(a_re)
    sh["a_im_c"] = cols(a_im)
    sh["ldt_c"] = cols(ldt)
    b_re = np.asarray(inp["ssm_b_re"][0], f)
    b_im = np.asarray(inp["ssm_b_im"][0], f)
    BR = np.zeros((8, 16, 4, 8, 64), f)
    BI = np.zeros((8, 16, 4, 8, 64), f)
    for c in range(4):
        for gl in range(8):
            BR[gl, :, c, gl, :] = b_re[8 * c + gl].T
            BI[gl, :, c, gl, :] = b_im[8 * c + gl].T
    sh["BR"] = BR.reshape(128, 4, 512)
    sh["BI"] = BI.reshape(128, 4, 512)
    c_re = np.asarray(inp["ssm_c_re"][0], f)
    c_im = np.asarray(inp["ssm_c_im"][0], f)
    CR = np.zeros((2, 64, 16, 2, 16), f)
    CI = np.zeros((2, 64, 16, 2, 16), f)
    for q in range(16):
        for gg in range(2):
            CR[gg, :, q, gg, :] = c_re[2 * q + gg].T
            CI[gg, :, q, gg, :] = c_im[2 * q + gg].T
    sh["CR"] = CR.reshape(128, 16, 32)
    sh["CI"] = CI.reshape(128, 16, 32)
    dsk = np.asarray(inp["ssm_d"][0], f).reshape(4, 128)
    Dd = np.zeros((128, 4, 128), f)
    for c in range(4):
        Dd[np.arange(128), c, np.arange(128)] = dsk[c]
    sh["Dd"] = Dd
    keys = np.asarray(inp["peer_keys"][0], f)
    kb = np.zeros((2, 64, 8, 2, 128), f)
    for h in range(8):
        for c in range(2):
            kb[c, :, h, c, :] = keys[h, c].T
    sh["keysblk"] = kb.reshape(128, 8, 256)
    sh["uT"] = np.ascontiguousarray(np.asarray(inp["peer_u"][0], f).T)
    sh["v"] = np.ascontiguousarray(inp["peer_v"][0], f)
    sh["g_final_r"] = np.ascontiguousarray(np.asarray(inp["g_final"], f).reshape(1, D))
    return sh


def make_in_maps(inp, T):
    sh = prep_shared(inp)
    maps = []
    for b in range(NCORES):
        m = dict(sh)
        m["x"] = np.ascontiguousarray(inp["x"][b, :T], np.float32)
        m["cvec"] = np.ascontiguousarray(np.asarray(inp["c"][b], np.float32).reshape(8, 128).T)
        maps.append(m)
    return maps


_CACHE = {}


def kernel(**inputs):
    inputs = {k: np.asarray(v) for k, v in inputs.items()}
    T = inputs["x"].shape[1]
    if T not in _CACHE:
        _CACHE[T] = build_program(T)[0]
    nc = _CACHE[T]
    maps = make_in_maps(inputs, T)
    res = run_bass_kernel_spmd(nc, maps, core_ids=list(range(NCORES)))
    out = np.stack([np.asarray(res.results[b]["out"], np.float32) for b in range(NCORES)], axis=0)
    return out
```

```python
import math
from contextlib import ExitStack

import numpy as np
import concourse.bass as bass
import concourse.mybir as mybir
from concourse.bass_utils import run_bass_kernel_spmd

F32 = mybir.dt.float32
BF16 = mybir.dt.bfloat16
U32 = mybir.dt.uint32
I32 = mybir.dt.int32
AF = mybir.ActivationFunctionType
ALU = mybir.AluOpType
AX = mybir.AxisListType

D = 1024
NCORES = 8
SEQ = 8192
EPS = 1e-6
PI = math.pi
TWO_PI = 2.0 * math.pi
ENGS = ("sync", "scalar", "vector", "gpsimd", "tensor")


class _DSem:
    def __init__(self, sem, name):
        self.sem = sem
        self.name = name
        self.issued = 0


class Sched:
    def __init__(self, nc, es, n_dsem=40):
        self.nc = nc
        self.sem = {e: es.enter_context(nc.semaphore("s_" + e)) for e in ENGS}
        self.cnt = {e: 0 for e in ENGS}
        self.ops = {e: [] for e in ENGS}
        self.waited = {e: {} for e in ENGS}
        self.dsems = [_DSem(es.enter_context(nc.semaphore("d%d" % i)), "d%d" % i) for i in range(n_dsem)]
        self.last_w = {}
        self.readers = {}
        self.pending = {e: ([], []) for e in ENGS}
        self.semobj = {e: self.sem[e] for e in ENGS}
        for d in self.dsems:
            self.semobj[d.name] = d.sem
        self.n_ins = 0

    def _need(self, eng, reads, writes, skip_self=False):
        need = {}

        def add(src, val):
            if skip_self and src == eng:
                return
            if need.get(src, 0) < val:
                need[src] = val

        for k in reads:
            w = self.last_w.get(k)
            if w:
                add(*w)
        for k in writes:
            w = self.last_w.get(k)
            if w:
                add(*w)
            for r in self.readers.get(k, {}).items():
                add(*r)
        out = []
        for src, val in need.items():
            if self.waited[eng].get(src, 0) >= val:
                continue
            self.waited[eng][src] = val
            out.append((src, val))
        return out

    def _emit_waits(self, eng, waits):
        for src, val in waits:
            so = self.semobj[src]
            self.ops[eng].append(lambda e, so=so, val=val: e.wait_ge(so, val))

    def _commit(self, tag, reads, writes):
        for k in reads:
            rd = self.readers.setdefault(k, {})
            if rd.get(tag[0], 0) < tag[1]:
                rd[tag[0]] = tag[1]
        for k in writes:
            self.last_w[k] = tag
            self.readers[k] = {}

    def op(self, eng, fn, reads=(), writes=(), inc=True):
        self.n_ins += 1
        waits = self._need(eng, reads, writes, skip_self=(eng == "tensor"))
        self._emit_waits(eng, waits)
        pr, pw = self.pending[eng]
        pr.extend(reads)
        pw.extend(writes)
        if inc:
            self.cnt[eng] += 1
            so = self.sem[eng]
            self.ops[eng].append(lambda e, fn=fn, so=so: fn(e).then_inc(so, 1))
            self._commit((eng, self.cnt[eng]), pr, pw)
            self.pending[eng] = ([], [])
        else:
            self.ops[eng].append(lambda e, fn=fn: fn(e))

    def dma(self, eng, out, in_, reads=(), writes=(), dsem=0, **kw):
        self.n_ins += 1
        d = self.dsems[dsem]
        waits = self._need(eng, reads, writes)
        if d.issued and self.waited[eng].get(d.name, 0) < 16 * d.issued:
            self.waited[eng][d.name] = 16 * d.issued
            waits.append((d.name, 16 * d.issued))
        self._emit_waits(eng, waits)
        d.issued += 1
        so = d.sem
        self.ops[eng].append(
            lambda e, so=so, out=out, in_=in_, kw=kw: e.dma_start(out=out, in_=in_, **kw).then_inc(so, 16))
        self._commit((d.name, 16 * d.issued), reads, writes)

    def wait_all(self, eng, keys):
        self._emit_waits(eng, self._need(eng, keys, ()))

    def barrier(self):
        for e in ENGS:
            assert not self.pending[e][0] and not self.pending[e][1], e
        for e in ENGS:
            waits = []
            for src in ENGS:
                if src == e and e == "tensor":
                    continue
                val = self.cnt[src]
                if val and self.waited[e].get(src, 0) < val:
                    self.waited[e][src] = val
                    waits.append((src, val))
            for d in self.dsems:
                val = 16 * d.issued
                if val and self.waited[e].get(d.name, 0) < val:
                    self.waited[e][d.name] = val
                    waits.append((d.name, val))
            self._emit_waits(e, waits)

    def replay(self):
        with self.nc.Block() as block:
            for name in ENGS:
                ops = self.ops[name]

                def body(e, ops=ops):
                    for f in ops:
                        f(e)

                getattr(block, name)(body)
        self.ops = {e: [] for e in ENGS}

    def flush(self):
        self.barrier()
        self.replay()


INTERLEAVE = True


def build_program(T=SEQ, debug=False, TB=256, GI=4):
    assert T % TB == 0 and TB % 128 == 0
    NCH = T // 128
    NBLK = T // TB
    NSUB = TB // 128
    nc = bass.Bass("TRN2", target_bir_lowering=False)

    def din(name, shape, dt=F32):
        return nc.dram_tensor(name, list(shape), dt, kind="ExternalInput").ap()

    x_d = din("x", [T, D])
    cvec_d = din("cvec", [128, 8])
    wada_d = din("w_ada", [D, 6 * D])
    badac_d = din("b_ada_c", [128, 48])
    badar_d = din("b_ada_r", [1, 6 * D])
    gmix_d = din("g_mix_c", [128, 8])
    gffn_d = din("g_ffn_c", [128, 8])
    win_d = din("w_in", [D, 1536])
    wout_d = din("w_out", [D, D])
    wq_d = din("w_q", [D, D])
    wglu_d = din("w_glu", [512, 512])
    lng_d = din("ln_g_r", [1, 512])
    lnb_d = din("ln_b_r", [1, 512])
    bgluc_d = din("b_glu_c", [128, 4])
    wsT_d = din("w_sT", [128, 4, 128])
    bsc_d = din("b_s_c", [128, 4])
    are_r_d = din("a_re_r", [1, 2048])
    aim_r_d = din("a_im_r", [1, 2048])
    ldt_r_d = din("ldt_r", [1, 2048])
    are_c_d = din("a_re_c", [128, 16])
    aim_c_d = din("a_im_c", [128, 16])
    ldt_c_d = din("ldt_c", [128, 16])
    BR_d = din("BR", [128, 4, 512])
    BI_d = din("BI", [128, 4, 512])
    CR_d = din("CR", [128, 16, 32])
    CI_d = din("CI", [128, 16, 32])
    Dd_d = din("Dd", [128, 4, 128])
    keys_d = din("keysblk", [128, 8, 256])
    uT_d = din("uT", [D, 16384])
    v_d = din("v", [16384, D])
    gfin_d = din("g_final_r", [1, D])
    out_d = nc.dram_tensor("out", [T, D], F32, kind="ExternalOutput").ap()

    NG = 128 // GI
    uTg_d = nc.dram_tensor("uTgs", [NG, 128, 8, GI * 128], BF16, kind="Internal").ap()
    vg_d = nc.dram_tensor("vgs", [NG, 128, GI, D], BF16, kind="Internal").ap()
    x1s_d = nc.dram_tensor("x1s", [T, D], F32, kind="Internal").ap()

    dbg_outs = {}

    with ExitStack() as es:
        S = Sched(nc, es)

        def V(fn, r=(), w=(), inc=True):
            S.op("vector", fn, r, w, inc)

        def G(fn, r=(), w=(), inc=True):
            S.op("gpsimd", fn, r, w, inc)

        def A(fn, r=(), w=(), inc=True):
            S.op("scalar", fn, r, w, inc)

        def PE(fn, r=(), w=(), inc=True):
            S.op("tensor", fn, r, w, inc)

        dsn = [8]

        def next_ds():
            dsn[0] += 1
            if dsn[0] >= 40:
                dsn[0] = 8
            return dsn[0]

        def dump(name, ap, key, shape, dt=F32):
            if not debug:
                return
            o = nc.dram_tensor("dbg_" + name, list(shape), dt, kind="ExternalOutput").ap()
            dbg_outs["dbg_" + name] = o
            S.dma("sync", o, ap, reads=[key], writes=["dbg_" + name], dsem=next_ds())

        def sb(stack, name, shape, dt):
            return stack.enter_context(nc.sbuf_tensor(name, list(shape), dt))

        def ps(stack, name, shape, dt):
            return stack.enter_context(nc.psum_tensor(name, list(shape), dt))

        P = [ps(es, "P%d" % i, [128, 512], F32) for i in range(7)]
        PT = ps(es, "PT", [128, 1024], BF16)
        PK = ["P%d" % i for i in range(7)]

        identb = sb(es, "identb", [128, 128], BF16)
        identf = sb(es, "identf", [128, 128], F32)
        trib = sb(es, "trib", [128, 128], BF16)
        iota_f = sb(es, "iota_f", [128, 128], F32)
        iota_b = sb(es, "iota_b", [128, 128], BF16)
        jcol = sb(es, "jcol", [128, 1], F32)
        njcol = sb(es, "njcol", [128, 1], F32)
        mhalf = sb(es, "mhalf", [128, 1], F32)
        a1c = sb(es, "a1c", [128, 8], F32)
        sh1c = sb(es, "sh1c", [128, 8], F32)
        a2c = sb(es, "a2c", [128, 8], F32)
        sh2c = sb(es, "sh2c", [128, 8], F32)
        gt1r = sb(es, "gt1r", [128, D], F32)
        gt2r = sb(es, "gt2r", [128, D], F32)
        gfr = sb(es, "gfr", [128, D], F32)

        G(lambda e: e.iota(iota_f[:], [[1, 128]], base=0, channel_multiplier=0, allow_small_or_imprecise_dtypes=True), w=["iota_f"])
        G(lambda e: e.iota(jcol[:], [[0, 1]], base=0, channel_multiplier=1, allow_small_or_imprecise_dtypes=True), w=["jcol"])
        V(lambda e: e.tensor_copy(iota_b[:], iota_f[:]), r=["iota_f"], w=["iota_b"])
        V(lambda e: e.tensor_scalar(njcol[:], jcol[:], -1.0, None, ALU.mult), r=["jcol"], w=["njcol"])
        V(lambda e: e.memset(mhalf[:], -0.5), w=["mhalf"])
        V(lambda e: e.tensor_scalar(identf[:], iota_f[:], jcol[:, 0:1], None, ALU.is_equal), r=["iota_f", "jcol"], w=["identf"])
        V(lambda e: e.tensor_copy(identb[:], identf[:]), r=["identf"], w=["identb"])
        V(lambda e: e.tensor_scalar(trib[:], iota_f[:], jcol[:, 0:1], None, ALU.is_ge), r=["iota_f", "jcol"], w=["trib"])
        S.dma("sync", gfr[:], gfin_d.partition_broadcast(128), writes=["gfr"], dsem=0)

        uT_v = uT_d.rearrange("(k p) e -> p k e", p=128)
        for g in range(NG):
            S.dma("gpsimd", uTg_d[g], uT_v[:, :, g * GI * 128:(g + 1) * GI * 128], writes=["uTg"], dsem=1)
        for g in range(NG):
            S.dma("gpsimd", vg_d[g], v_d[g * GI * 128:(g + 1) * GI * 128, :].rearrange("(il j) d -> j il d", j=128), writes=["vg"], dsem=2)

        with ExitStack() as sa:
            cv = sb(sa, "cv", [128, 8], F32)
            cond = sb(sa, "cond", [128, 8], F32)
            condrep = sb(sa, "condrep", [128, 8, 128], F32)
            wp = [sb(sa, "wadap%d" % i, [128, 8, D], F32) for i in range(2)]
            badac = sb(sa, "badac", [128, 48], F32)
            badar = sb(sa, "badar", [128, 2, D], F32)
            modc = sb(sa, "modc", [128, 48], F32)
            gmixc = sb(sa, "gmixc", [128, 8], F32)
            gffnc = sb(sa, "gffnc", [128, 8], F32)
            S.dma("sync", cv[:], cvec_d, writes=["cv"], dsem=3)
            S.dma("sync", badac[:], badac_d, writes=["badac"], dsem=4)
            S.dma("sync", badar[:, 0, :], badar_d[:, 2 * D:3 * D].partition_broadcast(128), writes=["badar0"], dsem=5)
            S.dma("sync", badar[:, 1, :], badar_d[:, 5 * D:6 * D].partition_broadcast(128), writes=["badar1"], dsem=6)
            S.dma("sync", gmixc[:], gmix_d, writes=["gmixc"], dsem=7)
            S.dma("sync", gffnc[:], gffn_d, writes=["gffnc"], dsem=3)
            A(lambda e: e.activation(cond[:], cv[:], AF.Silu), r=["cv"], w=["cond"])
            V(lambda e: e.tensor_copy(condrep[:], cond[:].unsqueeze(2).to_broadcast([128, 8, 128])), r=["cond"], w=["condrep"])
            wada_v = wada_d.rearrange("(k p) n -> p k n", p=128)
            for piece in range(6):
                sl = piece % 2
                S.dma("sync", wp[sl][:], wada_v[:, :, piece * D:(piece + 1) * D], writes=["wp%d" % sl], dsem=4 + sl)
                if piece in (2, 5):
                    ri = 0 if piece == 2 else 1
                    dst = gt1r if piece == 2 else gt2r
                    for half in range(2):
                        for k in range(8):
                            PE(lambda e, k=k, half=half, sl=sl: e.matmul(
                                P[half][:], condrep[:, k, :], wp[sl][:, k, half * 512:(half + 1) * 512],
                                start=(k == 0), stop=(k == 7)),
                               r=["condrep", "wp%d" % sl], w=[PK[half]], inc=(k == 7))
                        V(lambda e, half=half, ri=ri, dst=dst: e.tensor_tensor(
                            dst[:, half * 512:(half + 1) * 512], P[half][:], badar[:, ri, half * 512:(half + 1) * 512], ALU.add),
                          r=[PK[half], "badar%d" % ri], w=["gtr%d" % ri])
                else:
                    for nl in range(8):
                        col = piece * 8 + nl
                        for k in range(8):
                            PE(lambda e, k=k, nl=nl, sl=sl, col=col: e.matmul(
                                P[2][:, col:col + 1], wp[sl][:, k, nl * 128:(nl + 1) * 128], cond[:, k:k + 1],
                                start=(k == 0), stop=(k == 7)),
                               r=["cond", "wp%d" % sl], w=["P2"], inc=(k == 7 and nl == 7))
            V(lambda e: e.memset(modc[:], 0.0), w=["modc"])
            V(lambda e: e.tensor_tensor(modc[:, 0:16], P[2][:, 0:16], badac[:, 0:16], ALU.add), r=["P2", "badac"], w=["modc"])
            V(lambda e: e.tensor_tensor(modc[:, 24:40], P[2][:, 24:40], badac[:, 24:40], ALU.add), r=["P2", "badac"], w=["modc"])
            V(lambda e: e.tensor_copy(sh1c[:], modc[:, 0:8]), r=["modc"], w=["sh1c"])
            V(lambda e: e.tensor_copy(sh2c[:], modc[:, 24:32]), r=["modc"], w=["sh2c"])
            V(lambda e: e.scalar_tensor_tensor(a1c[:], modc[:, 8:16], 1.0, gmixc[:], ALU.add, ALU.mult), r=["modc", "gmixc"], w=["a1c"])
            V(lambda e: e.scalar_tensor_tensor(a2c[:], modc[:, 32:40], 1.0, gffnc[:], ALU.add, ALU.mult), r=["modc", "gffnc"], w=["a2c"])
            dump("a1c", a1c[:], "a1c", [128, 8])
            dump("sh1c", sh1c[:], "sh1c", [128, 8])
            dump("a2c", a2c[:], "a2c", [128, 8])
            dump("gt1r", gt1r[:], "gtr0", [128, D])
            dump("gt2r", gt2r[:], "gtr1", [128, D])
            S.flush()

        with ExitStack() as pa:
            win = sb(pa, "win", [128, 8, 1536], BF16)
            wout = sb(pa, "wout", [128, 8, D], BF16)
            wglu = sb(pa, "wglu", [128, 4, 512], BF16)
            bblk = sb(pa, "bblk", [128, 4, 1024], BF16)
            crb = sb(pa, "crb", [128, 16, 32], BF16)
            cinb = sb(pa, "cinb", [128, 16, 32], BF16)
            ncrb = sb(pa, "ncrb", [128, 16, 32], BF16)
            ntrib = sb(pa, "ntrib", [128, 128], BF16)
            ddb = sb(pa, "ddb", [128, 4, 128], BF16)
            wmT = sb(pa, "wmT", [128, 4, 128], BF16)
            bsc = sb(pa, "bsc", [128, 4], F32)
            bgluc = sb(pa, "bgluc", [128, 4], F32)
            lngr = sb(pa, "lngr", [128, 512], F32)
            lnbr = sb(pa, "lnbr", [128, 512], F32)
            Mre = sb(pa, "Mre", [128, 2048], F32)
            Mim = sb(pa, "Mim", [128, 2048], F32)
            Pre = sb(pa, "Pre", [128, 16, 128], F32)
            Pim = sb(pa, "Pim", [128, 16, 128], F32)
            A128re = sb(pa, "A128re", [128, 16], F32)
            A128im = sb(pa, "A128im", [128, 16], F32)
            Kre = sb(pa, "Kre", [128, 16], F32)
            Kim = sb(pa, "Kim", [128, 16], F32)

            S.dma("gpsimd", win[:], win_d.rearrange("(k p) n -> p k n", p=128), writes=["win"], dsem=20)
            S.dma("gpsimd", wglu[:], wglu_d.rearrange("(k p) n -> p k n", p=128), writes=["wglu"], dsem=20)
            S.dma("gpsimd", crb[:], CR_d, writes=["crb"], dsem=20)
            S.dma("gpsimd", ddb[:], Dd_d, writes=["ddb"], dsem=20)
            S.dma("sync", bsc[:], bsc_d, writes=["bsc"], dsem=4)
            S.dma("sync", bgluc[:], bgluc_d, writes=["bgluc"], dsem=4)
            S.dma("sync", lngr[:], lng_d.partition_broadcast(128), writes=["lngr"], dsem=5)
            S.dma("sync", lnbr[:], lnb_d.partition_broadcast(128), writes=["lnbr"], dsem=5)
            V(lambda e: e.memset(Kre[:], 0.0), w=["Kre"])
            V(lambda e: e.memset(Kim[:], 0.0), w=["Kim"])

            with ExitStack() as s1_:
                woutf = sb(s1_, "woutf", [128, 8, D], F32)
                S.dma("sync", woutf[:], wout_d.rearrange("(k p) n -> p k n", p=128), writes=["woutf"], dsem=6)
                for k in range(8):
                    V(lambda e, k=k: e.tensor_tensor(wout[:, k, :], woutf[:, k, :], gt1r[:], ALU.mult),
                      r=["woutf", "gtr0"], w=["wout"])
                wsTf = sb(s1_, "wsTf", [128, 4, 128], F32)
                S.dma("sync", wsTf[:], wsT_d, writes=["wsTf"], dsem=7)
                V(lambda e: e.tensor_tensor(wmT[:], wsTf[:], trib[:].unsqueeze(1).to_broadcast([128, 4, 128]), ALU.mult),
                  r=["wsTf", "trib"], w=["wmT"])
                cif = sb(s1_, "cif", [128, 16, 32], F32)
                S.dma("sync", cif[:], CI_d, writes=["cif"], dsem=7)
                V(lambda e: e.tensor_scalar(cinb[:], cif[:], -1.0, None, ALU.mult), r=["cif"], w=["cinb"])
                V(lambda e: e.tensor_scalar(ncrb[:], crb[:], -1.0, None, ALU.mult), r=["crb"], w=["ncrb"])
                V(lambda e: e.tensor_scalar(ntrib[:], trib[:], -1.0, None, ALU.mult), r=["trib"], w=["ntrib"])
                S.flush()

            with ExitStack() as ss_:
                W_ = 512

                def tmp(name, dt=F32):
                    return sb(ss_, name, [128, W_], dt)

                def sincos(ang, ak, osin, ks, ocos, kc, ti, kti, ap=lambda t: t[:]):
                    V(lambda e: e.tensor_scalar(ap(ocos), ap(ang), 1.0 / TWO_PI, None, ALU.mult), r=[ak], w=[kc])
                    V(lambda e: e.tensor_copy(ap(ti), ap(ocos)), r=[kc], w=[kti])
                    V(lambda e: e.tensor_copy(ap(ocos), ap(ti)), r=[kti], w=[kc])
                    V(lambda e: e.scalar_tensor_tensor(ap(ang), ap(ocos), -TWO_PI, ap(ang), ALU.mult, ALU.add), r=[kc, ak], w=[ak])
                    V(lambda e: e.tensor_scalar(ap(ang), ap(ang), -PI, PI, ALU.max, ALU.min), r=[ak], w=[ak])
                    A(lambda e: e.activation(ap(osin), ap(ang), AF.Sin), r=[ak], w=[ks])
                    V(lambda e: e.tensor_scalar(ap(ocos), ap(ang), PI / 2, -TWO_PI, ALU.is_gt, ALU.mult), r=[ak], w=[kc])
                    V(lambda e: e.scalar_tensor_tensor(ap(ocos), ap(ang), PI / 2, ap(ocos), ALU.add, ALU.add), r=[ak, kc], w=[kc])
                    V(lambda e: e.tensor_scalar(ap(ocos), ap(ocos), -PI, PI, ALU.max, ALU.min), r=[kc], w=[kc])
                    A(lambda e: e.activation(ap(ocos), ap(ocos), AF.Sin), r=[kc], w=[kc])

                ebase = tmp("ebase")
                V(lambda e: e.memset(ebase[:], math.e), w=["ebase"])
                are = tmp("are"); aim = tmp("aim"); dlt = tmp("dlt"); lr = tmp("lr"); th = tmp("th")
                ang = tmp("ang"); sn = tmp("sn"); cs_ = tmp("cs_"); ti = tmp("ti", I32); mg = tmp("mg")
                den = tmp("den"); t2 = tmp("t2"); cre = tmp("cre"); cim = tmp("cim")
                brf = tmp("brf"); bif = tmp("bif"); tb1 = tmp("tb1"); tb2 = tmp("tb2")
                for c in range(4):
                    csl = slice(c * W_, (c + 1) * W_)
                    S.dma("sync", are[:], are_r_d[:, csl].partition_broadcast(128), writes=["are"], dsem=3)
                    S.dma("sync", aim[:], aim_r_d[:, csl].partition_broadcast(128), writes=["aim"], dsem=4)
                    S.dma("sync", dlt[:], ldt_r_d[:, csl].partition_broadcast(128), writes=["dlt"], dsem=5)
                    S.dma("sync", brf[:], BR_d[:, c, :], writes=["brf"], dsem=6)
                    S.dma("sync", bif[:], BI_d[:, c, :], writes=["bif"], dsem=7)
                    G(lambda e: e.tensor_tensor(dlt[:], ebase[:], dlt[:], ALU.pow), r=["dlt", "ebase"], w=["dlt"])
                    V(lambda e: e.tensor_scalar(are[:], are[:], -1e-4, None, ALU.min), r=["are"], w=["are"])
                    V(lambda e: e.tensor_tensor(lr[:], are[:], dlt[:], ALU.mult), r=["are", "dlt"], w=["lr"])
                    V(lambda e: e.tensor_tensor(th[:], aim[:], dlt[:], ALU.mult), r=["aim", "dlt"], w=["th"])
                    V(lambda e: e.tensor_copy(ang[:], th[:]), r=["th"], w=["ang"])
                    sincos(ang, "ang", sn, "sn", cs_, "cs_", ti, "ti")
                    A(lambda e: e.activation(mg[:], lr[:], AF.Exp), r=["lr"], w=["mg"])
                    V(lambda e: e.tensor_tensor(cs_[:], mg[:], cs_[:], ALU.mult), r=["mg", "cs_"], w=["cs_"])
                    V(lambda e: e.tensor_scalar(cs_[:], cs_[:], -1.0, None, ALU.add), r=["cs_"], w=["cs_"])
                    V(lambda e: e.tensor_tensor(sn[:], mg[:], sn[:], ALU.mult), r=["mg", "sn"], w=["sn"])
                    V(lambda e: e.tensor_tensor(den[:], are[:], are[:], ALU.mult), r=["are"], w=["den"])
                    V(lambda e: e.tensor_tensor(t2[:], aim[:], aim[:], ALU.mult), r=["aim"], w=["t2"])
                    V(lambda e: e.tensor_tensor(den[:], den[:], t2[:], ALU.add), r=["den", "t2"], w=["den"])
                    V(lambda e: e.reciprocal(den[:], den[:]), r=["den"], w=["den"])
                    V(lambda e: e.tensor_tensor(cre[:], cs_[:], are[:], ALU.mult), r=["cs_", "are"], w=["cre"])
                    V(lambda e: e.tensor_tensor(t2[:], sn[:], aim[:], ALU.mult), r=["sn", "aim"], w=["t2"])
                    V(lambda e: e.tensor_tensor(cre[:], cre[:], t2[:], ALU.add), r=["cre", "t2"], w=["cre"])
                    V(lambda e: e.tensor_tensor(cre[:], cre[:], den[:], ALU.mult), r=["cre", "den"], w=["cre"])
                    V(lambda e: e.tensor_tensor(cim[:], sn[:], are[:], ALU.mult), r=["sn", "are"], w=["cim"])
                    V(lambda e: e.tensor_tensor(t2[:], cs_[:], aim[:], ALU.mult), r=["cs_", "aim"], w=["t2"])
                    V(lambda e: e.tensor_tensor(cim[:], cim[:], t2[:], ALU.subtract), r=["cim", "t2"], w=["cim"])
                    V(lambda e: e.tensor_tensor(cim[:], cim[:], den[:], ALU.mult), r=["cim", "den"], w=["cim"])
                    if c == 0:
                        dump("cre", cre[:], "cre", [128, W_])
                        dump("cim", cim[:], "cim", [128, W_])
                    V(lambda e: e.tensor_tensor(tb1[:], cre[:], brf[:], ALU.mult), r=["cre", "brf"], w=["tb1"])
                    V(lambda e: e.tensor_tensor(tb2[:], cim[:], bif[:], ALU.mult), r=["cim", "bif"], w=["tb2"])
                    V(lambda e, c=c: e.tensor_tensor(bblk[:, c, 0:512], tb1[:], tb2[:], ALU.subtract), r=["tb1", "tb2"], w=["bblk"])
                    V(lambda e: e.tensor_tensor(tb1[:], cre[:], bif[:], ALU.mult), r=["cre", "bif"], w=["tb1"])
                    V(lambda e: e.tensor_tensor(tb2[:], cim[:], brf[:], ALU.mult), r=["cim", "brf"], w=["tb2"])
                    V(lambda e, c=c: e.tensor_tensor(bblk[:, c, 512:1024], tb1[:], tb2[:], ALU.add), r=["tb1", "tb2"], w=["bblk"])
                    V(lambda e: e.tensor_scalar(ang[:], th[:], jcol[:, 0:1], None, ALU.mult), r=["th", "jcol"], w=["ang"])
                    sincos(ang, "ang", sn, "sn", cs_, "cs_", ti, "ti")
                    A(lambda e: e.activation(mg[:], lr[:], AF.Exp, scale=njcol[:, 0:1]), r=["lr", "njcol"], w=["mg"])
                    V(lambda e, csl=csl: e.tensor_tensor(Mre[:, csl], mg[:], cs_[:], ALU.mult), r=["mg", "cs_"], w=["Mre"])
                    V(lambda e, csl=csl: e.scalar_tensor_tensor(Mim[:, csl], mg[:], -1.0, sn[:], ALU.mult, ALU.mult), r=["mg", "sn"], w=["Mim"])
                arc = sb(ss_, "arc", [128, 16], F32)
                aic = sb(ss_, "aic", [128, 16], F32)
                dtc = sb(ss_, "dtc", [128, 16], F32)
                lrc = sb(ss_, "lrc", [128, 16], F32)
                thc = sb(ss_, "thc", [128, 16], F32)
                S.dma("sync", arc[:], are_c_d, writes=["arc"], dsem=3)
                S.dma("sync", aic[:], aim_c_d, writes=["aic"], dsem=4)
                S.dma("sync", dtc[:], ldt_c_d, writes=["dtc"], dsem=5)
                G(lambda e: e.tensor_tensor(dtc[:], ebase[:, 0:16], dtc[:], ALU.pow), r=["dtc", "ebase"], w=["dtc"])
                V(lambda e: e.tensor_scalar(arc[:], arc[:], -1e-4, None, ALU.min), r=["arc"], w=["arc"])
                V(lambda e: e.tensor_tensor(lrc[:], arc[:], dtc[:], ALU.mult), r=["arc", "dtc"], w=["lrc"])
                V(lambda e: e.tensor_tensor(thc[:], aic[:], dtc[:], ALU.mult), r=["aic", "dtc"], w=["thc"])
                v3 = lambda t: t[:].rearrange("p (q i) -> p q i", q=4)
                iot3 = iota_f[:].unsqueeze(1).to_broadcast([128, 4, 128])
                for c in range(4):
                    qs = slice(4 * c, 4 * c + 4)
                    V(lambda e, qs=qs: e.tensor_tensor(v3(ang), thc[:, qs].unsqueeze(2).to_broadcast([128, 4, 128]), iot3, ALU.mult),
                      r=["thc", "iota_f"], w=["ang"])
                    V(lambda e, qs=qs: e.tensor_tensor(v3(t2), lrc[:, qs].unsqueeze(2).to_broadcast([128, 4, 128]), iot3, ALU.mult),
                      r=["lrc", "iota_f"], w=["t2"])
                    sincos(ang, "ang", sn, "sn", cs_, "cs_", ti, "ti")
                    A(lambda e: e.activation(mg[:], t2[:], AF.Exp), r=["t2"], w=["mg"])
                    V(lambda e, qs=qs: e.tensor_tensor(Pre[:, qs, :], v3(mg), v3(cs_), ALU.mult), r=["mg", "cs_"], w=["Pre"])
                    V(lambda e, qs=qs: e.tensor_tensor(Pim[:, qs, :], v3(mg), v3(sn), ALU.mult), r=["mg", "sn"], w=["Pim"])
                a16 = lambda t: t[:, 0:16]
                V(lambda e: e.tensor_scalar(a16(ang), thc[:], 128.0, None, ALU.mult), r=["thc"], w=["ang"])
                sincos(ang, "ang", sn, "sn", cs_, "cs_", ti, "ti", ap=a16)
                A(lambda e: e.activation(a16(mg), lrc[:], AF.Exp, scale=128.0), r=["lrc"], w=["mg"])
                V(lambda e: e.tensor_tensor(A128re[:], a16(mg), a16(cs_), ALU.mult), r=["mg", "cs_"], w=["A128re"])
                V(lambda e: e.tensor_tensor(A128im[:], a16(mg), a16(sn), ALU.mult), r=["mg", "sn"], w=["A128im"])
                dump("Mre", Mre[:], "Mre", [128, 2048])
                dump("Mim", Mim[:], "Mim", [128, 2048])
                dump("Pre", Pre[:], "Pre", [128, 16, 128])
                dump("Pim", Pim[:], "Pim", [128, 16, 128])
                dump("A128re", A128re[:], "A128re", [128, 16])
                S.flush()

            XT = [sb(pa, "xt%d" % i, [128, D], F32) for i in range(2)]
            junk = sb(pa, "junk", [128, D], BF16)
            ssq = sb(pa, "ssq", [128, 1], F32)
            ssq2 = sb(pa, "ssq2", [128, 1], F32)
            rstd = sb(pa, "rstd", [128, 1], F32)
            xn = sb(pa, "xn", [128, D], BF16)
            hT = sb(pa, "hT", [128, 8, 128], BF16)
            ug = sb(pa, "ug", [128, 512], F32)
            vg = sb(pa, "vg", [128, 512], F32)
            ZST = [sb(pa, "zsT%d" % i, [128, 4, 128], BF16) for i in range(2)]
            FMAX = int(nc.vector.BN_STATS_FMAX)
            nst = max(1, 512 // FMAX)
            bst = sb(pa, "bst", [128, nst, int(nc.vector.BN_STATS_DIM)], F32)
            bmv = sb(pa, "bmv", [128, int(nc.vector.BN_AGGR_DIM)], F32)
            var2 = sb(pa, "var2", [128, 1], F32)
            rstdv = sb(pa, "rstdv", [128, 1], F32)
            vn = sb(pa, "vn", [128, 512], F32)
            vn2 = sb(pa, "vn2", [128, 512], F32)
            vb16 = sb(pa, "vb16", [128, 512], BF16)
            ygm = sb(pa, "ygm", [128, 512], BF16)
            YCT = [sb(pa, "ycatT%d" % i, [128, 8, 128], BF16) for i in range(2)]
            bq = sb(pa, "bq", [128, 4, 4, 512], BF16)
            ure = sb(pa, "ure", [128, 4, 128], F32)
            uim = sb(pa, "uim", [128, 4, 128], F32)
            ctmp = [sb(pa, "ctmp%d" % i, [128, 4], F32) for i in range(4)]
            xq = sb(pa, "xq", [128, 16, 4, 128], BF16)
            yg = sb(pa, "yg", [128, 512], BF16)
            ygT = sb(pa, "ygT", [128, 4, 128], BF16)
            sig = sb(pa, "sig", [128, 4, 128], BF16)
            x1t = sb(pa, "x1t", [128, D], F32)

            def front(n):
                sl = n % 2
                yield
                xt = XT[sl]
                yield
                xk = "xt%d" % sl
                zsT = ZST[sl]
                zk = "zsT%d" % sl
                ycatT = YCT[sl]
                yk = "ycatT%d" % sl
                yield
                S.dma("sync", xt[:], x_d[n * 128:(n + 1) * 128, :], writes=[xk], dsem=8 + sl)
                yield
                yield
                V(lambda e, xt=xt: e.scalar_tensor_tensor(junk[:], xt[:], 1.0, xt[:], ALU.mult, ALU.mult, accum_out=ssq[:]),
                  r=[xk], w=["junk", "ssq"])
                yield
                V(lambda e: e.tensor_scalar(ssq2[:], ssq[:], 1.0 / D, EPS, ALU.mult, ALU.add), r=["ssq"], w=["ssq2"])
                yield
                A(lambda e: e.activation(rstd[:], ssq2[:], AF.Sqrt), r=["ssq2"], w=["rstd"])
                V(lambda e: e.reciprocal(rstd[:], rstd[:]), r=["rstd"], w=["rstd"])
                yield
                V(lambda e, xt=xt: e.tensor_scalar(xn[:], xt[:], rstd[:, 0:1], None, ALU.mult), r=[xk, "rstd"], w=["xn"])
                yield
                for k in range(8):
                    PE(lambda e, k=k: e.transpose(PT[:, k * 128:(k + 1) * 128], xn[:, k * 128:(k + 1) * 128], identb[:]),
                       r=["xn", "identb"], w=["PT"], inc=(k == 7))
                yield
                for k in range(8):
                    A(lambda e, k=k: e.activation(hT[:, k, :], PT[:, k * 128:(k + 1) * 128], AF.Identity,
                                                  bias=sh1c[:, k:k + 1], scale=a1c[:, k:k + 1]),
                      r=["PT", "sh1c", "a1c"], w=["hT"])
                yield
                yield
                for half in range(2):
                    for k in range(8):
                        PE(lambda e, k=k, half=half: e.matmul(P[half][:], hT[:, k, :], win[:, k, half * 512:(half + 1) * 512],
                                                              start=(k == 0), stop=(k == 7)),
                           r=["hT", "win"], w=[PK[half]], inc=(k == 7))
                yield
                for f in range(4):
                    for k in range(8):
                        PE(lambda e, k=k, f=f: e.matmul(P[2][:, f * 128:(f + 1) * 128], win[:, k, 1024 + f * 128:1024 + (f + 1) * 128],
                                                        hT[:, k, :], start=(k == 0), stop=(k == 7)),
                           r=["hT", "win"], w=["P2"], inc=(k == 7 and f == 3))
                yield
                A(lambda e: e.activation(ug[:], P[0][:], AF.Gelu), r=["P0"], w=["ug"])
                yield
                A(lambda e: e.activation(vg[:], P[1][:], AF.Gelu), r=["P1"], w=["vg"])
                yield
                A(lambda e: e.copy(zsT[:].rearrange("p c t -> p (c t)"), P[2][:]), r=["P2"], w=[zk])
                yield
                yield
                for i in range(nst):
                    w_ = 512 // nst
                    V(lambda e, i=i, w_=w_: e.bn_stats(bst[:, i, :], vg[:, i * w_:(i + 1) * w_]), r=["vg"], w=["bst"])
                yield
                V(lambda e: e.bn_aggr(bmv[:], bst[:]), r=["bst"], w=["bmv"])
                yield
                V(lambda e: e.tensor_scalar(var2[:], bmv[:, 1:2], EPS, None, ALU.add), r=["bmv"], w=["var2"])
                yield
                A(lambda e: e.activation(rstdv[:], var2[:], AF.Sqrt), r=["var2"], w=["rstdv"])
                V(lambda e: e.reciprocal(rstdv[:], rstdv[:]), r=["rstdv"], w=["rstdv"])
                yield
                V(lambda e: e.tensor_scalar(vn[:], vg[:], bmv[:, 0:1], rstdv[:, 0:1], ALU.subtract, ALU.mult),
                  r=["vg", "bmv", "rstdv"], w=["vn"])
                yield
                V(lambda e: e.tensor_tensor(vn2[:], vn[:], lngr[:], ALU.mult), r=["vn", "lngr"], w=["vn2"])
                yield
                V(lambda e: e.tensor_tensor(vb16[:], vn2[:], lnbr[:], ALU.add), r=["vn2", "lnbr"], w=["vb16"])
                yield
                yield
                for h in range(4):
                    PE(lambda e, h=h: e.matmul(P[2][:, h * 128:(h + 1) * 128], wmT[:, h, :], vb16[:, h * 128:(h + 1) * 128],
                                               start=True, stop=True),
                       r=["wmT", "vb16"], w=["P2"], inc=(h == 3))
                yield
                for h in range(4):
                    V(lambda e, h=h: e.scalar_tensor_tensor(ygm[:, h * 128:(h + 1) * 128], P[2][:, h * 128:(h + 1) * 128],
                                                            bsc[:, h:h + 1], ug[:, h * 128:(h + 1) * 128], ALU.add, ALU.mult),
                      r=["P2", "bsc", "ug"], w=["ygm"])
                yield
                for h in range(4):
                    PE(lambda e, h=h: e.transpose(PT[:, h * 128:(h + 1) * 128], ygm[:, h * 128:(h + 1) * 128], identb[:]),
                       r=["ygm", "identb"], w=["PT"], inc=(h == 3))
                yield
                A(lambda e: e.copy(ycatT[:, 0:4, :].rearrange("p c t -> p (c t)"), PT[:, 0:512]), r=["PT"], w=[yk])
                yield

            def back(n):
                sl = n % 2
                xt = XT[sl]
                xk = "xt%d" % sl
                zsT = ZST[sl]
                zk = "zsT%d" % sl
                ycatT = YCT[sl]
                yk = "ycatT%d" % sl
                yield
                for c in range(4):
                    pr_, pi_ = (4, 5)
                    PE(lambda e, c=c, pr_=pr_: e.matmul(P[pr_][:], zsT[:, c, :], bblk[:, c, 0:512], start=True, stop=True),
                       r=[zk, "bblk"], w=[PK[pr_]], inc=False)
                    PE(lambda e, c=c, pi_=pi_: e.matmul(P[pi_][:], zsT[:, c, :], bblk[:, c, 512:1024], start=True, stop=True),
                       r=[zk, "bblk"], w=[PK[pi_]], inc=True)
                    csl = slice(c * 512, (c + 1) * 512)
                    bk_ = "bum%d" % c
                    V(lambda e, c=c, pr_=pr_, csl=csl: e.tensor_tensor(bq[:, c, 0, :], P[pr_][:], Mre[:, csl], ALU.mult),
                      r=[PK[pr_], "Mre"], w=[bk_])
                    V(lambda e, c=c, pi_=pi_, csl=csl: e.tensor_tensor(bq[:, c, 1, :], P[pi_][:], Mim[:, csl], ALU.mult),
                      r=[PK[pi_], "Mim"], w=[bk_])
                    V(lambda e, c=c, pi_=pi_, csl=csl: e.tensor_tensor(bq[:, c, 2, :], P[pi_][:], Mre[:, csl], ALU.mult),
                      r=[PK[pi_], "Mre"], w=[bk_])
                    V(lambda e, c=c, pr_=pr_, csl=csl: e.tensor_tensor(bq[:, c, 3, :], P[pr_][:], Mim[:, csl], ALU.mult),
                      r=[PK[pr_], "Mim"], w=[bk_])
                for c in range(4):
                    for (ja, jb, tb_, pb) in ((0, 1, ntrib, 6), (2, 3, trib, 3)):
                        for ql in range(4):
                            PE(lambda e, c=c, ja=ja, pb=pb, ql=ql: e.matmul(
                                P[pb][:, ql * 128:(ql + 1) * 128], bq[:, c, ja, ql * 128:(ql + 1) * 128], trib[:],
                                start=True, stop=False),
                               r=["bum%d" % c, "trib"], w=[PK[pb]], inc=False)
                            PE(lambda e, c=c, jb=jb, tb_=tb_, pb=pb, ql=ql: e.matmul(
                                P[pb][:, ql * 128:(ql + 1) * 128], bq[:, c, jb, ql * 128:(ql + 1) * 128], tb_[:],
                                start=False, stop=True),
                               r=["bum%d" % c, "trib", "ntrib"], w=[PK[pb]], inc=(ql == 3))
                    qs = slice(4 * c, 4 * c + 4)
                    V(lambda e, qs=qs: e.tensor_tensor(ure[:], P[6][:].rearrange("p (q i) -> p q i", q=4),
                                                       Kre[:, qs].unsqueeze(2).to_broadcast([128, 4, 128]), ALU.add),
                      r=["P6", "Kre"], w=["ure"])
                    V(lambda e, qs=qs: e.tensor_tensor(uim[:], P[3][:].rearrange("p (q i) -> p q i", q=4),
                                                       Kim[:, qs].unsqueeze(2).to_broadcast([128, 4, 128]), ALU.add),
                      r=["P3", "Kim"], w=["uim"])
                    ur7 = ure[:, :, 127:128].rearrange("p q o -> p (q o)")
                    ui7 = uim[:, :, 127:128].rearrange("p q o -> p (q o)")
                    G(lambda e, qs=qs, ur7=ur7: e.tensor_tensor(ctmp[0][:], A128re[:, qs], ur7, ALU.mult), r=["A128re", "ure"], w=["ct0"])
                    G(lambda e, qs=qs, ui7=ui7: e.tensor_tensor(ctmp[1][:], A128im[:, qs], ui7, ALU.mult), r=["A128im", "uim"], w=["ct1"])
                    G(lambda e, qs=qs, ui7=ui7: e.tensor_tensor(ctmp[2][:], A128re[:, qs], ui7, ALU.mult), r=["A128re", "uim"], w=["ct2"])
                    G(lambda e, qs=qs, ur7=ur7: e.tensor_tensor(ctmp[3][:], A128im[:, qs], ur7, ALU.mult), r=["A128im", "ure"], w=["ct3"])
                    G(lambda e, qs=qs: e.tensor_tensor(Kre[:, qs], ctmp[0][:], ctmp[1][:], ALU.subtract), r=["ct0", "ct1"], w=["Kre"])
                    G(lambda e, qs=qs: e.tensor_tensor(Kim[:, qs], ctmp[2][:], ctmp[3][:], ALU.add), r=["ct2", "ct3"], w=["Kim"])
                    xk_ = "xs%d" % c
                    u3r = ure[:]
                    u3i = uim[:]
                    V(lambda e, qs=qs, u3r=u3r: e.tensor_tensor(xq[:, qs, 0, :], u3r, Pre[:, qs, :], ALU.mult), r=["ure", "Pre"], w=[xk_])
                    V(lambda e, qs=qs, u3i=u3i: e.tensor_tensor(xq[:, qs, 1, :], u3i, Pim[:, qs, :], ALU.mult), r=["uim", "Pim"], w=[xk_])
                    G(lambda e, qs=qs, u3i=u3i: e.tensor_tensor(xq[:, qs, 2, :], u3i, Pre[:, qs, :], ALU.mult), r=["uim", "Pre"], w=[xk_])
                    G(lambda e, qs=qs, u3r=u3r: e.tensor_tensor(xq[:, qs, 3, :], u3r, Pim[:, qs, :], ALU.mult), r=["ure", "Pim"], w=[xk_])
                    yield
                yield
                for c in range(4):
                    PE(lambda e, c=c: e.matmul(P[4][:, c * 128:(c + 1) * 128], zsT[:, c, :], ddb[:, c, :], start=True, stop=False,
                                               skip_group_check=True),
                       r=[zk, "ddb"], w=["P4"], inc=False)
                    for ql in range(4):
                        q = 4 * c + ql
                        for (j_, ct_, ck_) in ((0, crb, "crb"), (1, ncrb, "ncrb"), (2, cinb, "cinb"), (3, cinb, "cinb")):
                            PE(lambda e, q=q, j_=j_, ct_=ct_: e.matmul(P[4][:, q * 32:(q + 1) * 32], xq[:, q, j_, :], ct_[:, q, :],
                                                                       start=False, stop=(j_ == 3), skip_group_check=True),
                               r=["xs%d" % c, ck_], w=["P4"], inc=(ql == 3 and j_ == 3))
                yield
                A(lambda e: e.activation(yg[:], P[4][:], AF.Gelu), r=["P4"], w=["yg"])
                yield
                for c in range(4):
                    PE(lambda e, c=c: e.transpose(PT[:, 512 + c * 128:512 + (c + 1) * 128], yg[:, c * 128:(c + 1) * 128], identb[:]),
                       r=["yg", "identb"], w=["PT"], inc=(c == 3))
                yield
                V(lambda e: e.tensor_copy(ygT[:].rearrange("p c t -> p (c t)"), PT[:, 512:1024]), r=["PT"], w=["ygT"])
                yield
                for fo in range(4):
                    for c in range(4):
                        PE(lambda e, fo=fo, c=c: e.matmul(P[6][:, fo * 128:(fo + 1) * 128], wglu[:, c, fo * 128:(fo + 1) * 128], ygT[:, c, :],
                                                          start=(c == 0), stop=(c == 3)),
                           r=["wglu", "ygT"], w=["P6"], inc=(c == 3 and fo == 3))
                yield
                for fo in range(4):
                    A(lambda e, fo=fo: e.activation(sig[:, fo, :], P[6][:, fo * 128:(fo + 1) * 128], AF.Sigmoid, bias=bgluc[:, fo:fo + 1]),
                      r=["P6", "bgluc"], w=["sig"])
                yield
                V(lambda e: e.tensor_tensor(ycatT[:, 4:8, :], ygT[:], sig[:], ALU.mult), r=["ygT", "sig"], w=[yk])
                yield
                yield
                for half in range(2):
                    pb = 4 + half
                    for k in range(8):
                        PE(lambda e, k=k, half=half, pb=pb: e.matmul(P[pb][:], ycatT[:, k, :], wout[:, k, half * 512:(half + 1) * 512],
                                                                     start=(k == 0), stop=(k == 7)),
                           r=[yk, "wout"], w=[PK[pb]], inc=(k == 7))
                    V(lambda e, half=half, pb=pb, xt=xt: e.tensor_tensor(x1t[:, half * 512:(half + 1) * 512], P[pb][:],
                                                                         xt[:, half * 512:(half + 1) * 512], ALU.add),
                      r=[PK[pb], xk], w=["x1t"])
                yield
                S.dma("sync", x1s_d[n * 128:(n + 1) * 128, :], x1t[:], reads=["x1t"], writes=["x1s"], dsem=10)
                yield
                if debug and n == 0:
                    dump("hT", hT[:], "hT", [128, 8, 128], BF16)
                    dump("ug", ug[:], "ug", [128, 512])
                    dump("vb16", vb16[:], "vb16", [128, 512], BF16)
                    dump("ygm", ygm[:], "ygm", [128, 512], BF16)
                    dump("zsT", zsT[:], zk, [128, 4, 128], BF16)
                    dump("yg", yg[:], "yg", [128, 512], BF16)
                    dump("ycatT", ycatT[:], yk, [128, 8, 128], BF16)
                    dump("x1t", x1t[:], "x1t", [128, D])
                yield

            def drain(g):
                for _ in g:
                    pass

            drain(front(0))
            for n in range(NCH):
                fg = front(n + 1) if n + 1 < NCH else iter(())
                for _ in back(n):
                    next(fg, None)
                    next(fg, None)
                    next(fg, None)
                drain(fg)
            S.wait_all("sync", ["x1s"])
            S.flush()

        with ExitStack() as pb_:
            wq = sb(pb_, "wq", [128, 8, D], BF16)
            keysb = sb(pb_, "keysb", [128, 8, 256], BF16)
            S.dma("gpsimd", wq[:], wq_d.rearrange("(k p) n -> p k n", p=128), writes=["wq"], dsem=21)
            S.dma("gpsimd", keysb[:], keys_d, writes=["keysb"], dsem=21)
            x1b = [sb(pb_, "x1b%d" % i, [128, NSUB, D], F32) for i in range(2)]
            h2T = [sb(pb_, "h2T%d" % i, [128, 8, TB], BF16) for i in range(2)]
            qT = sb(pb_, "qT", [128, 8, TB], BF16)
            gT = sb(pb_, "gT", [128, TB], BF16)
            ihT = sb(pb_, "ihT", [128, TB], BF16)
            jlT = sb(pb_, "jlT", [128, TB], BF16)
            Wsel = sb(pb_, "Wsel", [128, TB, 128], BF16)
            ssb = sb(pb_, "ssb", [128, 1], F32)
            ssb2 = sb(pb_, "ssb2", [128, 1], F32)
            rstb = sb(pb_, "rstb", [128, 1], F32)
            ssr = sb(pb_, "ssr", [128, 1], F32)
            ssr2 = sb(pb_, "ssr2", [128, 1], F32)
            rstr = sb(pb_, "rstr", [128, 1], F32)
            pt1 = sb(pb_, "pt1", [128, D], F32)
            junk2 = sb(pb_, "junk2", [128, D], BF16)
            TS = 8
            xn2 = sb(pb_, "xn2", [128, D], BF16)
            scs = sb(pb_, "scs", [128, 16, 128], F32)
            work = [sb(pb_, "work%d" % i, [128, 256], F32) for i in range(2)]
            v1 = sb(pb_, "v1", [128, 16, 16], F32)
            i1 = sb(pb_, "i1", [128, 16, 16], U32)
            i1f = sb(pb_, "i1f", [128, 16, 16], F32)
            cand = sb(pb_, "cand", [128, 8, 256], F32)
            b1 = sb(pb_, "b1", [128, 8, 16], F32)
            pos = sb(pb_, "pos", [128, 8, 16], U32)
            posa = sb(pb_, "posa", [128, 8, 16], U32)
            posb = sb(pb_, "posb", [128, 8, 16], U32)
            posaf = sb(pb_, "posaf", [128, 8, 16], F32)
            posbf = sb(pb_, "posbf", [128, 8, 16], F32)
            oh = sb(pb_, "oh", [128, 8, 16, 16], BF16)
            sm = sb(pb_, "sm", [128, 8, 16], F32)
            smz = sb(pb_, "smz", [128, 8], F32)
            gw = sb(pb_, "gw", [128, 128], F32)
            ihi = sb(pb_, "ihi", [128, 128], F32)
            jlo = sb(pb_, "jlo", [128, 128], F32)
            OA = [sb(pb_, "OA%d" % i, [128, TS, 128], BF16) for i in range(2)]
            OBw = [sb(pb_, "OBw%d" % i, [128, TS, 128], BF16) for i in range(2)]
            ub = [sb(pb_, "ub%d" % i, [128, 8, GI * 128], BF16) for i in range(2)]
            vbuf = [sb(pb_, "vbuf%d" % i, [128, GI, D], BF16) for i in range(2)]
            actg = [sb(pb_, "actg%d" % i, [128, TB], BF16) for i in range(2)]
            wf = [sb(pb_, "wf%d" % i, [128, TB], BF16) for i in range(2)]
            nact = [0]

            def routing_stages(blk):
                hb = blk % 2
                t0 = blk * TB
                xb = x1b[hb]
                xbk = "x1b%d" % hb
                hT_ = h2T[hb]
                hk = "h2T%d" % hb
                st = []

                def add(f):
                    st.append(f)

                add(lambda: S.dma("sync", xb[:], x1s_d[t0:t0 + TB, :].rearrange("(s p) d -> p s d", p=128),
                                  reads=["x1s"], writes=[xbk], dsem=18 + hb))
                for s in range(NSUB):
                    tsl = slice(s * 128, (s + 1) * 128)

                    def f_rms(s=s):
                        V(lambda e: e.scalar_tensor_tensor(junk2[:], xb[:, s, :], 1.0, xb[:, s, :], ALU.mult, ALU.mult, accum_out=ssr[:]),
                          r=[xbk], w=["junk2", "ssr"])
                        V(lambda e: e.tensor_scalar(ssr2[:], ssr[:], 1.0 / D, EPS, ALU.mult, ALU.add), r=["ssr"], w=["ssr2"])
                        G(lambda e: e.tensor_tensor(rstr[:], ssr2[:], mhalf[:], ALU.pow), r=["ssr2", "mhalf"], w=["rstr"])
                    add(f_rms)
                    add(lambda s=s: V(lambda e: e.tensor_scalar(xn2[:], xb[:, s, :], rstr[:, 0:1], None, ALU.mult), r=[xbk, "rstr"], w=["xn2"]))

                    def f_tr():
                        for k in range(8):
                            PE(lambda e, k=k: e.transpose(PT[:, k * 128:(k + 1) * 128], xn2[:, k * 128:(k + 1) * 128], identb[:]),
                               r=["xn2", "identb"], w=["PT"], inc=(k == 7))
                    add(f_tr)
                    for k0 in (0, 4):
                        def f_ev(k0=k0, tsl=tsl):
                            for k in range(k0, k0 + 4):
                                A(lambda e, k=k: e.activation(hT_[:, k, tsl], PT[:, k * 128:(k + 1) * 128], AF.Identity,
                                                              bias=sh2c[:, k:k + 1], scale=a2c[:, k:k + 1]),
                                  r=["PT", "sh2c", "a2c"], w=[hk])
                        add(f_ev)
                for m in range(8):
                    hsl = slice((m % 2) * 256, (m % 2) * 256 + TB)
                    pk6 = "P6"

                    def f_q(m=m, hsl=hsl, pk6=pk6):
                        for k in range(8):
                            PE(lambda e, k=k: e.matmul(P[6][:, hsl], wq[:, k, m * 128:(m + 1) * 128], hT_[:, k, :],
                                                       start=(k == 0), stop=(k == 7)),
                               r=["wq", hk], w=[pk6], inc=(k == 7))
                    add(f_q)
                    add(lambda m=m, hsl=hsl, pk6=pk6: A(lambda e: e.copy(qT[:, m, :], P[6][:, hsl]), r=[pk6], w=["qT"]))
                for s in range(NSUB):
                    tsl = slice(s * 128, (s + 1) * 128)
                    for h in range(8):
                        hsl = slice((h % 2) * 256, (h % 2 + 1) * 256)
                        pk6 = "P6"

                        def f_sc(h=h, hsl=hsl, pk6=pk6, tsl=tsl):
                            PE(lambda e: e.matmul(P[6][:, hsl], qT[:, h, tsl], keysb[:, h, :], start=True, stop=True),
                               r=["qT", "keysb"], w=[pk6])
                            A(lambda e: e.copy(scs[:, 2 * h:2 * h + 2, :].rearrange("p a k -> p (a k)"), P[6][:, hsl]),
                              r=[pk6], w=["scs%d" % (2 * h), "scs%d" % (2 * h + 1)])
                        add(f_sc)
                    for hc in range(16):
                        wk = work[hc % 2]
                        wkk = "work%d" % (hc % 2)
                        sk = "scs%d" % hc
                        vk = "v1_%d" % hc
                        ik = "i1_%d" % hc

                        def f_a(hc=hc, wk=wk, wkk=wkk, sk=sk, vk=vk):
                            V(lambda e: e.max(v1[:, hc, 0:8], scs[:, hc, :]), r=[sk], w=[vk])
                            V(lambda e: e.match_replace(wk[:, 0:128], v1[:, hc, 0:8], scs[:, hc, :], -1e30), r=[sk, vk], w=[wkk])
                            V(lambda e: e.max(v1[:, hc, 8:16], wk[:, 0:128]), r=[wkk], w=[vk])
                        add(f_a)

                        def f_b(hc=hc, sk=sk, vk=vk, ik=ik):
                            V(lambda e: e.max_index(i1[:, hc, 0:8], v1[:, hc, 0:8], scs[:, hc, :]), r=[sk, vk], w=[ik])
                            V(lambda e: e.max_index(i1[:, hc, 8:16], v1[:, hc, 8:16], scs[:, hc, :]), r=[sk, vk], w=[ik])
                        add(f_b)
                    i1keys = ["i1_%d" % hc for hc in range(16)]
                    add(lambda: V(lambda e: e.tensor_copy(i1f[:], i1[:]), r=i1keys, w=["i1f"]))
                    v1v = v1[:].rearrange("p (h c) k -> p h c k", c=2)
                    i1v = i1f[:].rearrange("p (h c) k -> p h c k", c=2)
                    for h in range(8):
                        wk = work[h % 2]
                        wkk = "work%d" % (h % 2)
                        ck = "cand%d" % h
                        bk = "b1_%d" % h
                        pk_ = "pos%d" % h

                        def f_c(h=h, wk=wk, wkk=wkk, ck=ck, bk=bk):
                            V(lambda e: e.tensor_tensor(cand[:, h, :].rearrange("p (a b) -> p a b", a=16),
                                                        v1v[:, h, 0, :].unsqueeze(2).to_broadcast([128, 16, 16]),
                                                        v1v[:, h, 1, :].unsqueeze(1).to_broadcast([128, 16, 16]), ALU.add),
                              r=["v1_%d" % (2 * h), "v1_%d" % (2 * h + 1)], w=[ck])
                            V(lambda e: e.max(b1[:, h, 0:8], cand[:, h, :]), r=[ck], w=[bk])
                            V(lambda e: e.match_replace(wk[:], b1[:, h, 0:8], cand[:, h, :], -1e30), r=[ck, bk], w=[wkk])
                        add(f_c)

                        def f_d(h=h, wk=wk, wkk=wkk, ck=ck, bk=bk, pk_=pk_):
                            V(lambda e: e.max(b1[:, h, 8:16], wk[:]), r=[wkk], w=[bk])
                            V(lambda e: e.max_index(pos[:, h, 0:8], b1[:, h, 0:8], cand[:, h, :]), r=[ck, bk], w=[pk_])
                            V(lambda e: e.max_index(pos[:, h, 8:16], b1[:, h, 8:16], cand[:, h, :]), r=[ck, bk], w=[pk_])
                        add(f_d)
                    b1keys = ["b1_%d" % h for h in range(8)]
                    poskeys = ["pos%d" % h for h in range(8)]

                    def f_sm():
                        V(lambda e: e.tensor_tensor(sm[:], b1[:], b1[:, :, 0:1].to_broadcast([128, 8, 16]), ALU.subtract), r=b1keys, w=["sm"])
                        A(lambda e: e.activation(sm[:], sm[:], AF.Exp), r=["sm"], w=["sm"])
                    add(f_sm)

                    def f_sm2():
                        V(lambda e: e.tensor_reduce(smz[:], sm[:], AX.X, ALU.add), r=["sm"], w=["smz"])
                        V(lambda e: e.reciprocal(smz[:], smz[:]), r=["smz"], w=["smz"])
                        V(lambda e: e.tensor_tensor(gw[:].rearrange("p (h k) -> p h k", h=8), sm[:],
                                                    smz[:].unsqueeze(2).to_broadcast([128, 8, 16]), ALU.mult),
                          r=["sm", "smz"], w=["gw"])
                    add(f_sm2)

                    def f_pos():
                        V(lambda e: e.tensor_single_scalar(posa[:], pos[:], 4, ALU.logical_shift_right), r=poskeys, w=["posa"])
                        V(lambda e: e.tensor_single_scalar(posb[:], pos[:], 15, ALU.bitwise_and), r=poskeys, w=["posb"])
                        V(lambda e: e.tensor_copy(posaf[:], posa[:]), r=["posa"], w=["posaf"])
                        V(lambda e: e.tensor_copy(posbf[:], posb[:]), r=["posb"], w=["posbf"])
                    add(f_pos)
                    io16 = iota_f[:, 0:16]
                    for (pf, pk_, cc, dst, dk) in ((posaf, "posaf", 0, ihi, "ihi"), (posbf, "posbf", 1, jlo, "jlo")):
                        for h in range(8):
                            def f_oh(h=h, pf=pf, pk_=pk_, cc=cc):
                                V(lambda e: e.tensor_tensor(oh[:, h, :, :], pf[:, h, :].unsqueeze(2).to_broadcast([128, 16, 16]),
                                                            io16.unsqueeze(1).to_broadcast([128, 16, 16]), ALU.is_equal),
                                  r=[pk_, "iota_f"], w=["oh%d" % h])
                                G(lambda e: e.tensor_tensor(oh[:, h, :, :], oh[:, h, :, :],
                                                            i1v[:, h, cc, :].unsqueeze(1).to_broadcast([128, 16, 16]), ALU.mult),
                                  r=["oh%d" % h, "i1f"], w=["oh%d" % h])
                            add(f_oh)
                        ohkeys = ["oh%d" % h for h in range(8)]
                        add(lambda dst=dst, dk=dk, ohkeys=ohkeys: V(
                            lambda e: e.tensor_reduce(dst[:], oh[:].rearrange("p h k a -> p (h k) a"), AX.X, ALU.add), r=ohkeys, w=[dk]))
                    if debug and blk == 0 and s == 0:
                        def f_dbg():
                            dump("scs", scs[:], "scs0", [128, 16, 128])
                            dump("gw", gw[:], "gw", [128, 128])
                            dump("ihi", ihi[:], "ihi", [128, 128])
                            dump("jlo", jlo[:], "jlo", [128, 128])
                        add(f_dbg)
                    for (src, sk_, dstT, dk) in ((gw, "gw", gT, "gT"), (ihi, "ihi", ihT, "ihT"), (jlo, "jlo", jlT, "jlT")):
                        def f_t(src=src, sk_=sk_, dstT=dstT, dk=dk, tsl=tsl):
                            PE(lambda e: e.transpose(P[6][:, 0:128], src[:], identf[:]), r=[sk_, "identf"], w=["P6"])
                            A(lambda e: e.copy(dstT[:, tsl], P[6][:, 0:128]), r=["P6"], w=[dk])
                        add(f_t)
                return st

            def wsel_build(blk):
                for sbk in range(TB // TS):
                    o = sbk % 2
                    ts0 = sbk * TS
                    io3 = iota_b[:].unsqueeze(1).to_broadcast([128, TS, 128])
                    V(lambda e, o=o, ts0=ts0, io3=io3: e.tensor_tensor(OA[o][:], io3, jlT[:, ts0:ts0 + TS].unsqueeze(2).to_broadcast([128, TS, 128]),
                                                                       ALU.is_equal),
                      r=["iota_b", "jlT"], w=["OA%d" % o])
                    V(lambda e, o=o, ts0=ts0, io3=io3: e.tensor_tensor(OBw[o][:], io3, ihT[:, ts0:ts0 + TS].unsqueeze(2).to_broadcast([128, TS, 128]),
                                                                       ALU.is_equal),
                      r=["iota_b", "ihT"], w=["OBw%d" % o])
                    V(lambda e, o=o, ts0=ts0: e.tensor_tensor(OBw[o][:], OBw[o][:], gT[:, ts0:ts0 + TS].unsqueeze(2).to_broadcast([128, TS, 128]),
                                                              ALU.mult),
                      r=["OBw%d" % o, "gT"], w=["OBw%d" % o])
                    for tl in range(TS):
                        pbk = 4 + (tl // 4) % 2
                        PE(lambda e, o=o, tl=tl, pbk=pbk: e.matmul(P[pbk][:, (tl % 4) * 128:(tl % 4 + 1) * 128], OA[o][:, tl, :], OBw[o][:, tl, :],
                                                                   start=True, stop=True),
                           r=["OA%d" % o, "OBw%d" % o], w=[PK[pbk]], inc=(tl % 4 == 3))
                        if tl % 4 == 3:
                            tb0 = ts0 + tl - 3
                            A(lambda e, pbk=pbk, tb0=tb0: e.copy(Wsel[:, tb0:tb0 + 4, :].rearrange("p t i -> p (t i)"), P[pbk][:]),
                              r=[PK[pbk]], w=["Wsel"])
                if debug and blk == 0:
                    dump("Wsel", Wsel[:, 0:8, :], "Wsel", [128, 8, 128], BF16)
                    dump("h2T", h2T[0][:], "h2T0", [128, 8, TB], BF16)

            def expert_loop(blk, stages):
                hb = blk % 2
                hT_ = h2T[hb]
                hk = "h2T%d" % hb
                nst = len(stages)
                per = (nst + 111) // 112 if nst else 0
                sp = [0]

                def run_stages(n):
                    while n > 0 and sp[0] < nst:
                        stages[sp[0]]()
                        sp[0] += 1
                        n -= 1

                def Vm(i):
                    o = (i // GI) % 2
                    il = i % GI
                    a_ = i % 2
                    for s in range(NSUB):
                        for half in range(2):
                            pbk = s * 2 + half
                            PE(lambda e, a_=a_, s=s, half=half, pbk=pbk, o=o, il=il, i=i: e.matmul(
                                P[pbk][:], wf[a_][:, s * 128:(s + 1) * 128], vbuf[o][:, il, half * 512:(half + 1) * 512],
                                start=(i == 0), stop=(i == 127)),
                               r=["wf%d" % a_, "vbuf%d" % o], w=[PK[pbk]], inc=(s == NSUB - 1 and half == 1))

                for i in range(128):
                    ig = i // GI
                    il = i % GI
                    o = ig % 2
                    a_ = i % 2
                    pa_ = 4 + a_
                    if il == 0:
                        S.dma("sync", ub[o][:], uTg_d[ig], reads=["uTg"], writes=["ub%d" % o], dsem=12 + o)
                        S.dma("sync", vbuf[o][:], vg_d[ig], reads=["vg"], writes=["vbuf%d" % o], dsem=14 + o)
                    for k in range(8):
                        PE(lambda e, o=o, il=il, k=k, pa_=pa_: e.matmul(P[pa_][:, 0:TB], ub[o][:, k, il * 128:(il + 1) * 128], hT_[:, k, :],
                                                                        start=(k == 0), stop=(k == 7)),
                           r=["ub%d" % o, hk], w=[PK[pa_]], inc=(k == 7))
                    A(lambda e, a_=a_, pa_=pa_: e.activation(actg[a_][:], P[pa_][:, 0:TB], AF.Gelu), r=[PK[pa_]], w=["actg%d" % a_])
                    G(lambda e, a_=a_, i=i: e.tensor_tensor(wf[a_][:], actg[a_][:], Wsel[:, :, i], ALU.mult),
                      r=["actg%d" % a_, "Wsel"], w=["wf%d" % a_])
                    if i >= 1:
                        Vm(i - 1)
                    if i >= 4:
                        run_stages(per)
                Vm(127)
                run_stages(nst)

            def final_evac(blk):
                hb = blk % 2
                t0 = blk * TB
                xb = x1b[hb]
                xbk = "x1b%d" % hb
                for s in range(NSUB):
                    for half in range(2):
                        pbk = s * 2 + half
                        hs = slice(half * 512, (half + 1) * 512)
                        V(lambda e, pbk=pbk, hs=hs: e.tensor_tensor(pt1[:, hs], P[pbk][:], gt2r[:, hs], ALU.mult), r=[PK[pbk], "gtr1"], w=["pt1"])
                    if debug and blk == 0 and s == 0:
                        dump("peer", pt1[:], "pt1", [128, D])
                    G(lambda e, s=s: e.tensor_tensor(pt1[:], pt1[:], xb[:, s, :], ALU.add), r=["pt1", xbk], w=["pt1"])
                    V(lambda e: e.scalar_tensor_tensor(junk2[:], pt1[:], 1.0, pt1[:], ALU.mult, ALU.mult, accum_out=ssb[:]),
                      r=["pt1"], w=["junk2", "ssb"])
                    V(lambda e: e.tensor_scalar(ssb2[:], ssb[:], 1.0 / D, EPS, ALU.mult, ALU.add), r=["ssb"], w=["ssb2"])
                    G(lambda e: e.tensor_tensor(rstb[:], ssb2[:], mhalf[:], ALU.pow), r=["ssb2", "mhalf"], w=["rstb"])
                    V(lambda e: e.scalar_tensor_tensor(pt1[:], pt1[:], rstb[:, 0:1], gfr[:], ALU.mult, ALU.mult),
                      r=["pt1", "rstb", "gfr"], w=["pt1"])
                    S.dma("sync", out_d[t0 + s * 128:t0 + (s + 1) * 128, :], pt1[:], reads=["pt1"], writes=["out"], dsem=16)

            if INTERLEAVE:
                for f in routing_stages(0):
                    f()
                for blk in range(NBLK):
                    wsel_build(blk)
                    nxt = routing_stages(blk + 1) if blk + 1 < NBLK else []
                    expert_loop(blk, nxt)
                    final_evac(blk)
            else:
                for blk in range(NBLK):
                    for f in routing_stages(blk):
                        f()
                    wsel_build(blk)
                    expert_loop(blk, [])
                    final_evac(blk)
            S.wait_all("sync", ["out"] + list(dbg_outs.keys()))
            S.flush()
    return nc, S.n_ins


def prep_shared(inp):
    f = np.float32
    sh = {}
    sh["w_ada"] = np.ascontiguousarray(inp["w_ada"][0], f)
    sh["b_ada_c"] = np.ascontiguousarray(inp["b_ada"][0].reshape(48, 128).T, f)
    sh["b_ada_r"] = np.ascontiguousarray(inp["b_ada"][0].reshape(1, 6 * D), f)
    sh["g_mix_c"] = np.ascontiguousarray(inp["g_mix"][0].reshape(8, 128).T, f)
    sh["g_ffn_c"] = np.ascontiguousarray(inp["g_ffn"][0].reshape(8, 128).T, f)
    sh["w_in"] = np.ascontiguousarray(inp["w_in"][0], f)
    sh["w_out"] = np.ascontiguousarray(inp["w_out"][0], f)
    sh["w_q"] = np.ascontiguousarray(inp["w_q"][0], f)
    sh["w_glu"] = np.ascontiguousarray(inp["w_glu"][0], f)
    sh["ln_g_r"] = np.ascontiguousarray(inp["sgu_ln_g"][0].reshape(1, 512), f)
    sh["ln_b_r"] = np.ascontiguousarray(inp["sgu_ln_b"][0].reshape(1, 512), f)
    sh["b_glu_c"] = np.ascontiguousarray(inp["b_glu"][0].reshape(4, 128).T, f)
    sh["w_sT"] = np.ascontiguousarray(np.transpose(inp["w_s"][0], (2, 0, 1)), f)
    sh["b_s_c"] = np.ascontiguousarray(inp["b_s"][0].T, f)
    a_re = np.asarray(inp["ssm_a_re"][0], f)
    a_im = np.asarray(inp["ssm_a_im"][0], f)
    ldt = np.repeat(np.asarray(inp["ssm_log_dt"][0], f)[:, None], 64, axis=1)
    sh["a_re_r"] = np.ascontiguousarray(a_re.reshape(1, 2048))
    sh["a_im_r"] = np.ascontiguousarray(a_im.reshape(1, 2048))
    sh["ldt_r"] = np.ascontiguousarray(ldt.reshape(1, 2048))

    def cols(a):
        return np.ascontiguousarray(a.reshape(16, 2, 64).transpose(1, 2, 0).reshape(128, 16))

    sh["a_re_c"] = cols(a_re)
    sh["a_im_c"] = cols(a_im)
    sh["ldt_c"] = cols(ldt)
    b_re = np.asarray(inp["ssm_b_re"][0], f)
    b_im = np.asarray(inp["ssm_b_im"][0], f)
    BR = np.zeros((8, 16, 4, 8, 64), f)
    BI = np.zeros((8, 16, 4, 8, 64), f)
    for c in range(4):
        for gl in range(8):
            BR[gl, :, c, gl, :] = b_re[8 * c + gl].T
            BI[gl, :, c, gl, :] = b_im[8 * c + gl].T
    sh["BR"] = BR.reshape(128, 4, 512)
    sh["BI"] = BI.reshape(128, 4, 512)
    c_re = np.asarray(inp["ssm_c_re"][0], f)
    c_im = np.asarray(inp["ssm_c_im"][0], f)
    CR = np.zeros((2, 64, 16, 2, 16), f)
    CI = np.zeros((2, 64, 16, 2, 16), f)
    for q in range(16):
        for gg in range(2):
            CR[gg, :, q, gg, :] = c_re[2 * q + gg].T
            CI[gg, :, q, gg, :] = c_im[2 * q + gg].T
    sh["CR"] = CR.reshape(128, 16, 32)
    sh["CI"] = CI.reshape(128, 16, 32)
    dsk = np.asarray(inp["ssm_d"][0], f).reshape(4, 128)
    Dd = np.zeros((128, 4, 128), f)
    for c in range(4):
        Dd[np.arange(128), c, np.arange(128)] = dsk[c]
    sh["Dd"] = Dd
    keys = np.asarray(inp["peer_keys"][0], f)
    kb = np.zeros((2, 64, 8, 2, 128), f)
    for h in range(8):
        for c in range(2):
            kb[c, :, h, c, :] = keys[h, c].T
    sh["keysblk"] = kb.reshape(128, 8, 256)
    sh["uT"] = np.ascontiguousarray(np.asarray(inp["peer_u"][0], f).T)
    sh["v"] = np.ascontiguousarray(inp["peer_v"][0], f)
    sh["g_final_r"] = np.ascontiguousarray(np.asarray(inp["g_final"], f).reshape(1, D))
    return sh


def make_in_maps(inp, T):
    sh = prep_shared(inp)
    maps = []
    for b in range(NCORES):
        m = dict(sh)
        m["x"] = np.ascontiguousarray(inp["x"][b, :T], np.float32)
        m["cvec"] = np.ascontiguousarray(np.asarray(inp["c"][b], np.float32).reshape(8, 128).T)
        maps.append(m)
    return maps


_CACHE = {}


def kernel(**inputs):
    inputs = {k: np.asarray(v) for k, v in inputs.items()}
    T = inputs["x"].shape[1]
    if T not in _CACHE:
        _CACHE[T] = build_program(T)[0]
    nc = _CACHE[T]
    maps = make_in_maps(inputs, T)
    res = run_bass_kernel_spmd(nc, maps, core_ids=list(range(NCORES)))
    out = np.stack([np.asarray(res.results[b]["out"], np.float32) for b in range(NCORES)], axis=0)
    return out
```

```python
import math
from contextlib import ExitStack

import numpy as np
import concourse.bass as bass
import concourse.mybir as mybir
from concourse.bass_utils import run_bass_kernel_spmd

F32 = mybir.dt.float32
BF16 = mybir.dt.bfloat16
U32 = mybir.dt.uint32
I32 = mybir.dt.int32
AF = mybir.ActivationFunctionType
ALU = mybir.AluOpType
AX = mybir.AxisListType

D = 1024
NCORES = 8
SEQ = 8192
EPS = 1e-6
PI = math.pi
TWO_PI = 2.0 * math.pi
ENGS = ("sync", "scalar", "vector", "gpsimd", "tensor")


class _DSem:
    def __init__(self, sem, name):
        self.sem = sem
        self.name = name
        self.issued = 0


class Sched:
    def __init__(self, nc, es, n_dsem=40):
        self.nc = nc
        self.sem = {e: es.enter_context(nc.semaphore("s_" + e)) for e in ENGS}
        self.cnt = {e: 0 for e in ENGS}
        self.ops = {e: [] for e in ENGS}
        self.waited = {e: {} for e in ENGS}
        self.dsems = [_DSem(es.enter_context(nc.semaphore("d%d" % i)), "d%d" % i) for i in range(n_dsem)]
        self.last_w = {}
        self.readers = {}
        self.pending = {e: ([], []) for e in ENGS}
        self.semobj = {e: self.sem[e] for e in ENGS}
        for d in self.dsems:
            self.semobj[d.name] = d.sem
        self.n_ins = 0

    def _need(self, eng, reads, writes, skip_self=False):
        need = {}

        def add(src, val):
            if skip_self and src == eng:
                return
            if need.get(src, 0) < val:
                need[src] = val

        for k in reads:
            w = self.last_w.get(k)
            if w:
                add(*w)
        for k in writes:
            w = self.last_w.get(k)
            if w:
                add(*w)
            for r in self.readers.get(k, {}).items():
                add(*r)
        out = []
        for src, val in need.items():
            if self.waited[eng].get(src, 0) >= val:
                continue
            self.waited[eng][src] = val
            out.append((src, val))
        return out

    def _emit_waits(self, eng, waits):
        for src, val in waits:
            so = self.semobj[src]
            self.ops[eng].append(lambda e, so=so, val=val: e.wait_ge(so, val))

    def _commit(self, tag, reads, writes):
        for k in reads:
            rd = self.readers.setdefault(k, {})
            if rd.get(tag[0], 0) < tag[1]:
                rd[tag[0]] = tag[1]
        for k in writes:
            self.last_w[k] = tag
            self.readers[k] = {}

    def op(self, eng, fn, reads=(), writes=(), inc=True):
        self.n_ins += 1
        waits = self._need(eng, reads, writes, skip_self=(eng == "tensor"))
        self._emit_waits(eng, waits)
        pr, pw = self.pending[eng]
        pr.extend(reads)
        pw.extend(writes)
        if inc:
            self.cnt[eng] += 1
            so = self.sem[eng]
            self.ops[eng].append(lambda e, fn=fn, so=so: fn(e).then_inc(so, 1))
            self._commit((eng, self.cnt[eng]), pr, pw)
            self.pending[eng] = ([], [])
        else:
            self.ops[eng].append(lambda e, fn=fn: fn(e))

    def dma(self, eng, out, in_, reads=(), writes=(), dsem=0, **kw):
        self.n_ins += 1
        d = self.dsems[dsem]
        waits = self._need(eng, reads, writes)
        if d.issued and self.waited[eng].get(d.name, 0) < 16 * d.issued:
            self.waited[eng][d.name] = 16 * d.issued
            waits.append((d.name, 16 * d.issued))
        self._emit_waits(eng, waits)
        d.issued += 1
        so = d.sem
        self.ops[eng].append(
            lambda e, so=so, out=out, in_=in_, kw=kw: e.dma_start(out=out, in_=in_, **kw).then_inc(so, 16))
        self._commit((d.name, 16 * d.issued), reads, writes)

    def wait_all(self, eng, keys):
        self._emit_waits(eng, self._need(eng, keys, ()))

    def barrier(self):
        for e in ENGS:
            assert not self.pending[e][0] and not self.pending[e][1], e
        for e in ENGS:
            waits = []
            for src in ENGS:
                if src == e and e == "tensor":
                    continue
                val = self.cnt[src]
                if val and self.waited[e].get(src, 0) < val:
                    self.waited[e][src] = val
                    waits.append((src, val))
            for d in self.dsems:
                val = 16 * d.issued
                if val and self.waited[e].get(d.name, 0) < val:
                    self.waited[e][d.name] = val
                    waits.append((d.name, val))
            self._emit_waits(e, waits)

    def replay(self):
        with self.nc.Block() as block:
            for name in ENGS:
                ops = self.ops[name]

                def body(e, ops=ops):
                    for f in ops:
                        f(e)

                getattr(block, name)(body)
        self.ops = {e: [] for e in ENGS}

    def flush(self):
        self.barrier()
        self.replay()


INTERLEAVE = True


def build_program(T=SEQ, debug=False, TB=256, GI=4):
    assert T % TB == 0 and TB % 128 == 0
    NCH = T // 128
    NBLK = T // TB
    NSUB = TB // 128
    nc = bass.Bass("TRN2", target_bir_lowering=False)

    def din(name, shape, dt=F32):
        return nc.dram_tensor(name, list(shape), dt, kind="ExternalInput").ap()

    x_d = din("x", [T, D])
    cvec_d = din("cvec", [128, 8])
    wada_d = din("w_ada", [D, 6 * D])
    badac_d = din("b_ada_c", [128, 48])
    badar_d = din("b_ada_r", [1, 6 * D])
    gmix_d = din("g_mix_c", [128, 8])
    gffn_d = din("g_ffn_c", [128, 8])
    win_d = din("w_in", [D, 1536])
    wout_d = din("w_out", [D, D])
    wq_d = din("w_q", [D, D])
    wglu_d = din("w_glu", [512, 512])
    lng_d = din("ln_g_r", [1, 512])
    lnb_d = din("ln_b_r", [1, 512])
    bgluc_d = din("b_glu_c", [128, 4])
    wsT_d = din("w_sT", [128, 4, 128])
    bsc_d = din("b_s_c", [128, 4])
    are_r_d = din("a_re_r", [1, 2048])
    aim_r_d = din("a_im_r", [1, 2048])
    ldt_r_d = din("ldt_r", [1, 2048])
    are_c_d = din("a_re_c", [128, 16])
    aim_c_d = din("a_im_c", [128, 16])
    ldt_c_d = din("ldt_c", [128, 16])
    BR_d = din("BR", [128, 4, 512])
    BI_d = din("BI", [128, 4, 512])
    CR_d = din("CR", [128, 16, 32])
    CI_d = din("CI", [128, 16, 32])
    Dd_d = din("Dd", [128, 4, 128])
    keys_d = din("keysblk", [128, 8, 256])
    uT_d = din("uT", [D, 16384])
    v_d = din("v", [16384, D])
    gfin_d = din("g_final_r", [1, D])
    out_d = nc.dram_tensor("out", [T, D], F32, kind="ExternalOutput").ap()

    NG = 128 // GI
    uTg_d = nc.dram_tensor("uTgs", [NG, 128, 8, GI * 128], BF16, kind="Internal").ap()
    vg_d = nc.dram_tensor("vgs", [NG, 128, GI, D], BF16, kind="Internal").ap()
    x1s_d = nc.dram_tensor("x1s", [T, D], F32, kind="Internal").ap()

    dbg_outs = {}

    with ExitStack() as es:
        S = Sched(nc, es)

        def V(fn, r=(), w=(), inc=True):
            S.op("vector", fn, r, w, inc)

        def G(fn, r=(), w=(), inc=True):
            S.op("gpsimd", fn, r, w, inc)

        def A(fn, r=(), w=(), inc=True):
            S.op("scalar", fn, r, w, inc)

        def PE(fn, r=(), w=(), inc=True):
            S.op("tensor", fn, r, w, inc)

        dsn = [8]

        def next_ds():
            dsn[0] += 1
            if dsn[0] >= 40:
                dsn[0] = 8
            return dsn[0]

        def dump(name, ap, key, shape, dt=F32):
            if not debug:
                return
            o = nc.dram_tensor("dbg_" + name, list(shape), dt, kind="ExternalOutput").ap()
            dbg_outs["dbg_" + name] = o
            S.dma("sync", o, ap, reads=[key], writes=["dbg_" + name], dsem=next_ds())

        def sb(stack, name, shape, dt):
            return stack.enter_context(nc.sbuf_tensor(name, list(shape), dt))

        def ps(stack, name, shape, dt):
            return stack.enter_context(nc.psum_tensor(name, list(shape), dt))

        P = [ps(es, "P%d" % i, [128, 512], F32) for i in range(7)]
        PT = ps(es, "PT", [128, 1024], BF16)
        PK = ["P%d" % i for i in range(7)]

        identb = sb(es, "identb", [128, 128], BF16)
        identf = sb(es, "identf", [128, 128], F32)
        trib = sb(es, "trib", [128, 128], BF16)
        iota_f = sb(es, "iota_f", [128, 128], F32)
        iota_b = sb(es, "iota_b", [128, 128], BF16)
        jcol = sb(es, "jcol", [128, 1], F32)
        njcol = sb(es, "njcol", [128, 1], F32)
        mhalf = sb(es, "mhalf", [128, 1], F32)
        a1c = sb(es, "a1c", [128, 8], F32)
        sh1c = sb(es, "sh1c", [128, 8], F32)
        a2c = sb(es, "a2c", [128, 8], F32)
        sh2c = sb(es, "sh2c", [128, 8], F32)
        gt1r = sb(es, "gt1r", [128, D], F32)
        gt2r = sb(es, "gt2r", [128, D], F32)
        gfr = sb(es, "gfr", [128, D], F32)

        G(lambda e: e.iota(iota_f[:], [[1, 128]], base=0, channel_multiplier=0, allow_small_or_imprecise_dtypes=True), w=["iota_f"])
        G(lambda e: e.iota(jcol[:], [[0, 1]], base=0, channel_multiplier=1, allow_small_or_imprecise_dtypes=True), w=["jcol"])
        V(lambda e: e.tensor_copy(iota_b[:], iota_f[:]), r=["iota_f"], w=["iota_b"])
        V(lambda e: e.tensor_scalar(njcol[:], jcol[:], -1.0, None, ALU.mult), r=["jcol"], w=["njcol"])
        V(lambda e: e.memset(mhalf[:], -0.5), w=["mhalf"])
        V(lambda e: e.tensor_scalar(identf[:], iota_f[:], jcol[:, 0:1], None, ALU.is_equal), r=["iota_f", "jcol"], w=["identf"])
        V(lambda e: e.tensor_copy(identb[:], identf[:]), r=["identf"], w=["identb"])
        V(lambda e: e.tensor_scalar(trib[:], iota_f[:], jcol[:, 0:1], None, ALU.is_ge), r=["iota_f", "jcol"], w=["trib"])
        S.dma("sync", gfr[:], gfin_d.partition_broadcast(128), writes=["gfr"], dsem=0)

        uT_v = uT_d.rearrange("(k p) e -> p k e", p=128)
        for g in range(NG):
            S.dma("gpsimd", uTg_d[g], uT_v[:, :, g * GI * 128:(g + 1) * GI * 128], writes=["uTg"], dsem=1)
        for g in range(NG):
            S.dma("gpsimd", vg_d[g], v_d[g * GI * 128:(g + 1) * GI * 128, :].rearrange("(il j) d -> j il d", j=128), writes=["vg"], dsem=2)

        with ExitStack() as sa:
            cv = sb(sa, "cv", [128, 8], F32)
            cond = sb(sa, "cond", [128, 8], F32)
            condrep = sb(sa, "condrep", [128, 8, 128], F32)
            wp = [sb(sa, "wadap%d" % i, [128, 8, D], F32) for i in range(2)]
            badac = sb(sa, "badac", [128, 48], F32)
            badar = sb(sa, "badar", [128, 2, D], F32)
            modc = sb(sa, "modc", [128, 48], F32)
            gmixc = sb(sa, "gmixc", [128, 8], F32)
            gffnc = sb(sa, "gffnc", [128, 8], F32)
            S.dma("sync", cv[:], cvec_d, writes=["cv"], dsem=3)
            S.dma("sync", badac[:], badac_d, writes=["badac"], dsem=4)
            S.dma("sync", badar[:, 0, :], badar_d[:, 2 * D:3 * D].partition_broadcast(128), writes=["badar0"], dsem=5)
            S.dma("sync", badar[:, 1, :], badar_d[:, 5 * D:6 * D].partition_broadcast(128), writes=["badar1"], dsem=6)
            S.dma("sync", gmixc[:], gmix_d, writes=["gmixc"], dsem=7)
            S.dma("sync", gffnc[:], gffn_d, writes=["gffnc"], dsem=3)
            A(lambda e: e.activation(cond[:], cv[:], AF.Silu), r=["cv"], w=["cond"])
            V(lambda e: e.tensor_copy(condrep[:], cond[:].unsqueeze(2).to_broadcast([128, 8, 128])), r=["cond"], w=["condrep"])
            wada_v = wada_d.rearrange("(k p) n -> p k n", p=128)
            for piece in range(6):
                sl = piece % 2
                S.dma("sync", wp[sl][:], wada_v[:, :, piece * D:(piece + 1) * D], writes=["wp%d" % sl], dsem=4 + sl)
                if piece in (2, 5):
                    ri = 0 if piece == 2 else 1
                    dst = gt1r if piece == 2 else gt2r
                    for half in range(2):
                        for k in range(8):
                            PE(lambda e, k=k, half=half, sl=sl: e.matmul(
                                P[half][:], condrep[:, k, :], wp[sl][:, k, half * 512:(half + 1) * 512],
                                start=(k == 0), stop=(k == 7)),
                               r=["condrep", "wp%d" % sl], w=[PK[half]], inc=(k == 7))
                        V(lambda e, half=half, ri=ri, dst=dst: e.tensor_tensor(
                            dst[:, half * 512:(half + 1) * 512], P[half][:], badar[:, ri, half * 512:(half + 1) * 512], ALU.add),
                          r=[PK[half], "badar%d" % ri], w=["gtr%d" % ri])
                else:
                    for nl in range(8):
                        col = piece * 8 + nl
                        for k in range(8):
                            PE(lambda e, k=k, nl=nl, sl=sl, col=col: e.matmul(
                                P[2][:, col:col + 1], wp[sl][:, k, nl * 128:(nl + 1) * 128], cond[:, k:k + 1],
                                start=(k == 0), stop=(k == 7)),
                               r=["cond", "wp%d" % sl], w=["P2"], inc=(k == 7 and nl == 7))
            V(lambda e: e.memset(modc[:], 0.0), w=["modc"])
            V(lambda e: e.tensor_tensor(modc[:, 0:16], P[2][:, 0:16], badac[:, 0:16], ALU.add), r=["P2", "badac"], w=["modc"])
            V(lambda e: e.tensor_tensor(modc[:, 24:40], P[2][:, 24:40], badac[:, 24:40], ALU.add), r=["P2", "badac"], w=["modc"])
            V(lambda e: e.tensor_copy(sh1c[:], modc[:, 0:8]), r=["modc"], w=["sh1c"])
            V(lambda e: e.tensor_copy(sh2c[:], modc[:, 24:32]), r=["modc"], w=["sh2c"])
            V(lambda e: e.scalar_tensor_tensor(a1c[:], modc[:, 8:16], 1.0, gmixc[:], ALU.add, ALU.mult), r=["modc", "gmixc"], w=["a1c"])
            V(lambda e: e.scalar_tensor_tensor(a2c[:], modc[:, 32:40], 1.0, gffnc[:], ALU.add, ALU.mult), r=["modc", "gffnc"], w=["a2c"])
            dump("a1c", a1c[:], "a1c", [128, 8])
            dump("sh1c", sh1c[:], "sh1c", [128, 8])
            dump("a2c", a2c[:], "a2c", [128, 8])
            dump("gt1r", gt1r[:], "gtr0", [128, D])
            dump("gt2r", gt2r[:], "gtr1", [128, D])
            S.flush()

        with ExitStack() as pa:
            win = sb(pa, "win", [128, 8, 1536], BF16)
            wout = sb(pa, "wout", [128, 8, D], BF16)
            wglu = sb(pa, "wglu", [128, 4, 512], BF16)
            bblk = sb(pa, "bblk", [128, 4, 1024], BF16)
            crb = sb(pa, "crb", [128, 16, 32], BF16)
            cinb = sb(pa, "cinb", [128, 16, 32], BF16)
            ncrb = sb(pa, "ncrb", [128, 16, 32], BF16)
            ntrib = sb(pa, "ntrib", [128, 128], BF16)
            ddb = sb(pa, "ddb", [128, 4, 128], BF16)
            wmT = sb(pa, "wmT", [128, 4, 128], BF16)
            bsc = sb(pa, "bsc", [128, 4], F32)
            bgluc = sb(pa, "bgluc", [128, 4], F32)
            lngr = sb(pa, "lngr", [128, 512], F32)
            lnbr = sb(pa, "lnbr", [128, 512], F32)
            Mre = sb(pa, "Mre", [128, 2048], F32)
            Mim = sb(pa, "Mim", [128, 2048], F32)
            Pre = sb(pa, "Pre", [128, 16, 128], F32)
            Pim = sb(pa, "Pim", [128, 16, 128], F32)
            A128re = sb(pa, "A128re", [128, 16], F32)
            A128im = sb(pa, "A128im", [128, 16], F32)
            Kre = sb(pa, "Kre", [128, 16], F32)
            Kim = sb(pa, "Kim", [128, 16], F32)

            S.dma("gpsimd", win[:], win_d.rearrange("(k p) n -> p k n", p=128), writes=["win"], dsem=20)
            S.dma("gpsimd", wglu[:], wglu_d.rearrange("(k p) n -> p k n", p=128), writes=["wglu"], dsem=20)
            S.dma("gpsimd", crb[:], CR_d, writes=["crb"], dsem=20)
            S.dma("gpsimd", ddb[:], Dd_d, writes=["ddb"], dsem=20)
            S.dma("sync", bsc[:], bsc_d, writes=["bsc"], dsem=4)
            S.dma("sync", bgluc[:], bgluc_d, writes=["bgluc"], dsem=4)
            S.dma("sync", lngr[:], lng_d.partition_broadcast(128), writes=["lngr"], dsem=5)
            S.dma("sync", lnbr[:], lnb_d.partition_broadcast(128), writes=["lnbr"], dsem=5)
            V(lambda e: e.memset(Kre[:], 0.0), w=["Kre"])
            V(lambda e: e.memset(Kim[:], 0.0), w=["Kim"])

            with ExitStack() as s1_:
                woutf = sb(s1_, "woutf", [128, 8, D], F32)
                S.dma("sync", woutf[:], wout_d.rearrange("(k p) n -> p k n", p=128), writes=["woutf"], dsem=6)
                for k in range(8):
                    V(lambda e, k=k: e.tensor_tensor(wout[:, k, :], woutf[:, k, :], gt1r[:], ALU.mult),
                      r=["woutf", "gtr0"], w=["wout"])
                wsTf = sb(s1_, "wsTf", [128, 4, 128], F32)
                S.dma("sync", wsTf[:], wsT_d, writes=["wsTf"], dsem=7)
                V(lambda e: e.tensor_tensor(wmT[:], wsTf[:], trib[:].unsqueeze(1).to_broadcast([128, 4, 128]), ALU.mult),
                  r=["wsTf", "trib"], w=["wmT"])
                cif = sb(s1_, "cif", [128, 16, 32], F32)
                S.dma("sync", cif[:], CI_d, writes=["cif"], dsem=7)
                V(lambda e: e.tensor_scalar(cinb[:], cif[:], -1.0, None, ALU.mult), r=["cif"], w=["cinb"])
                V(lambda e: e.tensor_scalar(ncrb[:], crb[:], -1.0, None, ALU.mult), r=["crb"], w=["ncrb"])
                V(lambda e: e.tensor_scalar(ntrib[:], trib[:], -1.0, None, ALU.mult), r=["trib"], w=["ntrib"])
                S.flush()

            with ExitStack() as ss_:
                W_ = 512

                def tmp(name, dt=F32):
                    return sb(ss_, name, [128, W_], dt)

                def sincos(ang, ak, osin, ks, ocos, kc, ti, kti, ap=lambda t: t[:]):
                    V(lambda e: e.tensor_scalar(ap(ocos), ap(ang), 1.0 / TWO_PI, None, ALU.mult), r=[ak], w=[kc])
                    V(lambda e: e.tensor_copy(ap(ti), ap(ocos)), r=[kc], w=[kti])
                    V(lambda e: e.tensor_copy(ap(ocos), ap(ti)), r=[kti], w=[kc])
                    V(lambda e: e.scalar_tensor_tensor(ap(ang), ap(ocos), -TWO_PI, ap(ang), ALU.mult, ALU.add), r=[kc, ak], w=[ak])
                    V(lambda e: e.tensor_scalar(ap(ang), ap(ang), -PI, PI, ALU.max, ALU.min), r=[ak], w=[ak])
                    A(lambda e: e.activation(ap(osin), ap(ang), AF.Sin), r=[ak], w=[ks])
                    V(lambda e: e.tensor_scalar(ap(ocos), ap(ang), PI / 2, -TWO_PI, ALU.is_gt, ALU.mult), r=[ak], w=[kc])
                    V(lambda e: e.scalar_tensor_tensor(ap(ocos), ap(ang), PI / 2, ap(ocos), ALU.add, ALU.add), r=[ak, kc], w=[kc])
                    V(lambda e: e.tensor_scalar(ap(ocos), ap(ocos), -PI, PI, ALU.max, ALU.min), r=[kc], w=[kc])
                    A(lambda e: e.activation(ap(ocos), ap(ocos), AF.Sin), r=[kc], w=[kc])

                ebase = tmp("ebase")
                V(lambda e: e.memset(ebase[:], math.e), w=["ebase"])
                are = tmp("are"); aim = tmp("aim"); dlt = tmp("dlt"); lr = tmp("lr"); th = tmp("th")
                ang = tmp("ang"); sn = tmp("sn"); cs_ = tmp("cs_"); ti = tmp("ti", I32); mg = tmp("mg")
                den = tmp("den"); t2 = tmp("t2"); cre = tmp("cre"); cim = tmp("cim")
                brf = tmp("brf"); bif = tmp("bif"); tb1 = tmp("tb1"); tb2 = tmp("tb2")
                for c in range(4):
                    csl = slice(c * W_, (c + 1) * W_)
                    S.dma("sync", are[:], are_r_d[:, csl].partition_broadcast(128), writes=["are"], dsem=3)
                    S.dma("sync", aim[:], aim_r_d[:, csl].partition_broadcast(128), writes=["aim"], dsem=4)
                    S.dma("sync", dlt[:], ldt_r_d[:, csl].partition_broadcast(128), writes=["dlt"], dsem=5)
                    S.dma("sync", brf[:], BR_d[:, c, :], writes=["brf"], dsem=6)
                    S.dma("sync", bif[:], BI_d[:, c, :], writes=["bif"], dsem=7)
                    G(lambda e: e.tensor_tensor(dlt[:], ebase[:], dlt[:], ALU.pow), r=["dlt", "ebase"], w=["dlt"])
                    V(lambda e: e.tensor_scalar(are[:], are[:], -1e-4, None, ALU.min), r=["are"], w=["are"])
                    V(lambda e: e.tensor_tensor(lr[:], are[:], dlt[:], ALU.mult), r=["are", "dlt"], w=["lr"])
                    V(lambda e: e.tensor_tensor(th[:], aim[:], dlt[:], ALU.mult), r=["aim", "dlt"], w=["th"])
                    V(lambda e: e.tensor_copy(ang[:], th[:]), r=["th"], w=["ang"])
                    sincos(ang, "ang", sn, "sn", cs_, "cs_", ti, "ti")
                    A(lambda e: e.activation(mg[:], lr[:], AF.Exp), r=["lr"], w=["mg"])
                    V(lambda e: e.tensor_tensor(cs_[:], mg[:], cs_[:], ALU.mult), r=["mg", "cs_"], w=["cs_"])
                    V(lambda e: e.tensor_scalar(cs_[:], cs_[:], -1.0, None, ALU.add), r=["cs_"], w=["cs_"])
                    V(lambda e: e.tensor_tensor(sn[:], mg[:], sn[:], ALU.mult), r=["mg", "sn"], w=["sn"])
                    V(lambda e: e.tensor_tensor(den[:], are[:], are[:], ALU.mult), r=["are"], w=["den"])
                    V(lambda e: e.tensor_tensor(t2[:], aim[:], aim[:], ALU.mult), r=["aim"], w=["t2"])
                    V(lambda e: e.tensor_tensor(den[:], den[:], t2[:], ALU.add), r=["den", "t2"], w=["den"])
                    V(lambda e: e.reciprocal(den[:], den[:]), r=["den"], w=["den"])
                    V(lambda e: e.tensor_tensor(cre[:], cs_[:], are[:], ALU.mult), r=["cs_", "are"], w=["cre"])
                    V(lambda e: e.tensor_tensor(t2[:], sn[:], aim[:], ALU.mult), r=["sn", "aim"], w=["t2"])
                    V(lambda e: e.tensor_tensor(cre[:], cre[:], t2[:], ALU.add), r=["cre", "t2"], w=["cre"])
                    V(lambda e: e.tensor_tensor(cre[:], cre[:], den[:], ALU.mult), r=["cre", "den"], w=["cre"])
                    V(lambda e: e.tensor_tensor(cim[:], sn[:], are[:], ALU.mult), r=["sn", "are"], w=["cim"])
                    V(lambda e: e.tensor_tensor(t2[:], cs_[:], aim[:], ALU.mult), r=["cs_", "aim"], w=["t2"])
                    V(lambda e: e.tensor_tensor(cim[:], cim[:], t2[:], ALU.subtract), r=["cim", "t2"], w=["cim"])
                    V(lambda e: e.tensor_tensor(cim[:], cim[:], den[:], ALU.mult), r=["cim", "den"], w=["cim"])
                    if c == 0:
                        dump("cre", cre[:], "cre", [128, W_])
                        dump("cim", cim[:], "cim", [128, W_])
                    V(lambda e: e.tensor_tensor(tb1[:], cre[:], brf[:], ALU.mult), r=["cre", "brf"], w=["tb1"])
                    V(lambda e: e.tensor_tensor(tb2[:], cim[:], bif[:], ALU.mult), r=["cim", "bif"], w=["tb2"])
                    V(lambda e, c=c: e.tensor_tensor(bblk[:, c, 0:512], tb1[:], tb2[:], ALU.subtract), r=["tb1", "tb2"], w=["bblk"])
                    V(lambda e: e.tensor_tensor(tb1[:], cre[:], bif[:], ALU.mult), r=["cre", "bif"], w=["tb1"])
                    V(lambda e: e.tensor_tensor(tb2[:], cim[:], brf[:], ALU.mult), r=["cim", "brf"], w=["tb2"])
                    V(lambda e, c=c: e.tensor_tensor(bblk[:, c, 512:1024], tb1[:], tb2[:], ALU.add), r=["tb1", "tb2"], w=["bblk"])
                    V(lambda e: e.tensor_scalar(ang[:], th[:], jcol[:, 0:1], None, ALU.mult), r=["th", "jcol"], w=["ang"])
                    sincos(ang, "ang", sn, "sn", cs_, "cs_", ti, "ti")
                    A(lambda e: e.activation(mg[:], lr[:], AF.Exp, scale=njcol[:, 0:1]), r=["lr", "njcol"], w=["mg"])
                    V(lambda e, csl=csl: e.tensor_tensor(Mre[:, csl], mg[:], cs_[:], ALU.mult), r=["mg", "cs_"], w=["Mre"])
                    V(lambda e, csl=csl: e.scalar_tensor_tensor(Mim[:, csl], mg[:], -1.0, sn[:], ALU.mult, ALU.mult), r=["mg", "sn"], w=["Mim"])
                arc = sb(ss_, "arc", [128, 16], F32)
                aic = sb(ss_, "aic", [128, 16], F32)
                dtc = sb(ss_, "dtc", [128, 16], F32)
                lrc = sb(ss_, "lrc", [128, 16], F32)
                thc = sb(ss_, "thc", [128, 16], F32)
                S.dma("sync", arc[:], are_c_d, writes=["arc"], dsem=3)
                S.dma("sync", aic[:], aim_c_d, writes=["aic"], dsem=4)
                S.dma("sync", dtc[:], ldt_c_d, writes=["dtc"], dsem=5)
                G(lambda e: e.tensor_tensor(dtc[:], ebase[:, 0:16], dtc[:], ALU.pow), r=["dtc", "ebase"], w=["dtc"])
                V(lambda e: e.tensor_scalar(arc[:], arc[:], -1e-4, None, ALU.min), r=["arc"], w=["arc"])
                V(lambda e: e.tensor_tensor(lrc[:], arc[:], dtc[:], ALU.mult), r=["arc", "dtc"], w=["lrc"])
                V(lambda e: e.tensor_tensor(thc[:], aic[:], dtc[:], ALU.mult), r=["aic", "dtc"], w=["thc"])
                v3 = lambda t: t[:].rearrange("p (q i) -> p q i", q=4)
                iot3 = iota_f[:].unsqueeze(1).to_broadcast([128, 4, 128])
                for c in range(4):
                    qs = slice(4 * c, 4 * c + 4)
                    V(lambda e, qs=qs: e.tensor_tensor(v3(ang), thc[:, qs].unsqueeze(2).to_broadcast([128, 4, 128]), iot3, ALU.mult),
                      r=["thc", "iota_f"], w=["ang"])
                    V(lambda e, qs=qs: e.tensor_tensor(v3(t2), lrc[:, qs].unsqueeze(2).to_broadcast([128, 4, 128]), iot3, ALU.mult),
                      r=["lrc", "iota_f"], w=["t2"])
                    sincos(ang, "ang", sn, "sn", cs_, "cs_", ti, "ti")
                    A(lambda e: e.activation(mg[:], t2[:], AF.Exp), r=["t2"], w=["mg"])
                    V(lambda e, qs=qs: e.tensor_tensor(Pre[:, qs, :], v3(mg), v3(cs_), ALU.mult), r=["mg", "cs_"], w=["Pre"])
                    V(lambda e, qs=qs: e.tensor_tensor(Pim[:, qs, :], v3(mg), v3(sn), ALU.mult), r=["mg", "sn"], w=["Pim"])
                a16 = lambda t: t[:, 0:16]
                V(lambda e: e.tensor_scalar(a16(ang), thc[:], 128.0, None, ALU.mult), r=["thc"], w=["ang"])
                sincos(ang, "ang", sn, "sn", cs_, "cs_", ti, "ti", ap=a16)
                A(lambda e: e.activation(a16(mg), lrc[:], AF.Exp, scale=128.0), r=["lrc"], w=["mg"])
                V(lambda e: e.tensor_tensor(A128re[:], a16(mg), a16(cs_), ALU.mult), r=["mg", "cs_"], w=["A128re"])
                V(lambda e: e.tensor_tensor(A128im[:], a16(mg), a16(sn), ALU.mult), r=["mg", "sn"], w=["A128im"])
                dump("Mre", Mre[:], "Mre", [128, 2048])
                dump("Mim", Mim[:], "Mim", [128, 2048])
                dump("Pre", Pre[:], "Pre", [128, 16, 128])
                dump("Pim", Pim[:], "Pim", [128, 16, 128])
                dump("A128re", A128re[:], "A128re", [128, 16])
                S.flush()

            XT = [sb(pa, "xt%d" % i, [128, D], F32) for i in range(2)]
            junk = sb(pa, "junk", [128, D], BF16)
            ssq = sb(pa, "ssq", [128, 1], F32)
            ssq2 = sb(pa, "ssq2", [128, 1], F32)
            rstd = sb(pa, "rstd", [128, 1], F32)
            xn = sb(pa, "xn", [128, D], BF16)
            hT = sb(pa, "hT", [128, 8, 128], BF16)
            ug = sb(pa, "ug", [128, 512], F32)
            vg = sb(pa, "vg", [128, 512], F32)
            ZST = [sb(pa, "zsT%d" % i, [128, 4, 128], BF16) for i in range(2)]
            FMAX = int(nc.vector.BN_STATS_FMAX)
            nst = max(1, 512 // FMAX)
            bst = sb(pa, "bst", [128, nst, int(nc.vector.BN_STATS_DIM)], F32)
            bmv = sb(pa, "bmv", [128, int(nc.vector.BN_AGGR_DIM)], F32)
            var2 = sb(pa, "var2", [128, 1], F32)
            rstdv = sb(pa, "rstdv", [128, 1], F32)
            vn = sb(pa, "vn", [128, 512], F32)
            vn2 = sb(pa, "vn2", [128, 512], F32)
            vb16 = sb(pa, "vb16", [128, 512], BF16)
            ygm = sb(pa, "ygm", [128, 512], BF16)
            YCT = [sb(pa, "ycatT%d" % i, [128, 8, 128], BF16) for i in range(2)]
            bq = sb(pa, "bq", [128, 4, 4, 512], BF16)
            ure = sb(pa, "ure", [128, 4, 128], F32)
            uim = sb(pa, "uim", [128, 4, 128], F32)
            ctmp = [sb(pa, "ctmp%d" % i, [128, 4], F32) for i in range(4)]
            xq = sb(pa, "xq", [128, 16, 4, 128], BF16)
            yg = sb(pa, "yg", [128, 512], BF16)
            ygT = sb(pa, "ygT", [128, 4, 128], BF16)
            sig = sb(pa, "sig", [128, 4, 128], BF16)
            x1t = sb(pa, "x1t", [128, D], F32)

            def front(n):
                sl = n % 2
                yield
                xt = XT[sl]
                yield
                xk = "xt%d" % sl
                zsT = ZST[sl]
                zk = "zsT%d" % sl
                ycatT = YCT[sl]
                yk = "ycatT%d" % sl
                yield
                S.dma("sync", xt[:], x_d[n * 128:(n + 1) * 128, :], writes=[xk], dsem=8 + sl)
                yield
                yield
                V(lambda e, xt=xt: e.scalar_tensor_tensor(junk[:], xt[:], 1.0, xt[:], ALU.mult, ALU.mult, accum_out=ssq[:]),
                  r=[xk], w=["junk", "ssq"])
                yield
                V(lambda e: e.tensor_scalar(ssq2[:], ssq[:], 1.0 / D, EPS, ALU.mult, ALU.add), r=["ssq"], w=["ssq2"])
                yield
                A(lambda e: e.activation(rstd[:], ssq2[:], AF.Sqrt), r=["ssq2"], w=["rstd"])
                V(lambda e: e.reciprocal(rstd[:], rstd[:]), r=["rstd"], w=["rstd"])
                yield
                V(lambda e, xt=xt: e.tensor_scalar(xn[:], xt[:], rstd[:, 0:1], None, ALU.mult), r=[xk, "rstd"], w=["xn"])
                yield
                for k in range(8):
                    PE(lambda e, k=k: e.transpose(PT[:, k * 128:(k + 1) * 128], xn[:, k * 128:(k + 1) * 128], identb[:]),
                       r=["xn", "identb"], w=["PT"], inc=(k == 7))
                yield
                for k in range(8):
                    A(lambda e, k=k: e.activation(hT[:, k, :], PT[:, k * 128:(k + 1) * 128], AF.Identity,
                                                  bias=sh1c[:, k:k + 1], scale=a1c[:, k:k + 1]),
                      r=["PT", "sh1c", "a1c"], w=["hT"])
                yield
                yield
                for half in range(2):
                    for k in range(8):
                        PE(lambda e, k=k, half=half: e.matmul(P[half][:], hT[:, k, :], win[:, k, half * 512:(half + 1) * 512],
                                                              start=(k == 0), stop=(k == 7)),
                           r=["hT", "win"], w=[PK[half]], inc=(k == 7))
                yield
                for f in range(4):
                    for k in range(8):
                        PE(lambda e, k=k, f=f: e.matmul(P[2][:, f * 128:(f + 1) * 128], win[:, k, 1024 + f * 128:1024 + (f + 1) * 128],
                                                        hT[:, k, :], start=(k == 0), stop=(k == 7)),
                           r=["hT", "win"], w=["P2"], inc=(k == 7 and f == 3))
                yield
                A(lambda e: e.activation(ug[:], P[0][:], AF.Gelu), r=["P0"], w=["ug"])
                yield
                A(lambda e: e.activation(vg[:], P[1][:], AF.Gelu), r=["P1"], w=["vg"])
                yield
                A(lambda e: e.copy(zsT[:].rearrange("p c t -> p (c t)"), P[2][:]), r=["P2"], w=[zk])
                yield
                yield
                for i in range(nst):
                    w_ = 512 // nst
                    V(lambda e, i=i, w_=w_: e.bn_stats(bst[:, i, :], vg[:, i * w_:(i + 1) * w_]), r=["vg"], w=["bst"])
                yield
                V(lambda e: e.bn_aggr(bmv[:], bst[:]), r=["bst"], w=["bmv"])
                yield
                V(lambda e: e.tensor_scalar(var2[:], bmv[:, 1:2], EPS, None, ALU.add), r=["bmv"], w=["var2"])
                yield
                A(lambda e: e.activation(rstdv[:], var2[:], AF.Sqrt), r=["var2"], w=["rstdv"])
                V(lambda e: e.reciprocal(rstdv[:], rstdv[:]), r=["rstdv"], w=["rstdv"])
                yield
                V(lambda e: e.tensor_scalar(vn[:], vg[:], bmv[:, 0:1], rstdv[:, 0:1], ALU.subtract, ALU.mult),
                  r=["vg", "bmv", "rstdv"], w=["vn"])
                yield
                V(lambda e: e.tensor_tensor(vn2[:], vn[:], lngr[:], ALU.mult), r=["vn", "lngr"], w=["vn2"])
                yield
                V(lambda e: e.tensor_tensor(vb16[:], vn2[:], lnbr[:], ALU.add), r=["vn2", "lnbr"], w=["vb16"])
                yield
                yield
                for h in range(4):
                    PE(lambda e, h=h: e.matmul(P[2][:, h * 128:(h + 1) * 128], wmT[:, h, :], vb16[:, h * 128:(h + 1) * 128],
                                               start=True, stop=True),
                       r=["wmT", "vb16"], w=["P2"], inc=(h == 3))
                yield
                for h in range(4):
                    V(lambda e, h=h: e.scalar_tensor_tensor(ygm[:, h * 128:(h + 1) * 128], P[2][:, h * 128:(h + 1) * 128],
                                                            bsc[:, h:h + 1], ug[:, h * 128:(h + 1) * 128], ALU.add, ALU.mult),
                      r=["P2", "bsc", "ug"], w=["ygm"])
                yield
                for h in range(4):
                    PE(lambda e, h=h: e.transpose(PT[:, h * 128:(h + 1) * 128], ygm[:, h * 128:(h + 1) * 128], identb[:]),
                       r=["ygm", "identb"], w=["PT"], inc=(h == 3))
                yield
                A(lambda e: e.copy(ycatT[:, 0:4, :].rearrange("p c t -> p (c t)"), PT[:, 0:512]), r=["PT"], w=[yk])
                yield

            def back(n):
                sl = n % 2
                xt = XT[sl]
                xk = "xt%d" % sl
                zsT = ZST[sl]
                zk = "zsT%d" % sl
                ycatT = YCT[sl]
                yk = "ycatT%d" % sl
                yield
                for c in range(4):
                    pr_, pi_ = (4, 5)
                    PE(lambda e, c=c, pr_=pr_: e.matmul(P[pr_][:], zsT[:, c, :], bblk[:, c, 0:512], start=True, stop=True),
                       r=[zk, "bblk"], w=[PK[pr_]], inc=False)
                    PE(lambda e, c=c, pi_=pi_: e.matmul(P[pi_][:], zsT[:, c, :], bblk[:, c, 512:1024], start=True, stop=True),
                       r=[zk, "bblk"], w=[PK[pi_]], inc=True)
                    csl = slice(c * 512, (c + 1) * 512)
                    bk_ = "bum%d" % c
                    V(lambda e, c=c, pr_=pr_, csl=csl: e.tensor_tensor(bq[:, c, 0, :], P[pr_][:], Mre[:, csl], ALU.mult),
                      r=[PK[pr_], "Mre"], w=[bk_])
                    V(lambda e, c=c, pi_=pi_, csl=csl: e.tensor_tensor(bq[:, c, 1, :], P[pi_][:], Mim[:, csl], ALU.mult),
                      r=[PK[pi_], "Mim"], w=[bk_])
                    V(lambda e, c=c, pi_=pi_, csl=csl: e.tensor_tensor(bq[:, c, 2, :], P[pi_][:], Mre[:, csl], ALU.mult),
                      r=[PK[pi_], "Mre"], w=[bk_])
                    V(lambda e, c=c, pr_=pr_, csl=csl: e.tensor_tensor(bq[:, c, 3, :], P[pr_][:], Mim[:, csl], ALU.mult),
                      r=[PK[pr_], "Mim"], w=[bk_])
                for c in range(4):
                    for (ja, jb, tb_, pb) in ((0, 1, ntrib, 6), (2, 3, trib, 3)):
                        for ql in range(4):
                            PE(lambda e, c=c, ja=ja, pb=pb, ql=ql: e.matmul(
                                P[pb][:, ql * 128:(ql + 1) * 128], bq[:, c, ja, ql * 128:(ql + 1) * 128], trib[:],
                                start=True, stop=False),
                               r=["bum%d" % c, "trib"], w=[PK[pb]], inc=False)
                            PE(lambda e, c=c, jb=jb, tb_=tb_, pb=pb, ql=ql: e.matmul(
                                P[pb][:, ql * 128:(ql + 1) * 128], bq[:, c, jb, ql * 128:(ql + 1) * 128], tb_[:],
                                start=False, stop=True),
                               r=["bum%d" % c, "trib", "ntrib"], w=[PK[pb]], inc=(ql == 3))
                    qs = slice(4 * c, 4 * c + 4)
                    V(lambda e, qs=qs: e.tensor_tensor(ure[:], P[6][:].rearrange("p (q i) -> p q i", q=4),
                                                       Kre[:, qs].unsqueeze(2).to_broadcast([128, 4, 128]), ALU.add),
                      r=["P6", "Kre"], w=["ure"])
                    V(lambda e, qs=qs: e.tensor_tensor(uim[:], P[3][:].rearrange("p (q i) -> p q i", q=4),
                                                       Kim[:, qs].unsqueeze(2).to_broadcast([128, 4, 128]), ALU.add),
                      r=["P3", "Kim"], w=["uim"])
                    ur7 = ure[:, :, 127:128].rearrange("p q o -> p (q o)")
                    ui7 = uim[:, :, 127:128].rearrange("p q o -> p (q o)")
                    G(lambda e, qs=qs, ur7=ur7: e.tensor_tensor(ctmp[0][:], A128re[:, qs], ur7, ALU.mult), r=["A128re", "ure"], w=["ct0"])
                    G(lambda e, qs=qs, ui7=ui7: e.tensor_tensor(ctmp[1][:], A128im[:, qs], ui7, ALU.mult), r=["A128im", "uim"], w=["ct1"])
                    G(lambda e, qs=qs, ui7=ui7: e.tensor_tensor(ctmp[2][:], A128re[:, qs], ui7, ALU.mult), r=["A128re", "uim"], w=["ct2"])
                    G(lambda e, qs=qs, ur7=ur7: e.tensor_tensor(ctmp[3][:], A128im[:, qs], ur7, ALU.mult), r=["A128im", "ure"], w=["ct3"])
                    G(lambda e, qs=qs: e.tensor_tensor(Kre[:, qs], ctmp[0][:], ctmp[1][:], ALU.subtract), r=["ct0", "ct1"], w=["Kre"])
                    G(lambda e, qs=qs: e.tensor_tensor(Kim[:, qs], ctmp[2][:], ctmp[3][:], ALU.add), r=["ct2", "ct3"], w=["Kim"])
                    xk_ = "xs%d" % c
                    u3r = ure[:]
                    u3i = uim[:]
                    V(lambda e, qs=qs, u3r=u3r: e.tensor_tensor(xq[:, qs, 0, :], u3r, Pre[:, qs, :], ALU.mult), r=["ure", "Pre"], w=[xk_])
                    V(lambda e, qs=qs, u3i=u3i: e.tensor_tensor(xq[:, qs, 1, :], u3i, Pim[:, qs, :], ALU.mult), r=["uim", "Pim"], w=[xk_])
                    G(lambda e, qs=qs, u3i=u3i: e.tensor_tensor(xq[:, qs, 2, :], u3i, Pre[:, qs, :], ALU.mult), r=["uim", "Pre"], w=[xk_])
                    G(lambda e, qs=qs, u3r=u3r: e.tensor_tensor(xq[:, qs, 3, :], u3r, Pim[:, qs, :], ALU.mult), r=["ure", "Pim"], w=[xk_])
                    yield
                yield
                for c in range(4):
                    PE(lambda e, c=c: e.matmul(P[4][:, c * 128:(c + 1) * 128], zsT[:, c, :], ddb[:, c, :], start=True, stop=False,
                                               skip_group_check=True),
                       r=[zk, "ddb"], w=["P4"], inc=False)
                    for ql in range(4):
                        q = 4 * c + ql
                        for (j_, ct_, ck_) in ((0, crb, "crb"), (1, ncrb, "ncrb"), (2, cinb, "cinb"), (3, cinb, "cinb")):
                            PE(lambda e, q=q, j_=j_, ct_=ct_: e.matmul(P[4][:, q * 32:(q + 1) * 32], xq[:, q, j_, :], ct_[:, q, :],
                                                                       start=False, stop=(j_ == 3), skip_group_check=True),
                               r=["xs%d" % c, ck_], w=["P4"], inc=(ql == 3 and j_ == 3))
                yield
                A(lambda e: e.activation(yg[:], P[4][:], AF.Gelu), r=["P4"], w=["yg"])
                yield
                for c in range(4):
                    PE(lambda e, c=c: e.transpose(PT[:, 512 + c * 128:512 + (c + 1) * 128], yg[:, c * 128:(c + 1) * 128], identb[:]),
                       r=["yg", "identb"], w=["PT"], inc=(c == 3))
                yield
                V(lambda e: e.tensor_copy(ygT[:].rearrange("p c t -> p (c t)"), PT[:, 512:1024]), r=["PT"], w=["ygT"])
                yield
                for fo in range(4):
                    for c in range(4):
                        PE(lambda e, fo=fo, c=c: e.matmul(P[6][:, fo * 128:(fo + 1) * 128], wglu[:, c, fo * 128:(fo + 1) * 128], ygT[:, c, :],
                                                          start=(c == 0), stop=(c == 3)),
                           r=["wglu", "ygT"], w=["P6"], inc=(c == 3 and fo == 3))
                yield
                for fo in range(4):
                    A(lambda e, fo=fo: e.activation(sig[:, fo, :], P[6][:, fo * 128:(fo + 1) * 128], AF.Sigmoid, bias=bgluc[:, fo:fo + 1]),
                      r=["P6", "bgluc"], w=["sig"])
                yield
                V(lambda e: e.tensor_tensor(ycatT[:, 4:8, :], ygT[:], sig[:], ALU.mult), r=["ygT", "sig"], w=[yk])
                yield
                yield
                for half in range(2):
                    pb = 4 + half
                    for k in range(8):
                        PE(lambda e, k=k, half=half, pb=pb: e.matmul(P[pb][:], ycatT[:, k, :], wout[:, k, half * 512:(half + 1) * 512],
                                                                     start=(k == 0), stop=(k == 7)),
                           r=[yk, "wout"], w=[PK[pb]], inc=(k == 7))
                    V(lambda e, half=half, pb=pb, xt=xt: e.tensor_tensor(x1t[:, half * 512:(half + 1) * 512], P[pb][:],
                                                                         xt[:, half * 512:(half + 1) * 512], ALU.add),
                      r=[PK[pb], xk], w=["x1t"])
                yield
                S.dma("sync", x1s_d[n * 128:(n + 1) * 128, :], x1t[:], reads=["x1t"], writes=["x1s"], dsem=10)
                yield
                if debug and n == 0:
                    dump("hT", hT[:], "hT", [128, 8, 128], BF16)
                    dump("ug", ug[:], "ug", [128, 512])
                    dump("vb16", vb16[:], "vb16", [128, 512], BF16)
                    dump("ygm", ygm[:], "ygm", [128, 512], BF16)
                    dump("zsT", zsT[:], zk, [128, 4, 128], BF16)
                    dump("yg", yg[:], "yg", [128, 512], BF16)
                    dump("ycatT", ycatT[:], yk, [128, 8, 128], BF16)
                    dump("x1t", x1t[:], "x1t", [128, D])
                yield

            def drain(g):
                for _ in g:
                    pass

            drain(front(0))
            for n in range(NCH):
                fg = front(n + 1) if n + 1 < NCH else iter(())
                for _ in back(n):
                    next(fg, None)
                    next(fg, None)
                    next(fg, None)
                drain(fg)
            S.wait_all("sync", ["x1s"])
            S.flush()

        with ExitStack() as pb_:
            wq = sb(pb_, "wq", [128, 8, D], BF16)
            keysb = sb(pb_, "keysb", [128, 8, 256], BF16)
            S.dma("gpsimd", wq[:], wq_d.rearrange("(k p) n -> p k n", p=128), writes=["wq"], dsem=21)
            S.dma("gpsimd", keysb[:], keys_d, writes=["keysb"], dsem=21)
            x1b = [sb(pb_, "x1b%d" % i, [128, NSUB, D], F32) for i in range(2)]
            h2T = [sb(pb_, "h2T%d" % i, [128, 8, TB], BF16) for i in range(2)]
            qT = sb(pb_, "qT", [128, 8, TB], BF16)
            gT = sb(pb_, "gT", [128, TB], BF16)
            ihT = sb(pb_, "ihT", [128, TB], BF16)
            jlT = sb(pb_, "jlT", [128, TB], BF16)
            Wsel = sb(pb_, "Wsel", [128, TB, 128], BF16)
            ssb = sb(pb_, "ssb", [128, 1], F32)
            ssb2 = sb(pb_, "ssb2", [128, 1], F32)
            rstb = sb(pb_, "rstb", [128, 1], F32)
            ssr = sb(pb_, "ssr", [128, 1], F32)
            ssr2 = sb(pb_, "ssr2", [128, 1], F32)
            rstr = sb(pb_, "rstr", [128, 1], F32)
            pt1 = sb(pb_, "pt1", [128, D], F32)
            junk2 = sb(pb_, "junk2", [128, D], BF16)
            TS = 8
            xn2 = sb(pb_, "xn2", [128, D], BF16)
            scs = sb(pb_, "scs", [128, 16, 128], F32)
            work = [sb(pb_, "work%d" % i, [128, 256], F32) for i in range(2)]
            v1 = sb(pb_, "v1", [128, 16, 16], F32)
            i1 = sb(pb_, "i1", [128, 16, 16], U32)
            i1f = sb(pb_, "i1f", [128, 16, 16], F32)
            cand = sb(pb_, "cand", [128, 8, 256], F32)
            b1 = sb(pb_, "b1", [128, 8, 16], F32)
            pos = sb(pb_, "pos", [128, 8, 16], U32)
            posa = sb(pb_, "posa", [128, 8, 16], U32)
            posb = sb(pb_, "posb", [128, 8, 16], U32)
            posaf = sb(pb_, "posaf", [128, 8, 16], F32)
            posbf = sb(pb_, "posbf", [128, 8, 16], F32)
            oh = sb(pb_, "oh", [128, 8, 16, 16], BF16)
            sm = sb(pb_, "sm", [128, 8, 16], F32)
            smz = sb(pb_, "smz", [128, 8], F32)
            gw = sb(pb_, "gw", [128, 128], F32)
            ihi = sb(pb_, "ihi", [128, 128], F32)
            jlo = sb(pb_, "jlo", [128, 128], F32)
            OA = [sb(pb_, "OA%d" % i, [128, TS, 128], BF16) for i in range(2)]
            OBw = [sb(pb_, "OBw%d" % i, [128, TS, 128], BF16) for i in range(2)]
            ub = [sb(pb_, "ub%d" % i, [128, 8, GI * 128], BF16) for i in range(2)]
            vbuf = [sb(pb_, "vbuf%d" % i, [128, GI, D], BF16) for i in range(2)]
            actg = [sb(pb_, "actg%d" % i, [128, TB], BF16) for i in range(2)]
            wf = [sb(pb_, "wf%d" % i, [128, TB], BF16) for i in range(2)]
            nact = [0]

            def routing_stages(blk):
                hb = blk % 2
                t0 = blk * TB
                xb = x1b[hb]
                xbk = "x1b%d" % hb
                hT_ = h2T[hb]
                hk = "h2T%d" % hb
                st = []

                def add(f):
                    st.append(f)

                add(lambda: S.dma("sync", xb[:], x1s_d[t0:t0 + TB, :].rearrange("(s p) d -> p s d", p=128),
                                  reads=["x1s"], writes=[xbk], dsem=18 + hb))
                for s in range(NSUB):
                    tsl = slice(s * 128, (s + 1) * 128)

                    def f_rms(s=s):
                        V(lambda e: e.scalar_tensor_tensor(junk2[:], xb[:, s, :], 1.0, xb[:, s, :], ALU.mult, ALU.mult, accum_out=ssr[:]),
                          r=[xbk], w=["junk2", "ssr"])
                        V(lambda e: e.tensor_scalar(ssr2[:], ssr[:], 1.0 / D, EPS, ALU.mult, ALU.add), r=["ssr"], w=["ssr2"])
                        G(lambda e: e.tensor_tensor(rstr[:], ssr2[:], mhalf[:], ALU.pow), r=["ssr2", "mhalf"], w=["rstr"])
                    add(f_rms)
                    add(lambda s=s: V(lambda e: e.tensor_scalar(xn2[:], xb[:, s, :], rstr[:, 0:1], None, ALU.mult), r=[xbk, "rstr"], w=["xn2"]))

                    def f_tr():
                        for k in range(8):
                            PE(lambda e, k=k: e.transpose(PT[:, k * 128:(k + 1) * 128], xn2[:, k * 128:(k + 1) * 128], identb[:]),
                               r=["xn2", "identb"], w=["PT"], inc=(k == 7))
                    add(f_tr)
                    for k0 in (0, 4):
                        def f_ev(k0=k0, tsl=tsl):
                            for k in range(k0, k0 + 4):
                                A(lambda e, k=k: e.activation(hT_[:, k, tsl], PT[:, k * 128:(k + 1) * 128], AF.Identity,
                                                              bias=sh2c[:, k:k + 1], scale=a2c[:, k:k + 1]),
                                  r=["PT", "sh2c", "a2c"], w=[hk])
                        add(f_ev)
                for m in range(8):
                    hsl = slice((m % 2) * 256, (m % 2) * 256 + TB)
                    pk6 = "P6"

                    def f_q(m=m, hsl=hsl, pk6=pk6):
                        for k in range(8):
                            PE(lambda e, k=k: e.matmul(P[6][:, hsl], wq[:, k, m * 128:(m + 1) * 128], hT_[:, k, :],
                                                       start=(k == 0), stop=(k == 7)),
                               r=["wq", hk], w=[pk6], inc=(k == 7))
                    add(f_q)
                    add(lambda m=m, hsl=hsl, pk6=pk6: A(lambda e: e.copy(qT[:, m, :], P[6][:, hsl]), r=[pk6], w=["qT"]))
                for s in range(NSUB):
                    tsl = slice(s * 128, (s + 1) * 128)
                    for h in range(8):
                        hsl = slice((h % 2) * 256, (h % 2 + 1) * 256)
                        pk6 = "P6"

                        def f_sc(h=h, hsl=hsl, pk6=pk6, tsl=tsl):
                            PE(lambda e: e.matmul(P[6][:, hsl], qT[:, h, tsl], keysb[:, h, :], start=True, stop=True),
                               r=["qT", "keysb"], w=[pk6])
                            A(lambda e: e.copy(scs[:, 2 * h:2 * h + 2, :].rearrange("p a k -> p (a k)"), P[6][:, hsl]),
                              r=[pk6], w=["scs%d" % (2 * h), "scs%d" % (2 * h + 1)])
                        add(f_sc)
                    for hc in range(16):
                        wk = work[hc % 2]
                        wkk = "work%d" % (hc % 2)
                        sk = "scs%d" % hc
                        vk = "v1_%d" % hc
                        ik = "i1_%d" % hc

                        def f_a(hc=hc, wk=wk, wkk=wkk, sk=sk, vk=vk):
                            V(lambda e: e.max(v1[:, hc, 0:8], scs[:, hc, :]), r=[sk], w=[vk])
                            V(lambda e: e.match_replace(wk[:, 0:128], v1[:, hc, 0:8], scs[:, hc, :], -1e30), r=[sk, vk], w=[wkk])
                            V(lambda e: e.max(v1[:, hc, 8:16], wk[:, 0:128]), r=[wkk], w=[vk])
                        add(f_a)

                        def f_b(hc=hc, sk=sk, vk=vk, ik=ik):
                            V(lambda e: e.max_index(i1[:, hc, 0:8], v1[:, hc, 0:8], scs[:, hc, :]), r=[sk, vk], w=[ik])
                            V(lambda e: e.max_index(i1[:, hc, 8:16], v1[:, hc, 8:16], scs[:, hc, :]), r=[sk, vk], w=[ik])
                        add(f_b)
                    i1keys = ["i1_%d" % hc for hc in range(16)]
                    add(lambda: V(lambda e: e.tensor_copy(i1f[:], i1[:]), r=i1keys, w=["i1f"]))
                    v1v = v1[:].rearrange("p (h c) k -> p h c k", c=2)
                    i1v = i1f[:].rearrange("p (h c) k -> p h c k", c=2)
                    for h in range(8):
                        wk = work[h % 2]
                        wkk = "work%d" % (h % 2)
                        ck = "cand%d" % h
                        bk = "b1_%d" % h
                        pk_ = "pos%d" % h

                        def f_c(h=h, wk=wk, wkk=wkk, ck=ck, bk=bk):
                            V(lambda e: e.tensor_tensor(cand[:, h, :].rearrange("p (a b) -> p a b", a=16),
                                                        v1v[:, h, 0, :].unsqueeze(2).to_broadcast([128, 16, 16]),
                                                        v1v[:, h, 1, :].unsqueeze(1).to_broadcast([128, 16, 16]), ALU.add),
                              r=["v1_%d" % (2 * h), "v1_%d" % (2 * h + 1)], w=[ck])
                            V(lambda e: e.max(b1[:, h, 0:8], cand[:, h, :]), r=[ck], w=[bk])
                            V(lambda e: e.match_replace(wk[:], b1[:, h, 0:8], cand[:, h, :], -1e30), r=[ck, bk], w=[wkk])
                        add(f_c)

                        def f_d(h=h, wk=wk, wkk=wkk, ck=ck, bk=bk, pk_=pk_):
                            V(lambda e: e.max(b1[:, h, 8:16], wk[:]), r=[wkk], w=[bk])
                            V(lambda e: e.max_index(pos[:, h, 0:8], b1[:, h, 0:8], cand[:, h, :]), r=[ck, bk], w=[pk_])
                            V(lambda e: e.max_index(pos[:, h, 8:16], b1[:, h, 8:16], cand[:, h, :]), r=[ck, bk], w=[pk_])
                        add(f_d)
                    b1keys = ["b1_%d" % h for h in range(8)]
                    poskeys = ["pos%d" % h for h in range(8)]

                    def f_sm():
                        V(lambda e: e.tensor_tensor(sm[:], b1[:], b1[:, :, 0:1].to_broadcast([128, 8, 16]), ALU.subtract), r=b1keys, w=["sm"])
                        A(lambda e: e.activation(sm[:], sm[:], AF.Exp), r=["sm"], w=["sm"])
                    add(f_sm)

                    def f_sm2():
                        V(lambda e: e.tensor_reduce(smz[:], sm[:], AX.X, ALU.add), r=["sm"], w=["smz"])
                        V(lambda e: e.reciprocal(smz[:], smz[:]), r=["smz"], w=["smz"])
                        V(lambda e: e.tensor_tensor(gw[:].rearrange("p (h k) -> p h k", h=8), sm[:],
                                                    smz[:].unsqueeze(2).to_broadcast([128, 8, 16]), ALU.mult),
                          r=["sm", "smz"], w=["gw"])
                    add(f_sm2)

                    def f_pos():
                        V(lambda e: e.tensor_single_scalar(posa[:], pos[:], 4, ALU.logical_shift_right), r=poskeys, w=["posa"])
                        V(lambda e: e.tensor_single_scalar(posb[:], pos[:], 15, ALU.bitwise_and), r=poskeys, w=["posb"])
                        V(lambda e: e.tensor_copy(posaf[:], posa[:]), r=["posa"], w=["posaf"])
                        V(lambda e: e.tensor_copy(posbf[:], posb[:]), r=["posb"], w=["posbf"])
                    add(f_pos)
                    io16 = iota_f[:, 0:16]
                    for (pf, pk_, cc, dst, dk) in ((posaf, "posaf", 0, ihi, "ihi"), (posbf, "posbf", 1, jlo, "jlo")):
                        for h in range(8):
                            def f_oh(h=h, pf=pf, pk_=pk_, cc=cc):
                                V(lambda e: e.tensor_tensor(oh[:, h, :, :], pf[:, h, :].unsqueeze(2).to_broadcast([128, 16, 16]),
                                                            io16.unsqueeze(1).to_broadcast([128, 16, 16]), ALU.is_equal),
                                  r=[pk_, "iota_f"], w=["oh%d" % h])
                                G(lambda e: e.tensor_tensor(oh[:, h, :, :], oh[:, h, :, :],
                                                            i1v[:, h, cc, :].unsqueeze(1).to_broadcast([128, 16, 16]), ALU.mult),
                                  r=["oh%d" % h, "i1f"], w=["oh%d" % h])
                            add(f_oh)
                        ohkeys = ["oh%d" % h for h in range(8)]
                        add(lambda dst=dst, dk=dk, ohkeys=ohkeys: V(
                            lambda e: e.tensor_reduce(dst[:], oh[:].rearrange("p h k a -> p (h k) a"), AX.X, ALU.add), r=ohkeys, w=[dk]))
                    if debug and blk == 0 and s == 0:
                        def f_dbg():
                            dump("scs", scs[:], "scs0", [128, 16, 128])
                            dump("gw", gw[:], "gw", [128, 128])
                            dump("ihi", ihi[:], "ihi", [128, 128])
                            dump("jlo", jlo[:], "jlo", [128, 128])
                        add(f_dbg)
                    for (src, sk_, dstT, dk) in ((gw, "gw", gT, "gT"), (ihi, "ihi", ihT, "ihT"), (jlo, "jlo", jlT, "jlT")):
                        def f_t(src=src, sk_=sk_, dstT=dstT, dk=dk, tsl=tsl):
                            PE(lambda e: e.transpose(P[6][:, 0:128], src[:], identf[:]), r=[sk_, "identf"], w=["P6"])
                            A(lambda e: e.copy(dstT[:, tsl], P[6][:, 0:128]), r=["P6"], w=[dk])
                        add(f_t)
                return st

            def wsel_build(blk):
                for sbk in range(TB // TS):
                    o = sbk % 2
                    ts0 = sbk * TS
                    io3 = iota_b[:].unsqueeze(1).to_broadcast([128, TS, 128])
                    V(lambda e, o=o, ts0=ts0, io3=io3: e.tensor_tensor(OA[o][:], io3, jlT[:, ts0:ts0 + TS].unsqueeze(2).to_broadcast([128, TS, 128]),
                                                                       ALU.is_equal),
                      r=["iota_b", "jlT"], w=["OA%d" % o])
                    V(lambda e, o=o, ts0=ts0, io3=io3: e.tensor_tensor(OBw[o][:], io3, ihT[:, ts0:ts0 + TS].unsqueeze(2).to_broadcast([128, TS, 128]),
                                                                       ALU.is_equal),
                      r=["iota_b", "ihT"], w=["OBw%d" % o])
                    V(lambda e, o=o, ts0=ts0: e.tensor_tensor(OBw[o][:], OBw[o][:], gT[:, ts0:ts0 + TS].unsqueeze(2).to_broadcast([128, TS, 128]),
                                                              ALU.mult),
                      r=["OBw%d" % o, "gT"], w=["OBw%d" % o])
                    for tl in range(TS):
                        pbk = 4 + (tl // 4) % 2
                        PE(lambda e, o=o, tl=tl, pbk=pbk: e.matmul(P[pbk][:, (tl % 4) * 128:(tl % 4 + 1) * 128], OA[o][:, tl, :], OBw[o][:, tl, :],
                                                                   start=True, stop=True),
                           r=["OA%d" % o, "OBw%d" % o], w=[PK[pbk]], inc=(tl % 4 == 3))
                        if tl % 4 == 3:
                            tb0 = ts0 + tl - 3
                            A(lambda e, pbk=pbk, tb0=tb0: e.copy(Wsel[:, tb0:tb0 + 4, :].rearrange("p t i -> p (t i)"), P[pbk][:]),
                              r=[PK[pbk]], w=["Wsel"])
                if debug and blk == 0:
                    dump("Wsel", Wsel[:, 0:8, :], "Wsel", [128, 8, 128], BF16)
                    dump("h2T", h2T[0][:], "h2T0", [128, 8, TB], BF16)

            def expert_loop(blk, stages):
                hb = blk % 2
                hT_ = h2T[hb]
                hk = "h2T%d" % hb
                nst = len(stages)
                per = (nst + 111) // 112 if nst else 0
                rate = nst / 118.0
                credit = [0.0]
                sp = [0]

                def run_stages(n):
                    while n > 0 and sp[0] < nst:
                        stages[sp[0]]()
                        sp[0] += 1
                        n -= 1

                def Vm(i):
                    o = (i // GI) % 2
                    il = i % GI
                    a_ = i % 2
                    for s in range(NSUB):
                        for half in range(2):
                            pbk = s * 2 + half
                            PE(lambda e, a_=a_, s=s, half=half, pbk=pbk, o=o, il=il, i=i: e.matmul(
                                P[pbk][:], wf[a_][:, s * 128:(s + 1) * 128], vbuf[o][:, il, half * 512:(half + 1) * 512],
                                start=(i == 0), stop=(i == 127)),
                               r=["wf%d" % a_, "vbuf%d" % o], w=[PK[pbk]], inc=(s == NSUB - 1 and half == 1))

                for i in range(128):
                    ig = i // GI
                    il = i % GI
                    o = ig % 2
                    a_ = i % 2
                    pa_ = 4 + a_
                    if il == 0:
                        S.dma("sync", ub[o][:], uTg_d[ig], reads=["uTg"], writes=["ub%d" % o], dsem=12 + o)
                        S.dma("sync", vbuf[o][:], vg_d[ig], reads=["vg"], writes=["vbuf%d" % o], dsem=14 + o)
                    for k in range(8):
                        PE(lambda e, o=o, il=il, k=k, pa_=pa_: e.matmul(P[pa_][:, 0:TB], ub[o][:, k, il * 128:(il + 1) * 128], hT_[:, k, :],
                                                                        start=(k == 0), stop=(k == 7)),
                           r=["ub%d" % o, hk], w=[PK[pa_]], inc=(k == 7))
                    A(lambda e, a_=a_, pa_=pa_: e.activation(actg[a_][:], P[pa_][:, 0:TB], AF.Gelu), r=[PK[pa_]], w=["actg%d" % a_])
                    G(lambda e, a_=a_, i=i: e.tensor_tensor(wf[a_][:], actg[a_][:], Wsel[:, :, i], ALU.mult),
                      r=["actg%d" % a_, "Wsel"], w=["wf%d" % a_])
                    if i >= 1:
                        Vm(i - 1)
                    if i >= 2:
                        credit[0] += rate
                        k_ = int(credit[0])
                        credit[0] -= k_
                        run_stages(k_)
                Vm(127)
                run_stages(nst)

            def final_evac(blk):
                hb = blk % 2
                t0 = blk * TB
                xb = x1b[hb]
                xbk = "x1b%d" % hb
                for s in range(NSUB):
                    for half in range(2):
                        pbk = s * 2 + half
                        hs = slice(half * 512, (half + 1) * 512)
                        V(lambda e, pbk=pbk, hs=hs: e.tensor_tensor(pt1[:, hs], P[pbk][:], gt2r[:, hs], ALU.mult), r=[PK[pbk], "gtr1"], w=["pt1"])
                    if debug and blk == 0 and s == 0:
                        dump("peer", pt1[:], "pt1", [128, D])
                    G(lambda e, s=s: e.tensor_tensor(pt1[:], pt1[:], xb[:, s, :], ALU.add), r=["pt1", xbk], w=["pt1"])
                    V(lambda e: e.scalar_tensor_tensor(junk2[:], pt1[:], 1.0, pt1[:], ALU.mult, ALU.mult, accum_out=ssb[:]),
                      r=["pt1"], w=["junk2", "ssb"])
                    V(lambda e: e.tensor_scalar(ssb2[:], ssb[:], 1.0 / D, EPS, ALU.mult, ALU.add), r=["ssb"], w=["ssb2"])
                    G(lambda e: e.tensor_tensor(rstb[:], ssb2[:], mhalf[:], ALU.pow), r=["ssb2", "mhalf"], w=["rstb"])
                    V(lambda e: e.scalar_tensor_tensor(pt1[:], pt1[:], rstb[:, 0:1], gfr[:], ALU.mult, ALU.mult),
                      r=["pt1", "rstb", "gfr"], w=["pt1"])
                    S.dma("sync", out_d[t0 + s * 128:t0 + (s + 1) * 128, :], pt1[:], reads=["pt1"], writes=["out"], dsem=16)

            if INTERLEAVE:
                for f in routing_stages(0):
                    f()
                for blk in range(NBLK):
                    wsel_build(blk)
                    nxt = routing_stages(blk + 1) if blk + 1 < NBLK else []
                    expert_loop(blk, nxt)
                    final_evac(blk)
            else:
                for blk in range(NBLK):
                    for f in routing_stages(blk):
                        f()
                    wsel_build(blk)
                    expert_loop(blk, [])
                    final_evac(blk)
            S.wait_all("sync", ["out"] + list(dbg_outs.keys()))
            S.flush()
    return nc, S.n_ins


def prep_shared(inp):
    f = np.float32
    sh = {}
    sh["w_ada"] = np.ascontiguousarray(inp["w_ada"][0], f)
    sh["b_ada_c"] = np.ascontiguousarray(inp["b_ada"][0].reshape(48, 128).T, f)
    sh["b_ada_r"] = np.ascontiguousarray(inp["b_ada"][0].reshape(1, 6 * D), f)
    sh["g_mix_c"] = np.ascontiguousarray(inp["g_mix"][0].reshape(8, 128).T, f)
    sh["g_ffn_c"] = np.ascontiguousarray(inp["g_ffn"][0].reshape(8, 128).T, f)
    sh["w_in"] = np.ascontiguousarray(inp["w_in"][0], f)
    sh["w_out"] = np.ascontiguousarray(inp["w_out"][0], f)
    sh["w_q"] = np.ascontiguousarray(inp["w_q"][0], f)
    sh["w_glu"] = np.ascontiguousarray(inp["w_glu"][0], f)
    sh["ln_g_r"] = np.ascontiguousarray(inp["sgu_ln_g"][0].reshape(1, 512), f)
    sh["ln_b_r"] = np.ascontiguousarray(inp["sgu_ln_b"][0].reshape(1, 512), f)
    sh["b_glu_c"] = np.ascontiguousarray(inp["b_glu"][0].reshape(4, 128).T, f)
    sh["w_sT"] = np.ascontiguousarray(np.transpose(inp["w_s"][0], (2, 0, 1)), f)
    sh["b_s_c"] = np.ascontiguousarray(inp["b_s"][0].T, f)
    a_re = np.asarray(inp["ssm_a_re"][0], f)
    a_im = np.asarray(inp["ssm_a_im"][0], f)
    ldt = np.repeat(np.asarray(inp["ssm_log_dt"][0], f)[:, None], 64, axis=1)
    sh["a_re_r"] = np.ascontiguousarray(a_re.reshape(1, 2048))
    sh["a_im_r"] = np.ascontiguousarray(a_im.reshape(1, 2048))
    sh["ldt_r"] = np.ascontiguousarray(ldt.reshape(1, 2048))

    def cols(a):
        return np.ascontiguousarray(a.reshape(16, 2, 64).transpose(1, 2, 0).reshape(128, 16))

    sh["a_re_c"] = cols(a_re)
    sh["a_im_c"] = cols(a_im)
    sh["ldt_c"] = cols(ldt)
    b_re = np.asarray(inp["ssm_b_re"][0], f)
    b_im = np.asarray(inp["ssm_b_im"][0], f)
    BR = np.zeros((8, 16, 4, 8, 64), f)
    BI = np.zeros((8, 16, 4, 8, 64), f)
    for c in range(4):
        for gl in range(8):
            BR[gl, :, c, gl, :] = b_re[8 * c + gl].T
            BI[gl, :, c, gl, :] = b_im[8 * c + gl].T
    sh["BR"] = BR.reshape(128, 4, 512)
    sh["BI"] = BI.reshape(128, 4, 512)
    c_re = np.asarray(inp["ssm_c_re"][0], f)
    c_im = np.asarray(inp["ssm_c_im"][0], f)
    CR = np.zeros((2, 64, 16, 2, 16), f)
    CI = np.zeros((2, 64, 16, 2, 16), f)
    for q in range(16):
        for gg in range(2):
            CR[gg, :, q, gg, :] = c_re[2 * q + gg].T
            CI[gg, :, q, gg, :] = c_im[2 * q + gg].T
    sh["CR"] = CR.reshape(128, 16, 32)
    sh["CI"] = CI.reshape(128, 16, 32)
    dsk = np.asarray(inp["ssm_d"][0], f).reshape(4, 128)
    Dd = np.zeros((128, 4, 128), f)
    for c in range(4):
        Dd[np.arange(128), c, np.arange(128)] = dsk[c]
    sh["Dd"] = Dd
    keys = np.asarray(inp["peer_keys"][0], f)
    kb = np.zeros((2, 64, 8, 2, 128), f)
    for h in range(8):
        for c in range(2):
            kb[c, :, h, c, :] = keys[h, c].T
    sh["keysblk"] = kb.reshape(128, 8, 256)
    sh["uT"] = np.ascontiguousarray(np.asarray(inp["peer_u"][0], f).T)
    sh["v"] = np.ascontiguousarray(inp["peer_v"][0], f)
    sh["g_final_r"] = np.ascontiguousarray(np.asarray(inp["g_final"], f).reshape(1, D))
    return sh


def make_in_maps(inp, T):
    sh = prep_shared(inp)
    maps = []
    for b in range(NCORES):
        m = dict(sh)
        m["x"] = np.ascontiguousarray(inp["x"][b, :T], np.float32)
        m["cvec"] = np.ascontiguousarray(np.asarray(inp["c"][b], np.float32).reshape(8, 128).T)
        maps.append(m)
    return maps


_CACHE = {}


def kernel(**inputs):
    inputs = {k: np.asarray(v) for k, v in inputs.items()}
    T = inputs["x"].shape[1]
    if T not in _CACHE:
        _CACHE[T] = build_program(T)[0]
    nc = _CACHE[T]
    maps = make_in_maps(inputs, T)
    res = run_bass_kernel_spmd(nc, maps, core_ids=list(range(NCORES)))
    out = np.stack([np.asarray(res.results[b]["out"], np.float32) for b in range(NCORES)], axis=0)
    return out
```
